# Optimizing a Trainium2 kernel written in Bass

```python
import math
import jax, jax.numpy as jnp
from jax import lax
import numpy as np

D_MODEL = 1024
BATCH = 8
SEQ = 8192
DEPTH = 2

N_MIXERS = 2
HGRN_EXPAND = 128
HGRN_HEADS = D_MODEL // HGRN_EXPAND
HGRN_CHUNK = 64
DIFF_HEADS = 8
DIFF_HEAD_DIM = D_MODEL // DIFF_HEADS // 2
Q_BLOCK = 128
FFN_DIM = 2816
N_EXPERTS = 8
TOP_K = 2
EXPERT_DIM = 3584
MOE_SEQ_BLOCK = 128
NORM_EPS = 1e-6

kernel_name = "hgrn2_diffattn_moe_hybrid"

F32 = jnp.float32


def rms_norm(x, g):
    xf = x.astype(F32)
    y = xf * lax.rsqrt(jnp.mean(xf * xf, axis=-1, keepdims=True) + NORM_EPS)
    return (y * g.astype(F32)).astype(x.dtype)


def diff_lambda_init(layer_idx):
    return 0.8 - 0.6 * math.exp(-0.3 * layer_idx)


def alibi_slopes(n_heads):
    return 2.0 ** (-8.0 * jnp.arange(1, n_heads + 1, dtype=F32) / n_heads)


def hgrn2_mixer(h, w_in, out_norm, w_out, lower_bound):
    B, T, _ = h.shape
    H, dh, C = HGRN_HEADS, HGRN_EXPAND, HGRN_CHUNK
    nC = T // C
    q, f, v, g = jnp.split(h @ w_in, 4, axis=-1)
    q = jax.nn.silu(q.astype(F32))
    lb = lower_bound.astype(F32)
    forget = lb + (1.0 - lb) * jax.nn.sigmoid(f.astype(F32))
    k = 1.0 - forget
    log_f = jnp.log(forget)

    def chunks(a):
        return a.astype(F32).reshape(B, nC, C, H, dh).transpose(1, 0, 3, 2, 4)

    causal = jnp.tril(jnp.ones((C, C), dtype=bool))[None, None, :, :, None]

    def step(S, inp):
        qc, kc, vc, lc = inp
        b = jnp.cumsum(lc, axis=2)
        rel = b[:, :, :, None, :] - b[:, :, None, :, :]
        decay = jnp.exp(jnp.where(causal, rel, -jnp.inf))
        scores = jnp.einsum('bhtk,bhsk,bhtsk->bhts', qc, kc, decay)
        o = (jnp.einsum('bhts,bhsv->bhtv', scores, vc)
             + jnp.einsum('bhtk,bhkv->bhtv', qc * jnp.exp(b), S))
        b_end = b[:, :, -1:, :]
        S = (jnp.exp(b_end[:, :, 0, :])[..., None] * S
             + jnp.einsum('bhsk,bhsv->bhkv', kc * jnp.exp(b_end - b), vc))
        return S, o

    S0 = jnp.zeros((B, H, dh, dh), F32)
    _, o = lax.scan(step, S0, (chunks(q), chunks(k), chunks(v), chunks(log_f)))
    o = o.transpose(1, 0, 3, 2, 4).reshape(B, T, H, dh)
    o = rms_norm(o, out_norm.reshape(H, dh)) * jax.nn.silu(g.astype(F32)).reshape(B, T, H, dh)
    return o.reshape(B, T, D_MODEL).astype(h.dtype) @ w_out


def diff_attention_mixer(h, w_in, q_norm, k_norm, lq1, lk1, lq2, lk2, sub_norm, w_out, lambda_init):
    B, T, _ = h.shape
    H, dh, QB = DIFF_HEADS, DIFF_HEAD_DIM, Q_BLOCK
    nQ = T // QB
    q, k, v = jnp.split(h @ w_in, 3, axis=-1)
    q = rms_norm(q.reshape(B, T, H, 2, dh), q_norm)
    k = rms_norm(k.reshape(B, T, H, 2, dh), k_norm)
    v = v.reshape(B, T, H, 2 * dh)
    lam = (jnp.exp(jnp.sum(lq1.astype(F32) * lk1.astype(F32)))
           - jnp.exp(jnp.sum(lq2.astype(F32) * lk2.astype(F32))) + lambda_init)
    k_t = k.transpose(0, 2, 3, 1, 4).astype(F32)
    v_t = v.transpose(0, 2, 1, 3).astype(F32)
    q_blocks = q.reshape(B, nQ, QB, H, 2, dh).transpose(1, 0, 3, 4, 2, 5).astype(F32)
    slopes = alibi_slopes(H)[None, :, None, None, None]
    key_pos = jnp.arange(T)
    scale = dh ** -0.5

    def attend(args):
        qb, blk = args
        dist = (blk * QB + jnp.arange(QB))[:, None] - key_pos[None, :]
        logits = (jnp.einsum('bhcqd,bhckd->bhcqk', qb, k_t) * scale
                  - slopes * dist.astype(F32))
        logits = jnp.where(dist >= 0, logits, -jnp.inf)
        p = jax.nn.softmax(logits, axis=-1)
        w = p[:, :, 0] - lam * p[:, :, 1]
        return jnp.einsum('bhqk,bhkv->bhqv', w, v_t)

    o = lax.map(attend, (q_blocks, jnp.arange(nQ)))
    o = o.transpose(1, 0, 3, 2, 4).reshape(B, T, H, 2 * dh)
    o = rms_norm(o, sub_norm) * (1.0 - lambda_init)
    return o.reshape(B, T, D_MODEL).astype(h.dtype) @ w_out


def swiglu(h, w_gate_up, w_down):
    a, b = jnp.split(h @ w_gate_up, 2, axis=-1)
    return (jax.nn.silu(a) * b) @ w_down


def moe_swiglu(h, router, w_gate_up, w_down):
    B, T, Dm = h.shape
    nb = T // MOE_SEQ_BLOCK
    logits = (h @ router).astype(F32)
    top_val, top_idx = lax.top_k(logits, TOP_K)
    top_w = jax.nn.softmax(top_val, axis=-1)
    gates = jnp.sum(jax.nn.one_hot(top_idx, N_EXPERTS, dtype=F32) * top_w[..., None], axis=-2)

    def block(args):
        xb, gb = args
        a, b = jnp.split(jnp.einsum('bnd,edf->ebnf', xb, w_gate_up), 2, axis=-1)
        y = jnp.einsum('ebnf,efd->ebnd', jax.nn.silu(a) * b, w_down)
        return jnp.einsum('ebnd,bne->bnd', y, gb.astype(y.dtype))

    xs = h.reshape(B, nb, MOE_SEQ_BLOCK, Dm).transpose(1, 0, 2, 3)
    gs = gates.reshape(B, nb, MOE_SEQ_BLOCK, N_EXPERTS).transpose(1, 0, 2, 3)
    y = lax.map(block, (xs, gs))
    return y.transpose(1, 0, 2, 3).reshape(B, T, Dm)


def setup_inputs(seed: int = 0) -> dict:
    key = jax.random.key(seed)
    ks = jax.random.split(key, 24)
    D = D_MODEL
    nrm = lambda k, shape, s: jax.random.normal(k, shape, F32) * s
    gain = lambda k, n: 1.0 + 0.02 * jax.random.normal(k, (n,), F32)
    return {
        "x": jax.random.normal(ks[0], (BATCH, SEQ, D), F32),
        "lower_bounds": nrm(ks[1], (DEPTH + 1, D), 0.1),
        "l0_mix_norm": gain(ks[2], D),
        "l0_hgrn_w_in": nrm(ks[3], (D, 4 * D), D ** -0.5),
        "l0_hgrn_out_norm": gain(ks[4], D),
        "l0_hgrn_w_out": nrm(ks[5], (D, D), D ** -0.5),
        "l0_ffn_norm": gain(ks[6], D),
        "l0_ffn_w_gate_up": nrm(ks[7], (D, 2 * FFN_DIM), D ** -0.5),
        "l0_ffn_w_down": nrm(ks[8], (FFN_DIM, D), FFN_DIM ** -0.5),
        "l1_mix_norm": gain(ks[9], D),
        "l1_diff_w_in": nrm(ks[10], (D, 3 * D), D ** -0.5),
        "l1_q_norm": gain(ks[11], DIFF_HEAD_DIM),
        "l1_k_norm": gain(ks[12], DIFF_HEAD_DIM),
        "l1_lambda_q1": nrm(ks[13], (DIFF_HEAD_DIM,), 0.1),
        "l1_lambda_k1": nrm(ks[14], (DIFF_HEAD_DIM,), 0.1),
        "l1_lambda_q2": nrm(ks[15], (DIFF_HEAD_DIM,), 0.1),
        "l1_lambda_k2": nrm(ks[16], (DIFF_HEAD_DIM,), 0.1),
        "l1_diff_sub_norm": gain(ks[17], 2 * DIFF_HEAD_DIM),
        "l1_diff_w_out": nrm(ks[18], (D, D), D ** -0.5),
        "l1_ffn_norm": gain(ks[19], D),
        "l1_router": nrm(ks[20], (D, N_EXPERTS), D ** -0.5),
        "l1_moe_w_gate_up": nrm(ks[21], (N_EXPERTS, D, 2 * EXPERT_DIM), D ** -0.5),
        "l1_moe_w_down": nrm(ks[22], (N_EXPERTS, EXPERT_DIM, D), EXPERT_DIM ** -0.5),
    }


def reference(x, lower_bounds, l0_mix_norm, l0_hgrn_w_in, l0_hgrn_out_norm, l0_hgrn_w_out,
              l0_ffn_norm, l0_ffn_w_gate_up, l0_ffn_w_down, l1_mix_norm, l1_diff_w_in,
              l1_q_norm, l1_k_norm, l1_lambda_q1, l1_lambda_k1, l1_lambda_q2, l1_lambda_k2,
              l1_diff_sub_norm, l1_diff_w_out, l1_ffn_norm, l1_router, l1_moe_w_gate_up,
              l1_moe_w_down):
    lb_all = jnp.cumsum(jax.nn.softmax(lower_bounds.astype(F32), axis=0), axis=0)
    layers = [
        dict(mix_norm=l0_mix_norm, ffn_norm=l0_ffn_norm,
             mixer=(l0_hgrn_w_in, l0_hgrn_out_norm, l0_hgrn_w_out),
             ffn=(l0_ffn_w_gate_up, l0_ffn_w_down)),
        dict(mix_norm=l1_mix_norm, ffn_norm=l1_ffn_norm,
             mixer=(l1_diff_w_in, l1_q_norm, l1_k_norm, l1_lambda_q1, l1_lambda_k1,
                    l1_lambda_q2, l1_lambda_k2, l1_diff_sub_norm, l1_diff_w_out),
             ffn=(l1_router, l1_moe_w_gate_up, l1_moe_w_down)),
    ]
    h = x
    for i in range(DEPTH):
        p = layers[i]
        hn = rms_norm(h, p["mix_norm"])
        if i % N_MIXERS == 0:
            h = h + hgrn2_mixer(hn, *p["mixer"], lb_all[i])
        else:
            h = h + diff_attention_mixer(hn, *p["mixer"], diff_lambda_init(i))
        hn = rms_norm(h, p["ffn_norm"])
        if i % 2 == 0:
            h = h + swiglu(hn, *p["ffn"])
        else:
            h = h + moe_swiglu(hn, *p["ffn"])
    return h
```

```python
import contextlib
import numpy as np
import concourse.bass as bass
import concourse.mybir as mybir
from concourse.bass_utils import run_bass_kernel_spmd

F32 = mybir.dt.float32
BF16 = mybir.dt.bfloat16
AF = mybir.ActivationFunctionType
ALU = mybir.AluOpType
AX = mybir.AxisListType

D = 1024
KC = 8
EPS = 1e-6
SEM_LIMIT = 30000
MAX_OPS = [10 ** 9]


class Buf:
    __slots__ = ("name", "w", "r", "uid", "excl")
    _n = [0]

    def __init__(self, name, excl=False):
        Buf._n[0] += 1
        self.uid = Buf._n[0]
        self.excl = excl or name.startswith(("p", "tp", "acc"))
        self.name = name
        self.w = None
        self.r = []


class Prog:
    def __init__(self, nc, stack):
        self.nc = nc
        self.stack = stack
        self.eng = {"pe": nc.tensor, "act": nc.scalar, "dve": nc.vector, "pool": nc.gpsimd, "sp": nc.sync}
        self.cur = {}
        self.waited = {k: {} for k in self.eng}
        self.semobj = {}
        self.nsem = 0
        self.n_ops = 0

    def _newsem(self, key):
        s = self.stack.enter_context(self.nc.semaphore("sm%d_%s" % (self.nsem, key)))
        self.nsem += 1
        self.semobj[id(s)] = s
        return s

    def _tick(self, key, inc):
        ent = self.cur.get(key)
        if ent is None or ent[1] + inc > SEM_LIMIT:
            ent = [self._newsem(key), 0]
            self.cur[key] = ent
        ent[1] += inc
        return ent[0], ent[1]

    def _wait(self, e, dep):
        if dep is None:
            return
        sem, val = dep
        w = self.waited[e]
        if w.get(id(sem), 0) >= val:
            return
        self.eng[e].wait_ge(sem, val)
        w[id(sem)] = val

    def op(self, e, fn, reads=(), writes=(), chan=None):
        if self.n_ops >= MAX_OPS[0]:
            return None
        for b in reads:
            self._wait(e, b.w)
            if b.excl:
                for r in b.r:
                    self._wait(e, r)
        for b in writes:
            self._wait(e, b.w)
            for r in b.r:
                self._wait(e, r)
        ins = fn(self.eng[e])
        if chan is None:
            sem, val = self._tick(e, 1)
            ins.then_inc(sem, 1)
        else:
            key = ("ld%d" % writes[0].uid) if chan != "st" else ("st%d" % reads[0].uid)
            sem, val = self._tick(key, 16)
            ins.then_inc(sem, 16)
        tok = (sem, val)
        for b in reads:
            b.r.append(tok)
            if len(b.r) > 64:
                b.r = b.r[-64:] if False else b.r
        for b in writes:
            b.w = tok
            b.r = []
        self.n_ops += 1
        return tok

    def barrier(self, engines=None):
        for e in (engines or list(self.eng)):
            for key, (sem, val) in list(self.cur.items()):
                if val > 0:
                    self._wait(e, (sem, val))


class Ctx:
    pass


def weave(gens, pattern):
    live = {k: g for k, g in gens.items() if g is not None}
    for ch in pattern:
        g = live.get(ch)
        if g is not None:
            try:
                next(g)
            except StopIteration:
                live.pop(ch)
    for k in list(live):
        for _ in live[k]:
            pass


def _alloc(stack, nc, name, shape, dt):
    return stack.enter_context(nc.sbuf_tensor(name, list(shape), dt))


def _psum(stack, nc, name, shape, dt=F32):
    return stack.enter_context(nc.psum_tensor(name, list(shape), dt))


def emit_consts(P, nc, stack, C):
    C.ident_f = _alloc(stack, nc, "ident_f", [128, 128], F32)
    C.ident = _alloc(stack, nc, "ident", [128, 128], BF16)
    C.b_ident = Buf("ident")
    C.b_identf = Buf("identf")

    P.op("pool", lambda e: e.memset(C.ident_f[:], 1.0), writes=[C.b_identf])
    P.op("pool", lambda e: e.affine_select(out=C.ident_f[:], in_=C.ident_f[:], pattern=[[-1, 128]],
                                           compare_op=ALU.is_equal, fill=0.0, base=0, channel_multiplier=1),
         writes=[C.b_identf])
    P.op("dve", lambda e: e.tensor_copy(out=C.ident[:], in_=C.ident_f[:]), reads=[C.b_identf], writes=[C.b_ident])


def load_bcast(P, nc, dst, b_dst, src_vec_ap, chan="ld"):
    P.op("sp", lambda e: e.dma_start(out=dst, in_=src_vec_ap.partition_broadcast(128)), writes=[b_dst], chan=chan)


def rmsnorm_rows(P, nc, S, h_ap, b_h, g_bc, b_g, xn_out, b_xn, tag):
    P.op("act", lambda e: e.activation(out=S.junk[:], in_=h_ap, func=AF.Square, accum_out=S.ss[:, 0:1]),
         reads=[b_h], writes=[S.b_junk, S.b_ss])
    P.op("act", lambda e: e.activation(out=S.ss[:, 1:2], in_=S.ss[:, 0:1], func=AF.Ln, scale=1.0 / D, bias=S.eps_t[:, 0:1]),
         reads=[S.b_ss, S.b_eps], writes=[S.b_ss])
    P.op("act", lambda e: e.activation(out=S.ss[:, 2:3], in_=S.ss[:, 1:2], func=AF.Exp, scale=-0.5), reads=[S.b_ss], writes=[S.b_ss])
    P.op("dve", lambda e: e.scalar_tensor_tensor(out=xn_out, in0=h_ap, scalar=S.ss[:, 2:3], in1=g_bc,
                                                 op0=ALU.mult, op1=ALU.mult),
         reads=[b_h, S.b_ss, b_g], writes=[b_xn])


def alloc_norm_scratch(P, nc, stack, tag):
    S = Ctx()
    S.junk = _alloc(stack, nc, "junk_" + tag, [128, D], BF16)
    S.ss = _alloc(stack, nc, "ss_" + tag, [128, 4], F32)
    S.eps_t = _alloc(stack, nc, "eps_" + tag, [128, 1], F32)
    S.one_t = _alloc(stack, nc, "one_" + tag, [128, 1], F32)
    S.b_junk, S.b_ss, S.b_eps = Buf("junk"), Buf("ss"), Buf("eps")
    P.op("dve", lambda e: e.memset(S.eps_t[:], EPS), writes=[S.b_eps])
    P.op("dve", lambda e: e.memset(S.one_t[:], 1.0), writes=[S.b_eps])
    return S


def transpose_rows(P, nc, C, src_tile_ap_fn, b_src, tp, b_tp, dst_ap, b_dst, evac="act"):
    def pe(e):
        ins = None
        for kc in range(KC):
            ins = e.transpose(out=tp[:, kc * 128:(kc + 1) * 128], in_=src_tile_ap_fn(kc), identity=C.ident[:])
        return ins
    P.op("pe", pe, reads=[b_src, C.b_ident], writes=[b_tp])
    src3 = tp[:].rearrange("p (k t) -> p k t", k=KC)
    if evac == "act":
        P.op("act", lambda e: e.copy(out=dst_ap, in_=src3), reads=[b_tp], writes=[b_dst])
    else:
        P.op("dve", lambda e: e.tensor_copy(out=dst_ap, in_=src3), reads=[b_tp], writes=[b_dst])


def phase_glu(P, nc, C, T, h_in, h_out, bufs_in, bufs_out, norm_g, w_gu, w_down, F, E, router=None,
              N=1024, FG=4, tag="glu"):
    NS = N // 128
    NQ = N // 512
    FCH = F // 128
    groups = [(g0, min(FG, FCH - g0)) for g0 in range(0, FCH, FG)]
    nblk = T // N
    with contextlib.ExitStack() as st:
        S2 = [alloc_norm_scratch(P, nc, st, tag + "a"), alloc_norm_scratch(P, nc, st, tag + "b")]
        g_bc = _alloc(st, nc, "gbc_" + tag, [128, D], F32)
        b_g = Buf("g")
        load_bcast(P, nc, g_bc[:], b_g, norm_g)
        h_t = _alloc(st, nc, "h_" + tag, [128, NS, D], F32)
        b_h = [Buf("h%d" % i) for i in range(NS)]
        xnT = _alloc(st, nc, "xnT_" + tag, [128, KC, N], BF16)
        b_xnT = [Buf("xnT%d" % i) for i in range(NS)]
        xn = [_alloc(st, nc, "xn%d_%s" % (i, tag), [128, D], BF16) for i in range(2)]
        b_xn = [Buf("xn0"), Buf("xn1")]
        NW = 3
        wgu_t = [_alloc(st, nc, "wgu%d_%s" % (i, tag), [128, KC, 2, FG * 128], BF16) for i in range(NW)]
        wd_t = [_alloc(st, nc, "wd%d_%s" % (i, tag), [128, FG, D], BF16) for i in range(NW)]
        b_wgu = [Buf("wgu%d" % i) for i in range(NW)]
        b_wd = [Buf("wd%d" % i) for i in range(NW)]
        hT = [_alloc(st, nc, "hT%d_%s" % (i, tag), [128, FG, N], BF16) for i in range(2)]
        b_hT = [[Buf("hT%d_%d" % (i, j)) for j in range(FG)] for i in range(2)]
        sg = [_alloc(st, nc, "sg%d_%s" % (i, tag), [128, 512], BF16) for i in range(2)]
        b_sg = [Buf("sg0"), Buf("sg1")]
        tp = _psum(st, nc, "tp_" + tag, [128, KC * 128], BF16)
        b_tp = Buf("tp")
        pg = [_psum(st, nc, "pg%d_%s" % (i, tag), [128, 512]) for i in range(2)]
        pu = [_psum(st, nc, "pu%d_%s" % (i, tag), [128, 512]) for i in range(2)]
        py = [_psum(st, nc, "py%d_%s" % (i, tag), [128, 512]) for i in range(3)]
        b_pg = [Buf("pg0"), Buf("pg1")]
        b_pu = [Buf("pu0"), Buf("pu1")]
        b_py = [Buf("py0"), Buf("py1"), Buf("py2")]
        if E > 1:
            xn32_2 = [_alloc(st, nc, "xn32_%d_%s" % (i, tag), [128, D], F32) for i in range(2)]
            b_xn32_2 = [Buf("xn32a"), Buf("xn32b")]
            r32 = _alloc(st, nc, "r32_" + tag, [128, KC, E], F32)
            b_r = Buf("r32")
            P.op("sp", lambda eng: eng.dma_start(out=r32[:], in_=router.rearrange("(k p) e -> p k e", p=128)), writes=[b_r], chan="ld")
            xT32 = _alloc(st, nc, "xT32_" + tag, [128, KC, 128], F32)
            b_xT32 = Buf("xT32")
            lg = _alloc(st, nc, "lg_" + tag, [128, NS, 8], F32)
            gates = _alloc(st, nc, "gates_" + tag, [128, NS, 8], F32)
            top8 = _alloc(st, nc, "top8_" + tag, [128, 8], F32)
            gsm = _alloc(st, nc, "gsm_" + tag, [128, 4], F32)
            b_lg = [Buf("lg%d" % i) for i in range(NS)]
            b_gates = [Buf("gates%d" % i) for i in range(NS)]
            b_top8, b_gsm = Buf("top8"), Buf("gsm")

        wcount = 0
        for blk in range(nblk):
            src = h_in[blk * N:(blk + 1) * N, :].rearrange("(s p) d -> p s d", p=128)
            P.op("sp", lambda e, src=src: e.dma_start(out=h_t[:], in_=src), reads=[bufs_in[blk]],
                 writes=b_h, chan="ld")
            for ts in range(NS):
                s2 = ts % 2
                S = S2[s2]
                if E > 1:
                    xn32, b_xn32 = xn32_2[s2], b_xn32_2[s2]
                    rmsnorm_rows(P, nc, S, h_t[:, ts, :], b_h[ts], g_bc[:], b_g, xn32[:], b_xn32, tag)
                    P.op("act", lambda e, s2=s2: e.copy(out=xn[s2][:], in_=xn32[:]), reads=[b_xn32],
                         writes=[b_xn[s2]])
                    def t32(e):
                        ins = None
                        for kc in range(KC):
                            ins = e.transpose(out=pg[kc // 4][:, (kc % 4) * 128:(kc % 4 + 1) * 128], in_=xn32[:, kc * 128:(kc + 1) * 128],
                                              identity=C.ident_f[:])
                        return ins
                    P.op("pe", t32, reads=[b_xn32, C.b_identf], writes=[b_pg[0], b_pg[1]])
                    for hf in range(2):
                        P.op("act", lambda e, hf=hf: e.copy(out=xT32[:, hf * 4:hf * 4 + 4, :],
                                                            in_=pg[hf][:].rearrange("p (k t) -> p k t", k=4)),
                             reads=[b_pg[hf]], writes=[b_xT32])

                    def mlg(e):
                        ins = None
                        for kc in range(KC):
                            ins = e.matmul(pu[0][:, 0:E], lhsT=xT32[:, kc, :], rhs=r32[:, kc, :], start=(kc == 0), stop=(kc == KC - 1))
                        return ins
                    P.op("pe", mlg, reads=[b_xT32, b_r], writes=[b_pu[0]])
                    P.op("dve", lambda e, ts=ts: e.tensor_copy(out=lg[:, ts, :], in_=pu[0][:, 0:E]), reads=[b_pu[0]], writes=[b_lg[ts]])
                    P.op("dve", lambda e, ts=ts: e.max(out=top8[:], in_=lg[:, ts, :]), reads=[b_lg[ts]], writes=[b_top8])
                    P.op("dve", lambda e: e.tensor_scalar(out=gsm[:, 0:1], in0=top8[:, 0:1], scalar1=-1.0, scalar2=None,
                                                          op0=ALU.mult), reads=[b_top8], writes=[b_gsm])
                    P.op("act", lambda e, ts=ts: e.activation(out=gates[:, ts, :], in_=lg[:, ts, :], func=AF.Exp,
                                                              bias=gsm[:, 0:1], scale=1.0),
                         reads=[b_lg[ts], b_gsm], writes=[b_gates[ts]])
                    P.op("dve", lambda e, ts=ts: e.tensor_scalar(out=lg[:, ts, :], in0=lg[:, ts, :], scalar1=top8[:, 1:2],
                                                                 scalar2=None, op0=ALU.is_ge),
                         reads=[b_top8], writes=[b_lg[ts]])
                    P.op("dve", lambda e, ts=ts: e.tensor_tensor(out=gates[:, ts, :], in0=gates[:, ts, :], in1=lg[:, ts, :],
                                                                 op=ALU.mult), reads=[b_lg[ts]], writes=[b_gates[ts]])
                    P.op("dve", lambda e, ts=ts: e.reduce_sum(out=gsm[:, 1:2], in_=gates[:, ts, :], axis=AX.X),
                         reads=[b_gates[ts]], writes=[b_gsm])
                    P.op("dve", lambda e: e.reciprocal(out=gsm[:, 2:3], in_=gsm[:, 1:2]), reads=[b_gsm], writes=[b_gsm])
                    P.op("dve", lambda e, ts=ts: e.tensor_scalar(out=gates[:, ts, :], in0=gates[:, ts, :], scalar1=gsm[:, 2:3],
                                                                 scalar2=None, op0=ALU.mult),
                         reads=[b_gsm], writes=[b_gates[ts]])
                else:
                    rmsnorm_rows(P, nc, S, h_t[:, ts, :], b_h[ts], g_bc[:], b_g, xn[s2][:], b_xn[s2], tag)
                transpose_rows(P, nc, C, lambda kc, s2=s2: xn[s2][:, kc * 128:(kc + 1) * 128], b_xn[s2], tp, b_tp,
                               xnT[:, :, ts * 128:(ts + 1) * 128], b_xnT[ts], evac="act")
            for ex in range(E):
                wgu_e = w_gu[ex] if E > 1 else w_gu
                wd_e = w_down[ex] if E > 1 else w_down
                for (g0, gn) in groups:
                    ws = wcount % NW
                    hs_ = wcount % 2
                    wcount += 1
                    for half in range(2):
                        srcw = wgu_e[:, half * F + g0 * 128: half * F + (g0 + gn) * 128].rearrange("(k p) f -> p k f", p=128)
                        P.op("pool", lambda e, srcw=srcw, ws=ws, half=half, gn=gn: e.dma_start(
                            out=wgu_t[ws][:, :, half, 0:gn * 128], in_=srcw), writes=[b_wgu[ws]], chan="ld")
                    srcd = wd_e[g0 * 128:(g0 + gn) * 128, :].rearrange("(c p) d -> p c d", p=128)
                    P.op("pool", lambda e, srcd=srcd, ws=ws, gn=gn: e.dma_start(out=wd_t[ws][:, 0:gn, :], in_=srcd),
                         writes=[b_wd[ws]], chan="ld")
                    pi = 0
                    for fcl in range(gn):
                        for q in range(NQ):
                            p2 = pi % 2
                            pi += 1
                            tsl = [b_xnT[q * 4 + i] for i in range(4)]

                            def mm(e, which, dst, fcl=fcl, q=q, ws=ws):
                                ins = None
                                for kc in range(KC):
                                    ins = e.matmul(dst[:], lhsT=wgu_t[ws][:, kc, which, fcl * 128:(fcl + 1) * 128],
                                                   rhs=xnT[:, kc, q * 512:(q + 1) * 512], start=(kc == 0), stop=(kc == KC - 1))
                                return ins
                            P.op("pe", lambda e, p2=p2, mm=mm: mm(e, 0, pg[p2]), reads=tsl + [b_wgu[ws]], writes=[b_pg[p2]])
                            P.op("pe", lambda e, p2=p2, mm=mm: mm(e, 1, pu[p2]), reads=tsl + [b_wgu[ws]], writes=[b_pu[p2]])
                            P.op("act", lambda e, p2=p2: e.activation(out=sg[p2][:], in_=pg[p2][:], func=AF.Silu),
                                 reads=[b_pg[p2]], writes=[b_sg[p2]])
                            P.op("dve", lambda e, p2=p2, hs_=hs_, fcl=fcl, q=q: e.tensor_tensor(
                                out=hT[hs_][:, fcl, q * 512:(q + 1) * 512], in0=sg[p2][:], in1=pu[p2][:], op=ALU.mult),
                                reads=[b_sg[p2], b_pu[p2]], writes=[b_hT[hs_][fcl]])
                    yi = 0
                    for ts in range(NS):
                        for dh in range(2):
                            y2 = yi % 3
                            yi += 1

                            def mmd(e, ts=ts, dh=dh, y2=y2, ws=ws, gn=gn, hs_=hs_):
                                ins = None
                                for fcl in range(gn):
                                    ins = e.matmul(py[y2][:], lhsT=hT[hs_][:, fcl, ts * 128:(ts + 1) * 128],
                                                   rhs=wd_t[ws][:, fcl, dh * 512:(dh + 1) * 512], start=(fcl == 0), stop=(fcl == gn - 1))
                                return ins
                            P.op("pe", mmd, reads=b_hT[hs_][0:gn] + [b_wd[ws]], writes=[b_py[y2]])
                            hs = h_t[:, ts, dh * 512:(dh + 1) * 512]
                            if E > 1:
                                P.op("dve", lambda e, hs=hs, y2=y2, ts=ts, ex=ex: e.scalar_tensor_tensor(
                                    out=hs, in0=py[y2][:], scalar=gates[:, ts, ex:ex + 1], in1=hs, op0=ALU.mult, op1=ALU.add),
                                    reads=[b_py[y2], b_gates[ts]], writes=[b_h[ts]])
                            else:
                                P.op("dve", lambda e, hs=hs, y2=y2: e.tensor_tensor(out=hs, in0=py[y2][:], in1=hs, op=ALU.add),
                                     reads=[b_py[y2]], writes=[b_h[ts]])
            dst = h_out[blk * N:(blk + 1) * N, :].rearrange("(s p) d -> p s d", p=128)
            P.op("sp", lambda e, dst=dst: e.dma_start(out=dst, in_=h_t[:]), reads=b_h, writes=[bufs_out[blk]],
                 chan="st")
        P.barrier()


def phase_hgrn(P, nc, C, T, h_in, h_out, bufs_in, bufs_out, NB, mix_norm, w_in, out_norm, w_out, lower_bounds, tag="hg"):
    H = 8
    nblk = T // 128
    per = NB // 128
    with contextlib.ExitStack() as st:
        S = alloc_norm_scratch(P, nc, st, tag)
        A = lambda name, shape, dt: _alloc(st, nc, name + "_" + tag, shape, dt)
        g_bc = A("gbc", [128, D], F32); b_g = Buf("g")
        load_bcast(P, nc, g_bc[:], b_g, mix_norm)
        on_bc = A("onbc", [128, D], F32); b_on = Buf("on")
        load_bcast(P, nc, on_bc[:], b_on, out_norm)
        lb_bc = A("lbbc", [128, D], F32); oml_bc = A("omlbc", [128, D], F32); b_lb = Buf("lb")
        with contextlib.ExitStack() as st2:
            lbr = _alloc(st2, nc, "lbraw_" + tag, [128, 3, D], F32); b_lbr = Buf("lbr")
            for r in range(3):
                P.op("sp", lambda e, r=r: e.dma_start(out=lbr[:, r, :], in_=lower_bounds[r, :].partition_broadcast(128)),
                     writes=[b_lbr], chan="ld")
            P.op("act", lambda e: e.activation(out=lbr[:], in_=lbr[:], func=AF.Exp), writes=[b_lbr])
            P.op("dve", lambda e: e.tensor_tensor(out=oml_bc[:], in0=lbr[:, 0, :], in1=lbr[:, 1, :], op=ALU.add),
                 reads=[b_lbr], writes=[b_lb])
            P.op("dve", lambda e: e.tensor_tensor(out=oml_bc[:], in0=oml_bc[:], in1=lbr[:, 2, :], op=ALU.add),
                 reads=[b_lbr], writes=[b_lb])
            P.op("dve", lambda e: e.reciprocal(out=oml_bc[:], in_=oml_bc[:]), writes=[b_lb])
            P.op("dve", lambda e: e.tensor_tensor(out=lb_bc[:], in0=lbr[:, 0, :], in1=oml_bc[:], op=ALU.mult),
                 reads=[b_lbr], writes=[b_lb])
            P.op("dve", lambda e: e.tensor_scalar(out=oml_bc[:], in0=lb_bc[:], scalar1=-1.0, scalar2=1.0, op0=ALU.mult, op1=ALU.add),
                 writes=[b_lb])
            P.barrier()
        mask = A("mask", [128, 4, 128], F32); b_mask = Buf("mask")
        esel = A("esel", [128, 2], F32); b_esel = Buf("esel")

        P.op("pool", lambda e: e.memset(mask[:], 1.0), writes=[b_mask])
        P.op("pool", lambda e: e.affine_select(out=mask[:], in_=mask[:], pattern=[[0, 4], [1, 128]], compare_op=ALU.is_ge,
                                               fill=0.0, base=0, channel_multiplier=-1), writes=[b_mask])
        P.op("pool", lambda e: e.affine_select(out=mask[0:64], in_=mask[0:64], pattern=[[0, 4], [-1, 128]],
                                               compare_op=ALU.is_ge, fill=0.0, base=63, channel_multiplier=0),
             writes=[b_mask])
        P.op("pool", lambda e: e.memset(esel[:], 0.0), writes=[b_esel])
        P.op("pool", lambda e: e.memset(esel[0:64, 0:1], 1.0), writes=[b_esel])
        P.op("pool", lambda e: e.memset(esel[64:128, 1:2], 1.0), writes=[b_esel])
        win = A("win", [128, KC, 4 * D], BF16); b_win = Buf("win")
        wout = A("wout", [128, KC, D], BF16); b_wout = Buf("wout")
        for q4 in range(4):
            srcw = w_in[:, q4 * D:(q4 + 1) * D].rearrange("(k p) f -> p k f", p=128)
            P.op("pool", lambda e, srcw=srcw, q4=q4: e.dma_start(out=win[:, :, q4 * D:(q4 + 1) * D], in_=srcw),
                 writes=[b_win], chan="ld")
        P.op("pool", lambda e: e.dma_start(out=wout[:], in_=w_out.rearrange("(k p) f -> p k f", p=128)),
             writes=[b_wout], chan="ld")
        def two(name, shape, dt):
            return [A(name + str(i), shape, dt) for i in range(2)], [Buf(name + str(i)) for i in range(2)]
        h_t, b_h = two("h", [128, D], F32)
        qtT, b_qtT = two("qtT", [128, H, 128], BF16)
        qtA, b_qtA = two("qtA", [128, H, 128], BF16)
        qtB, b_qtB = two("qtB", [128, H, 128], BF16)
        ktT, b_ktT = two("ktT", [128, H, 128], BF16)
        kt, b_kt = two("kt", [128, D], BF16)
        v16, b_v16 = two("v16", [128, D], BF16)
        gsn, b_gsn = two("gsn", [128, D], F32)
        dec, b_dec = two("dec", [128, H, 2], F32)
        for i in range(2):
            P.op("pool", lambda e, i=i: e.memset(qtA[i][:], 0.0), writes=[b_qtA[i]])
            P.op("pool", lambda e, i=i: e.memset(qtB[i][:], 0.0), writes=[b_qtB[i]])
        xn = A("xn", [128, D], BF16); b_xn = Buf("xn")
        xnT = A("xnT", [128, KC, 128], BF16); b_xnT = Buf("xnT")
        qs = A("qs", [128, D], F32); b_qs = Buf("qs")
        fg = A("fg", [128, D], F32); b_fg = Buf("fg")
        kk = A("kk", [128, D], F32); b_kk = Buf("kk")
        eb = A("eb", [128, D], F32); b_eb = Buf("eb")
        enb = A("enb", [128, D], F32); b_enb = Buf("enb")
        qt = A("qt", [128, D], BF16); b_qt = Buf("qt")
        SA = A("SA", [128, H, 128], BF16); b_SA = Buf("SA")
        SB = A("SB", [128, H, 128], BF16); b_SB = Buf("SB")
        P.op("pool", lambda e: e.memset(SA[:], 0.0), writes=[b_SA])
        scT = A("scT", [128, H, 128], BF16); b_scT = Buf("scT")
        osb = A("osb", [128, D], F32); b_osb = Buf("osb")
        sq = A("sq", [128, D], F32); b_sq = Buf("sq")
        ssq = A("ssq", [128, 3, H], F32); b_ssq = Buf("ssq")
        on16 = A("on16", [128, D], BF16); b_on16 = Buf("on16")
        onT = A("onT", [128, KC, 128], BF16); b_onT = Buf("onT")
        hout = A("hout", [128, D], F32); b_hout = Buf("hout")
        pz = [_psum(st, nc, "pz%d_%s" % (i, tag), [128, 512]) for i in range(2)]; b_pz = [Buf("pz0"), Buf("pz1")]
        tp = _psum(st, nc, "tp_" + tag, [128, KC * 128], BF16); b_tp = Buf("tp")
        pbe_bank = _psum(st, nc, "pbe_" + tag, [128, 512]); b_pbe = Buf("pbe")
        pbe = pbe_bank[:, 0:2 * H].rearrange("p (h c) -> p h c", c=2)
        pk = [_psum(st, nc, "pk%d_%s" % (i, tag), [128, 4, 128]) for i in range(4)]; b_pk = [Buf("pk%d" % i) for i in range(4)]

        def front(i):
            s = i % 2
            src = h_in[i * 128:(i + 1) * 128, :]
            P.op("sp", lambda e: e.dma_start(out=h_t[s][:], in_=src), reads=[bufs_in[i // per]], writes=[b_h[s]], chan="ld")
            rmsnorm_rows(P, nc, S, h_t[s][:], b_h[s], g_bc[:], b_g, xn[:], b_xn, tag)
            transpose_rows(P, nc, C, lambda kc: xn[:, kc * 128:(kc + 1) * 128], b_xn, tp, b_tp, xnT[:], b_xnT, evac="act")
            for oi, cb in enumerate((0, 1, 6, 7, 2, 3, 4, 5)):
                p2 = oi % 2

                def mm(e, cb=cb, p2=p2):
                    ins = None
                    for kc in range(KC):
                        ins = e.matmul(pz[p2][:], lhsT=xnT[:, kc, :], rhs=win[:, kc, cb * 512:(cb + 1) * 512],
                                       start=(kc == 0), stop=(kc == KC - 1))
                    return ins
                P.op("pe", mm, reads=[b_xnT, b_win], writes=[b_pz[p2]])
                cs = slice((cb % 2) * 512, (cb % 2) * 512 + 512)
                if cb < 2:
                    P.op("act", lambda e, p2=p2, cs=cs: e.activation(out=qs[:, cs], in_=pz[p2][:], func=AF.Silu),
                         reads=[b_pz[p2]], writes=[b_qs])
                elif cb < 4:
                    P.op("act", lambda e, p2=p2, cs=cs: e.activation(out=fg[:, cs], in_=pz[p2][:], func=AF.Sigmoid, scale=-1.0),
                         reads=[b_pz[p2]], writes=[b_fg])
                elif cb < 6:
                    P.op("act", lambda e, p2=p2, cs=cs: e.copy(out=v16[s][:, cs], in_=pz[p2][:]),
                         reads=[b_pz[p2]], writes=[b_v16[s]])
                else:
                    P.op("act", lambda e, p2=p2, cs=cs: e.activation(out=gsn[s][:, cs], in_=pz[p2][:], func=AF.Silu),
                         reads=[b_pz[p2]], writes=[b_gsn[s]])
            P.op("dve", lambda e: e.tensor_tensor(out=gsn[s][:], in0=gsn[s][:], in1=on_bc[:], op=ALU.mult),
                 reads=[b_on], writes=[b_gsn[s]])
            P.op("dve", lambda e: e.tensor_tensor(out=kk[:], in0=fg[:], in1=oml_bc[:], op=ALU.mult), reads=[b_lb, b_fg], writes=[b_kk])
            P.op("act", lambda e: e.activation(out=fg[:], in_=kk[:], func=AF.Ln, scale=-1.0, bias=S.one_t[:, 0:1]),
                 reads=[b_kk, S.b_eps], writes=[b_fg])
            yield
            for half in range(2):
                P.op("pe", lambda e, half=half: e.matmul(pz[half][:], lhsT=mask[:, 0, :], rhs=fg[:, half * 512:(half + 1) * 512],
                                                         start=True, stop=True),
                     reads=[b_mask, b_fg], writes=[b_pz[half]])

            def mbe(e):
                ins = None
                for hh in range(H):
                    ins = e.matmul(pbe[:, hh, :], lhsT=fg[:, hh * 128:(hh + 1) * 128], rhs=esel[:, 0:2], start=True, stop=True)
                return ins
            P.op("pe", mbe, reads=[b_fg, b_esel], writes=[b_pbe])
            for half in range(2):
                cs = slice(half * 512, half * 512 + 512)
                P.op("act", lambda e, half=half, cs=cs: e.activation(out=eb[:, cs], in_=pz[half][:], func=AF.Exp),
                     reads=[b_pz[half]], writes=[b_eb])
                P.op("act", lambda e, half=half, cs=cs: e.activation(out=enb[:, cs], in_=pz[half][:], func=AF.Exp, scale=-1.0),
                     reads=[b_pz[half]], writes=[b_enb])
            P.op("act", lambda e: e.activation(out=dec[s][:], in_=pbe[:], func=AF.Exp), reads=[b_pbe], writes=[b_dec[s]])
            P.op("dve", lambda e: e.tensor_tensor(out=qt[:], in0=qs[:], in1=eb[:], op=ALU.mult), reads=[b_qs, b_eb], writes=[b_qt])
            P.op("dve", lambda e: e.tensor_tensor(out=kt[s][:], in0=kk[:], in1=enb[:], op=ALU.mult), reads=[b_kk, b_enb],
                 writes=[b_kt[s]])
            yield
            def tq(e):
                ins = None
                for hh in range(H):
                    ins = e.transpose(out=tp[:, hh * 128:(hh + 1) * 128], in_=qt[:, hh * 128:(hh + 1) * 128], identity=C.ident[:])
                return ins
            P.op("pe", tq, reads=[b_qt, C.b_ident], writes=[b_tp])
            tp3 = tp[:].rearrange("p (k t) -> p k t", k=H)
            P.op("dve", lambda e: e.tensor_copy(out=qtT[s][:], in_=tp3), reads=[b_tp], writes=[b_qtT[s]])
            P.op("dve", lambda e: e.tensor_copy(out=qtA[s][:, :, 0:64], in_=tp3[:, :, 0:64]), reads=[b_tp], writes=[b_qtA[s]])
            P.op("dve", lambda e: e.tensor_copy(out=qtB[s][:, :, 64:128], in_=tp3[:, :, 64:128]), reads=[b_tp], writes=[b_qtB[s]])

            def tk(e):
                ins = None
                for hh in range(H):
                    ins = e.transpose(out=tp[:, hh * 128:(hh + 1) * 128], in_=kt[s][:, hh * 128:(hh + 1) * 128], identity=C.ident[:])
                return ins
            P.op("pe", tk, reads=[b_kt[s], C.b_ident], writes=[b_tp])
            P.op("act", lambda e: e.copy(out=ktT[s][:], in_=tp3), reads=[b_tp], writes=[b_ktT[s]])

        def state_update(s, c, Sin, b_Sin, Sout, b_Sout, banks):
            rows = slice(c * 64, c * 64 + 64)
            for half in range(2):
                bk = banks[half]

                def mu(e, half=half, bk=bk):
                    ins = None
                    for j in range(4):
                        hh = half * 4 + j
                        e.matmul(pk[bk][:, j, :], lhsT=kt[s][rows, hh * 128:(hh + 1) * 128], rhs=v16[s][rows, hh * 128:(hh + 1) * 128],
                                 start=True, stop=False)
                        ins = e.matmul(pk[bk][:, j, :], lhsT=C.ident[:], rhs=Sin[:, hh, :], start=False, stop=True)
                    return ins
                P.op("pe", mu, reads=[b_kt[s], b_v16[s], b_Sin, C.b_ident], writes=[b_pk[bk]])
                dsl = dec[s][:, half * 4:half * 4 + 4, c:c + 1].to_broadcast([128, 4, 128])
                P.op("dve", lambda e, half=half, bk=bk, dsl=dsl: e.tensor_tensor(
                    out=Sout[:, half * 4:half * 4 + 4, :], in0=pk[bk][:], in1=dsl, op=ALU.mult),
                    reads=[b_pk[bk], b_dec[s]], writes=[b_Sout])

        def back(i):
            s = i % 2
            state_update(s, 0, SA, b_SA, SB, b_SB, (0, 1))
            yield
            for half in range(2):
                bk = 2 + half

                def msc(e, half=half, bk=bk):
                    ins = None
                    for j in range(4):
                        hh = half * 4 + j
                        ins = e.matmul(pk[bk][:, j, :], lhsT=ktT[s][:, hh, :], rhs=qtT[s][:, hh, :], start=True, stop=True)
                    return ins
                P.op("pe", msc, reads=[b_ktT[s], b_qtT[s]], writes=[b_pk[bk]])
                P.op("dve", lambda e, half=half, bk=bk: e.tensor_tensor(out=scT[:, half * 4:half * 4 + 4, :], in0=pk[bk][:],
                                                                         in1=mask[:], op=ALU.mult),
                     reads=[b_pk[bk], b_mask], writes=[b_scT])
            yield
            for half in range(2):
                bk = half

                def mo(e, half=half, bk=bk):
                    ins = None
                    for j in range(4):
                        hh = half * 4 + j
                        e.matmul(pk[bk][:, j, :], lhsT=scT[:, hh, :], rhs=v16[s][:, hh * 128:(hh + 1) * 128], start=True, stop=False)
                        e.matmul(pk[bk][:, j, :], lhsT=qtA[s][:, hh, :], rhs=SA[:, hh, :], start=False, stop=False)
                        ins = e.matmul(pk[bk][:, j, :], lhsT=qtB[s][:, hh, :], rhs=SB[:, hh, :], start=False, stop=True)
                    return ins
                P.op("pe", mo, reads=[b_scT, b_v16[s], b_qtA[s], b_qtB[s], b_SA, b_SB], writes=[b_pk[bk]])
                P.op("act", lambda e, half=half, bk=bk: e.copy(out=osb[:, half * 512:(half + 1) * 512],
                                                              in_=pk[bk][:].rearrange("p j v -> p (j v)")),
                     reads=[b_pk[bk]], writes=[b_osb])
            yield
            state_update(s, 1, SB, b_SB, SA, b_SA, (2, 3))
            yield
            P.op("dve", lambda e: e.tensor_tensor(out=sq[:], in0=osb[:], in1=osb[:], op=ALU.mult), reads=[b_osb], writes=[b_sq])
            P.op("dve", lambda e: e.tensor_reduce(out=ssq[:, 0, :], in_=sq[:].rearrange("p (h v) -> p h v", h=H), axis=AX.X, op=ALU.add),
                 reads=[b_sq], writes=[b_ssq])
            P.op("act", lambda e: e.activation(out=ssq[:, 1, :], in_=ssq[:, 0, :], func=AF.Ln, scale=1.0 / 128, bias=S.eps_t[:, 0:1]),
                 reads=[S.b_eps], writes=[b_ssq])
            P.op("act", lambda e: e.activation(out=ssq[:, 2, :], in_=ssq[:, 1, :], func=AF.Exp, scale=-0.5), writes=[b_ssq])
            P.op("dve", lambda e: e.tensor_tensor(out=sq[:].rearrange("p (h v) -> p h v", h=H), in0=osb[:].rearrange("p (h v) -> p h v", h=H),
                                                  in1=ssq[:, 2, :].unsqueeze(2).to_broadcast([128, H, 128]), op=ALU.mult),
                 reads=[b_osb, b_ssq], writes=[b_sq])
            P.op("dve", lambda e: e.tensor_tensor(out=on16[:], in0=sq[:], in1=gsn[s][:], op=ALU.mult), reads=[b_gsn[s]],
                 writes=[b_sq, b_on16])
            yield
            transpose_rows(P, nc, C, lambda kc: on16[:, kc * 128:(kc + 1) * 128], b_on16, tp, b_tp, onT[:], b_onT, evac="act")
            for half in range(2):
                bk = half

                def my(e, half=half, bk=bk):
                    ins = None
                    pyv = pk[bk][:].rearrange("p j v -> p (j v)")
                    for kc in range(KC):
                        ins = e.matmul(pyv, lhsT=onT[:, kc, :], rhs=wout[:, kc, half * 512:(half + 1) * 512],
                                       start=(kc == 0), stop=(kc == KC - 1))
                    return ins
                P.op("pe", my, reads=[b_onT, b_wout], writes=[b_pk[bk]])
                P.op("dve", lambda e, half=half, bk=bk: e.tensor_tensor(
                    out=hout[:, half * 512:(half + 1) * 512], in0=pk[bk][:].rearrange("p j v -> p (j v)"),
                    in1=h_t[s][:, half * 512:(half + 1) * 512], op=ALU.add), reads=[b_pk[bk], b_h[s]], writes=[b_hout])
            dst = h_out[i * 128:(i + 1) * 128, :]
            wr = [bufs_out[i // per]] if (i % per == per - 1) else []
            P.op("sp", lambda e: e.dma_start(out=dst, in_=hout[:]), reads=[b_hout], writes=wr, chan="st")

        for _ in front(0):
            pass
        for i in range(nblk):
            gf = front(i + 1) if i + 1 < nblk else None
            weave({"f": gf, "b": back(i)}, "fbbfbbfbb")
        P.barrier()


def phase_qkv(P, nc, C, T, h_in, qT_d, kT_d, v_d, mix_norm, w_in, q_norm, k_norm, tag="qkv"):
    H = 8
    NB = min(512, T)
    NS = NB // 128
    nblk = T // NB
    with contextlib.ExitStack() as st:
        S = alloc_norm_scratch(P, nc, st, tag)
        A = lambda name, shape, dt: _alloc(st, nc, name + "_" + tag, shape, dt)
        g_bc = A("gbc", [128, D], F32); b_g = Buf("g")
        load_bcast(P, nc, g_bc[:], b_g, mix_norm)
        gq = A("gq", [128, 16, 64], F32); gk = A("gk", [128, 16, 64], F32); b_gq = Buf("gq"); b_gk = Buf("gk")
        P.op("sp", lambda e: e.dma_start(out=gq[:, 0, :], in_=q_norm.partition_broadcast(128)), writes=[b_gq], chan="ld")
        P.op("sp", lambda e: e.dma_start(out=gk[:, 0, :], in_=k_norm.partition_broadcast(128)), writes=[b_gk], chan="ld")
        for r in range(1, 16):
            P.op("pool", lambda e, r=r: e.tensor_copy(out=gq[:, r, :], in_=gq[:, 0, :]), reads=[b_gq], writes=[b_gq])
            P.op("pool", lambda e, r=r: e.tensor_copy(out=gk[:, r, :], in_=gk[:, 0, :]), reads=[b_gk], writes=[b_gk])
        win = A("win", [128, KC, 3 * D], BF16); b_win = Buf("win")
        for q3 in range(3):
            srcw = w_in[:, q3 * D:(q3 + 1) * D].rearrange("(k p) f -> p k f", p=128)
            P.op("pool", lambda e, srcw=srcw, q3=q3: e.dma_start(out=win[:, :, q3 * D:(q3 + 1) * D], in_=srcw),
                 writes=[b_win], chan="ld")
        h_t = [A("h%d" % i, [128, NS, D], F32) for i in range(2)]; b_h = [Buf("h0"), Buf("h1")]
        xn = [A("xn%d" % i, [128, D], BF16) for i in range(2)]; b_xn = [Buf("xn0"), Buf("xn1")]
        xnT = [A("xnT%d" % i, [128, KC, 128], BF16) for i in range(2)]; b_xnT = [Buf("xnT0"), Buf("xnT1")]
        qf = [[A("qf%d%d" % (w, i), [128, D], F32) for i in range(2)] for w in range(2)]
        b_qf = [[Buf("qf%d%d" % (w, i)) for i in range(2)] for w in range(2)]
        sq = [[A("sq%d%d" % (w, i), [128, D], F32) for i in range(2)] for w in range(2)]
        b_sq = [[Buf("sq%d%d" % (w, i)) for i in range(2)] for w in range(2)]
        ssq = [[A("ssq%d%d" % (w, i), [128, 3, 16], F32) for i in range(2)] for w in range(2)]
        b_ssq = [[Buf("ssq%d%d" % (w, i)) for i in range(2)] for w in range(2)]
        qn16 = [A("qn16_%d" % w, [128, D], BF16) for w in range(2)]; b_qn16 = [Buf("qn0"), Buf("qn1")]
        qTs = [A("qTs%d" % i, [128, H, NB], BF16) for i in range(2)]; b_qTs = [Buf("qTs0"), Buf("qTs1")]
        kTs = [A("kTs%d" % i, [128, H, NB], BF16) for i in range(2)]; b_kTs = [Buf("kTs0"), Buf("kTs1")]
        v16 = [A("v16%d" % i, [128, NS, D], BF16) for i in range(2)]; b_v16 = [Buf("v0"), Buf("v1")]
        pz = [_psum(st, nc, "pz%d_%s" % (i, tag), [128, 512]) for i in range(4)]; b_pz = [Buf("pz%d" % i) for i in range(4)]
        tp = _psum(st, nc, "tp_" + tag, [128, KC * 128], BF16); b_tp = Buf("tp")
        tpq = [_psum(st, nc, "tpq%d_%s" % (i, tag), [128, KC * 128], BF16) for i in range(2)]; b_tpq = [Buf("tpq0"), Buf("tpq1")]

        def load(blk):
            s = blk % 2
            src = h_in[blk * NB:(blk + 1) * NB, :].rearrange("(s p) d -> p s d", p=128)
            P.op("sp", lambda e: e.dma_start(out=h_t[s][:], in_=src), writes=[b_h[s]], chan="ld")

        pzc = [0]

        def stageA(u):
            blk, ts = divmod(u, NS)
            s = blk % 2
            u2 = u % 2
            if ts == 0 and blk + 1 < nblk:
                load(blk + 1)
            rmsnorm_rows(P, nc, S, h_t[s][:, ts, :], b_h[s], g_bc[:], b_g, xn[u2][:], b_xn[u2], tag)
            transpose_rows(P, nc, C, lambda kc: xn[u2][:, kc * 128:(kc + 1) * 128], b_xn[u2], tp, b_tp, xnT[u2][:], b_xnT[u2], evac="dve")
            for which in range(3):
                for half in range(2):
                    cb = which * 2 + half
                    p4 = pzc[0] % 4
                    pzc[0] += 1

                    def mm(e, cb=cb, p4=p4):
                        ins = None
                        for kc in range(KC):
                            ins = e.matmul(pz[p4][:], lhsT=xnT[u2][:, kc, :], rhs=win[:, kc, cb * 512:(cb + 1) * 512],
                                           start=(kc == 0), stop=(kc == KC - 1))
                        return ins
                    P.op("pe", mm, reads=[b_xnT[u2], b_win], writes=[b_pz[p4]])
                    cs = slice(half * 512, half * 512 + 512)
                    if which == 2:
                        P.op("act", lambda e, p4=p4, cs=cs: e.copy(out=v16[s][:, ts, cs], in_=pz[p4][:]),
                             reads=[b_pz[p4]], writes=[b_v16[s]])
                    else:
                        P.op("dve", lambda e, p4=p4, cs=cs, which=which: e.tensor_copy(out=qf[which][u2][:, cs], in_=pz[p4][:]),
                             reads=[b_pz[p4]], writes=[b_qf[which][u2]])
                        P.op("act", lambda e, cs=cs, which=which: e.activation(out=sq[which][u2][:, cs], in_=qf[which][u2][:, cs], func=AF.Square),
                             reads=[b_qf[which][u2]], writes=[b_sq[which][u2]])

        def stageB(u):
            blk, ts = divmod(u, NS)
            s = blk % 2
            u2 = u % 2
            for which in range(2):
                gg, b_gg = (gq, b_gq) if which == 0 else (gk, b_gk)
                dstT, b_dstT = (qTs[s], b_qTs[s]) if which == 0 else (kTs[s], b_kTs[s])
                tpx, b_tpx = tpq[which], b_tpq[which]
                qf_, sq_, ssq_ = qf[which][u2], sq[which][u2], ssq[which][u2]
                bq, bs, bss = b_qf[which][u2], b_sq[which][u2], b_ssq[which][u2]
                P.op("dve", lambda e, sq_=sq_, ssq_=ssq_: e.tensor_reduce(out=ssq_[:, 0, :], in_=sq_[:].rearrange("p (g d) -> p g d", g=16),
                                                                         axis=AX.X, op=ALU.add), reads=[bs], writes=[bss])
                P.op("act", lambda e, ssq_=ssq_: e.activation(out=ssq_[:, 1, :], in_=ssq_[:, 0, :], func=AF.Ln, scale=1.0 / 64,
                                                              bias=S.eps_t[:, 0:1]), reads=[S.b_eps], writes=[bss])
                P.op("act", lambda e, ssq_=ssq_: e.activation(out=ssq_[:, 2, :], in_=ssq_[:, 1, :], func=AF.Exp, scale=-0.5), writes=[bss])
                P.op("dve", lambda e, qf_=qf_, ssq_=ssq_: e.tensor_tensor(
                    out=qf_[:].rearrange("p (g d) -> p g d", g=16), in0=qf_[:].rearrange("p (g d) -> p g d", g=16),
                    in1=ssq_[:, 2, :].unsqueeze(2).to_broadcast([128, 16, 64]), op=ALU.mult), reads=[bss], writes=[bq])
                P.op("dve", lambda e, gg=gg, qf_=qf_, which=which: e.tensor_tensor(
                    out=qn16[which][:], in0=qf_[:], in1=gg[:].rearrange("p g d -> p (g d)"), op=ALU.mult),
                    reads=[bq, b_gg], writes=[b_qn16[which]])

                def tq(e, tpx=tpx, which=which):
                    ins = None
                    for hh in range(H):
                        ins = e.transpose(out=tpx[:, hh * 128:(hh + 1) * 128], in_=qn16[which][:, hh * 128:(hh + 1) * 128],
                                          identity=C.ident[:])
                    return ins
                P.op("pe", tq, reads=[b_qn16[which], C.b_ident], writes=[b_tpx])
                P.op("dve", lambda e, tpx=tpx, dstT=dstT, ts=ts: e.tensor_copy(
                    out=dstT[:, :, ts * 128:(ts + 1) * 128], in_=tpx[:].rearrange("p (k t) -> p k t", k=H)),
                    reads=[b_tpx], writes=[b_dstT])
            if ts == NS - 1:
                tsl = slice(blk * NB, (blk + 1) * NB)
                P.op("sp", lambda e: e.dma_start(out=qT_d[:, :, tsl].rearrange("h p t -> p h t"), in_=qTs[s][:]),
                     reads=[b_qTs[s]], chan="st")
                P.op("sp", lambda e: e.dma_start(out=kT_d[:, :, tsl].rearrange("h p t -> p h t"), in_=kTs[s][:]),
                     reads=[b_kTs[s]], chan="st")
                P.op("sp", lambda e: e.dma_start(out=v_d[tsl, :].rearrange("(s p) d -> p s d", p=128), in_=v16[s][:]),
                     reads=[b_v16[s]], chan="st")

        load(0)
        nu = nblk * NS
        stageA(0)
        for u in range(nu):
            if u + 1 < nu:
                stageA(u + 1)
            stageB(u)
        P.barrier()


def phase_attn(P, nc, C, T, qT_d, kT_d, v_d, on_d, q_norm, k_norm, lq1, lk1, lq2, lk2, sub_norm, lambda_init, tag="att"):
    H = 8
    NI = T // 256
    NJ = T // 128
    with contextlib.ExitStack() as st:
        A = lambda name, shape, dt: _alloc(st, nc, name + "_" + tag, shape, dt)
        vec = A("vec", [128, 6, 64], F32); b_vec = Buf("vec")
        for i, v_ in enumerate((q_norm, k_norm, lq1, lk1, lq2, lk2)):
            P.op("sp", lambda e, i=i, v_=v_: e.dma_start(out=vec[:, i, :], in_=v_.partition_broadcast(128)), writes=[b_vec], chan="ld")
        subn = A("subn", [128, 128], F32); b_subn = Buf("subn")
        load_bcast(P, nc, subn[:], b_subn, sub_norm)
        P.op("dve", lambda e: e.tensor_scalar(out=subn[:], in0=subn[:], scalar1=float(1.0 - lambda_init), scalar2=None, op0=ALU.mult),
             writes=[b_subn])
        sc = A("sc", [128, 12], F32); b_sc = Buf("sc")
        junk = A("junk", [128, 128], F32); b_junk = Buf("junk")
        eps_t = A("eps", [128, 1], F32); b_eps = Buf("eps")
        P.op("dve", lambda e: e.memset(eps_t[:], EPS), writes=[b_eps])
        P.op("dve", lambda e: e.tensor_reduce(out=sc[:, 0:2], in_=vec[:, 0:2, :], axis=AX.X, op=ALU.max, apply_absolute_value=True),
             reads=[b_vec], writes=[b_sc])
        P.op("dve", lambda e: e.tensor_tensor(out=sc[:, 2:3], in0=sc[:, 0:1], in1=sc[:, 1:2], op=ALU.mult), writes=[b_sc])
        P.op("dve", lambda e: e.tensor_scalar(out=sc[:, 3:4], in0=sc[:, 2:3], scalar1=-8.0, scalar2=None, op0=ALU.mult), writes=[b_sc])
        P.op("dve", lambda e: e.scalar_tensor_tensor(out=junk[:, 0:64], in0=vec[:, 2, :], scalar=1.0, in1=vec[:, 3, :], op0=ALU.mult,
                                                     op1=ALU.mult, accum_out=sc[:, 4:5]), reads=[b_vec], writes=[b_junk, b_sc])
        P.op("dve", lambda e: e.scalar_tensor_tensor(out=junk[:, 0:64], in0=vec[:, 4, :], scalar=1.0, in1=vec[:, 5, :], op0=ALU.mult,
                                                     op1=ALU.mult, accum_out=sc[:, 5:6]), reads=[b_vec], writes=[b_junk, b_sc])
        P.op("act", lambda e: e.activation(out=sc[:, 6:8], in_=sc[:, 4:6], func=AF.Exp), writes=[b_sc])
        P.op("dve", lambda e: e.tensor_tensor(out=sc[:, 8:9], in0=sc[:, 7:8], in1=sc[:, 6:7], op=ALU.subtract), writes=[b_sc])
        P.op("dve", lambda e: e.tensor_scalar(out=sc[:, 8:9], in0=sc[:, 8:9], scalar1=-float(lambda_init), scalar2=None, op0=ALU.add),
             writes=[b_sc])
        NM = 2 * NI + 1
        dtab_i = A("dtabi", [128, NM], mybir.dt.int32); dtab = A("dtab", [128, NM], F32); b_dtab = Buf("dtab")
        P.op("pool", lambda e: e.iota(out=dtab_i[:], pattern=[[-128, NM]], base=128, channel_multiplier=1), writes=[b_dtab])
        P.op("dve", lambda e: e.tensor_copy(out=dtab[:], in_=dtab_i[:]), writes=[b_dtab])
        btab = A("btab", [128, H, NM], F32); b_btab = Buf("btab")
        for hh in range(H):
            slope = 2.0 ** (-8.0 * (hh + 1) / H)
            P.op("dve", lambda e, hh=hh, slope=slope: e.tensor_scalar(out=btab[:, hh, :], in0=dtab[:], scalar1=slope, scalar2=sc[:, 3:4],
                                                                      op0=ALU.mult, op1=ALU.add), reads=[b_dtab, b_sc], writes=[b_btab])
        kT_t = [A("kT%d" % i, [128, 2, T], BF16) for i in range(2)]; b_kT = [Buf("kT0"), Buf("kT1")]
        qT_t = [A("qT%d" % i, [128, 2, T], BF16) for i in range(2)]; b_qT = [Buf("qT0"), Buf("qT1")]
        v_t = [A("vt%d" % i, [128, NJ, 129], BF16) for i in range(2)]; b_v = [Buf("vt0"), Buf("vt1")]
        qaug_i = A("qaugi", [128, 256], mybir.dt.int32); b_qaug = Buf("qaug")
        P.op("pool", lambda e: e.iota(out=qaug_i[64:65, :], pattern=[[-8, 256]], base=0, channel_multiplier=0), writes=[b_qaug])
        for i in range(2):
            P.op("pool", lambda e, i=i: e.memset(v_t[i][:, :, 128:129], 1.0), writes=[b_v[i]])
            for c in range(2):
                for I in range(NI):
                    P.op("dve", lambda e, i=i, c=c, I=I: e.tensor_copy(out=qT_t[i][64:65, c, I * 256:(I + 1) * 256], in_=qaug_i[64:65, :]),
                         reads=[b_qaug], writes=[b_qT[i]])
        NPT = 4
        PT = [A("PT%d" % i, [128, 2, 256], BF16) for i in range(NPT)]; b_PT = [Buf("PT%d" % i) for i in range(NPT)]
        tri = A("tri", [128, 2, 128], BF16); b_tri = Buf("tri")
        P.op("pool", lambda e: e.memset(tri[:], 1.0), writes=[b_tri])
        P.op("pool", lambda e: e.affine_select(out=tri[:], in_=tri[:], pattern=[[0, 2], [1, 128]], compare_op=ALU.is_ge, fill=0.0,
                                               base=0, channel_multiplier=-1), writes=[b_tri])
        o_t = A("o", [128, 128], F32); b_o = Buf("o")
        rz = A("rz", [128, 8], F32); b_rz = Buf("rz")
        on16 = [A("on16_%d" % i, [128, 128], BF16) for i in range(2)]; b_on16 = [Buf("on0"), Buf("on1")]
        NPS = 3
        pS = [_psum(st, nc, "pS%d_%s" % (i, tag), [128, 2, 256]) for i in range(NPS)]; b_pS = [Buf("pS%d" % i) for i in range(NPS)]
        acc = [[_psum(st, nc, "acc%d%d_%s" % (par, sb, tag), [128, 512]) for sb in range(2)] for par in range(2)]
        b_acc = [[Buf("acc%d%d" % (par, sb)) for sb in range(2)] for par in range(2)]

        def load_head(hh):
            s = hh % 2
            slope = 2.0 ** (-8.0 * (hh + 1) / H)
            for c in range(2):
                P.op("sp", lambda e, c=c: e.dma_start(out=kT_t[s][0:64, c, :], in_=kT_d[hh, c * 64:(c + 1) * 64, :]),
                     writes=[b_kT[s]], chan="ld")
                P.op("sp", lambda e, c=c: e.dma_start(out=qT_t[s][0:64, c, :], in_=qT_d[hh, c * 64:(c + 1) * 64, :]),
                     writes=[b_qT[s]], chan="ld")
            P.op("dve", lambda e: e.memset(kT_t[s][64:65, :, :], slope), writes=[b_kT[s]])
            P.op("sp", lambda e: e.dma_start(out=v_t[s][:, :, 0:128],
                                             in_=v_d[:, hh * 128:(hh + 1) * 128].rearrange("(j p) v -> p j v", p=128)),
                 writes=[b_v[s]], chan="ld")

        SKIP = 160.0
        pairs = []
        for hh in range(H):
            slope = 2.0 ** (-8.0 * (hh + 1) / H)
            for I in range(NI):
                js = [j for j in range(2 * I + 2) if slope * max(0, 256 * I - (128 * j + 127)) < SKIP]
                for j in js:
                    pairs.append((hh, I, j, js[0], js))
        fin = [0]

        def emit_qk(n):
            hh, I, j, j0, js = pairs[n]
            s = hh % 2
            lo = 128 if j == 2 * I + 1 else 0
            ps = n % NPS

            def mqk(e):
                ins = None
                for c in range(2):
                    ins = e.matmul(pS[ps][:, c, lo:256], lhsT=kT_t[s][0:65, c, j * 128:(j + 1) * 128],
                                   rhs=qT_t[s][0:65, c, I * 256 + lo:(I + 1) * 256], start=True, stop=True)
                return ins
            P.op("pe", mqk, reads=[b_kT[s], b_qT[s]], writes=[b_pS[ps]])

        def emit_exp(n):
            hh, I, j, j0, js = pairs[n]
            lo = 128 if j == 2 * I + 1 else 0
            ps = n % NPS
            pt = n % NPT
            mi = (2 * I - j) + 1
            P.op("act", lambda e: e.activation(out=PT[pt][:, :, lo:256], in_=pS[ps][:, :, lo:256], func=AF.Exp, scale=0.125,
                                               bias=btab[:, hh, mi:mi + 1]), reads=[b_pS[ps], b_btab], writes=[b_PT[pt]])
            if j >= 2 * I:
                sbd = j - 2 * I
                P.op("dve", lambda e: e.tensor_tensor(out=PT[pt][:, :, sbd * 128:(sbd + 1) * 128], in0=PT[pt][:, :, sbd * 128:(sbd + 1) * 128],
                                                      in1=tri[:], op=ALU.mult), reads=[b_tri], writes=[b_PT[pt]])

        def emit_pv(n):
            hh, I, j, j0, js = pairs[n]
            s = hh % 2
            lo = 128 if j == 2 * I + 1 else 0
            pt = n % NPT
            for sb in range(lo // 128, 2):
                last = (j == 2 * I + sb)

                par = I % 2

                def mpv(e, sb=sb, last=last, par=par):
                    ins = None
                    for c in range(2):
                        ins = e.matmul(acc[par][sb][:, c * 256:c * 256 + 129], lhsT=PT[pt][:, c, sb * 128:(sb + 1) * 128], rhs=v_t[s][:, j, :],
                                       start=(j == j0 and c == 0), stop=last, skip_group_check=True)
                    return ins
                P.op("pe", mpv, reads=[b_PT[pt], b_v[s]], writes=[b_acc[par][sb]])
                if last:
                    f2 = fin[0] % 2
                    fin[0] += 1
                    a0, a1 = acc[par][sb][:, 0:129], acc[par][sb][:, 256:385]
                    rb = [b_acc[par][sb]]
                    P.op("dve", lambda e, a0=a0: e.reciprocal(out=rz[:, 0:1], in_=a0[:, 128:129]), reads=rb, writes=[b_rz])
                    P.op("dve", lambda e, a1=a1: e.reciprocal(out=rz[:, 1:2], in_=a1[:, 128:129]), reads=rb, writes=[b_rz])
                    P.op("dve", lambda e: e.tensor_tensor(out=rz[:, 2:3], in0=rz[:, 1:2], in1=sc[:, 8:9], op=ALU.mult),
                         reads=[b_sc], writes=[b_rz])
                    P.op("dve", lambda e, a0=a0: e.tensor_scalar(out=o_t[:], in0=a0[:, 0:128], scalar1=rz[:, 0:1], scalar2=None,
                                                                 op0=ALU.mult), reads=rb + [b_rz], writes=[b_o])
                    P.op("dve", lambda e, a1=a1: e.scalar_tensor_tensor(out=o_t[:], in0=a1[:, 0:128], scalar=rz[:, 2:3], in1=o_t[:],
                                                                        op0=ALU.mult, op1=ALU.add), reads=rb + [b_rz], writes=[b_o])
                    P.op("dve", lambda e: e.scalar_tensor_tensor(out=junk[:], in0=o_t[:], scalar=1.0, in1=o_t[:], op0=ALU.mult,
                                                                 op1=ALU.mult, accum_out=rz[:, 3:4]), reads=[b_o], writes=[b_junk, b_rz])
                    P.op("act", lambda e: e.activation(out=rz[:, 4:5], in_=rz[:, 3:4], func=AF.Ln, scale=1.0 / 128, bias=eps_t[:, 0:1]),
                         reads=[b_eps], writes=[b_rz])
                    P.op("act", lambda e: e.activation(out=rz[:, 5:6], in_=rz[:, 4:5], func=AF.Exp, scale=-0.5), writes=[b_rz])
                    P.op("dve", lambda e, f2=f2: e.scalar_tensor_tensor(out=on16[f2][:], in0=o_t[:], scalar=rz[:, 5:6], in1=subn[:],
                                                                        op0=ALU.mult, op1=ALU.mult),
                         reads=[b_o, b_rz, b_subn], writes=[b_on16[f2]])
                    t0 = I * 256 + sb * 128
                    P.op("sp", lambda e, f2=f2, t0=t0: e.dma_start(out=on_d[t0:t0 + 128, hh * 128:(hh + 1) * 128], in_=on16[f2][:]),
                         reads=[b_on16[f2]], chan="st")

        LA = 2
        load_head(0)
        loaded = {0}
        npairs = len(pairs)
        for n in range(min(LA, npairs)):
            emit_qk(n)
        for n in range(npairs):
            hh = pairs[n][0]
            if hh + 1 < H and (hh + 1) not in loaded:
                load_head(hh + 1)
                loaded.add(hh + 1)
            if n + LA < npairs:
                emit_qk(n + LA)
            emit_exp(n)
            emit_pv(n)
        P.barrier()


def phase_attn_out(P, nc, C, T, h_in, on_d, h_out, w_out, tag="ao"):
    nblk = T // 128
    with contextlib.ExitStack() as st:
        A = lambda name, shape, dt: _alloc(st, nc, name + "_" + tag, shape, dt)
        wout = A("wout", [128, KC, D], BF16); b_wout = Buf("wout")
        P.op("pool", lambda e: e.dma_start(out=wout[:], in_=w_out.rearrange("(k p) f -> p k f", p=128)), writes=[b_wout], chan="ld")
        h_t = [A("h%d" % i, [128, D], F32) for i in range(2)]; b_h = [Buf("h0"), Buf("h1")]
        on = [A("on%d" % i, [128, D], BF16) for i in range(2)]; b_on = [Buf("on0"), Buf("on1")]
        onT = A("onT", [128, KC, 128], BF16); b_onT = Buf("onT")
        ho = [A("ho%d" % i, [128, D], F32) for i in range(2)]; b_ho = [Buf("ho0"), Buf("ho1")]
        tp = _psum(st, nc, "tp_" + tag, [128, KC * 128], BF16); b_tp = Buf("tp")
        py = [_psum(st, nc, "py%d_%s" % (i, tag), [128, 512]) for i in range(2)]; b_py = [Buf("py0"), Buf("py1")]

        def load(i):
            s = i % 2
            P.op("sp", lambda e: e.dma_start(out=h_t[s][:], in_=h_in[i * 128:(i + 1) * 128, :]), writes=[b_h[s]], chan="ld")
            P.op("sp", lambda e: e.dma_start(out=on[s][:], in_=on_d[i * 128:(i + 1) * 128, :]), writes=[b_on[s]], chan="ld")
        load(0)
        for i in range(nblk):
            s = i % 2
            if i + 1 < nblk:
                load(i + 1)
            transpose_rows(P, nc, C, lambda kc: on[s][:, kc * 128:(kc + 1) * 128], b_on[s], tp, b_tp, onT[:], b_onT, evac="dve")
            for half in range(2):
                def my(e, half=half):
                    ins = None
                    for kc in range(KC):
                        ins = e.matmul(py[half][:], lhsT=onT[:, kc, :], rhs=wout[:, kc, half * 512:(half + 1) * 512],
                                       start=(kc == 0), stop=(kc == KC - 1))
                    return ins
                P.op("pe", my, reads=[b_onT, b_wout], writes=[b_py[half]])
                P.op("dve", lambda e, half=half: e.tensor_tensor(out=ho[s][:, half * 512:(half + 1) * 512], in0=py[half][:],
                                                                 in1=h_t[s][:, half * 512:(half + 1) * 512], op=ALU.add),
                     reads=[b_py[half], b_h[s]], writes=[b_ho[s]])
            P.op("sp", lambda e: e.dma_start(out=h_out[i * 128:(i + 1) * 128, :], in_=ho[s][:]), reads=[b_ho[s]], chan="st")
        P.barrier()


def build_program(T, phases=("glu0",), N=1024):
    nc = bass.Bass("TRN2", target_bir_lowering=False)
    stack = contextlib.ExitStack()
    with stack:
        P = Prog(nc, stack)
        C = Ctx()
        inp = lambda name, shape: nc.dram_tensor(name, list(shape), F32, kind="ExternalInput").ap()
        x = inp("x", [T, D])
        out = nc.dram_tensor("out", [T, D], F32, kind="ExternalOutput").ap()
        emit_consts(P, nc, stack, C)
        nblk = T // N
        cur, b_cur = x, [Buf("x%d" % i) for i in range(nblk)]
        nscratch = 0
        for pi, ph in enumerate(phases):
            last = pi == len(phases) - 1
            if last:
                nxt = out
            else:
                nxt = nc.dram_tensor("scr%d" % nscratch, [T, D], F32, kind="Internal").ap()
                nscratch += 1
            b_nxt = [Buf("s%d_%d" % (pi, i)) for i in range(nblk)]
            if ph == "glu0":
                phase_glu(P, nc, C, T, cur, nxt, b_cur, b_nxt, inp("l0_ffn_norm", [D]),
                          inp("l0_ffn_w_gate_up", [D, 2 * 2816]), inp("l0_ffn_w_down", [2816, D]), 2816, 1, N=N, tag="f0")
            elif ph == "hgrn":
                phase_hgrn(P, nc, C, T, cur, nxt, b_cur, b_nxt, N, inp("l0_mix_norm", [D]), inp("l0_hgrn_w_in", [D, 4 * D]),
                           inp("l0_hgrn_out_norm", [D]), inp("l0_hgrn_w_out", [D, D]), inp("lower_bounds", [3, D]))
            elif ph == "attn":
                bf = lambda name, shape: nc.dram_tensor(name, list(shape), BF16, kind="Internal").ap()
                qT_d, kT_d = bf("qT_d", [8, 128, T]), bf("kT_d", [8, 128, T])
                v_d, on_d = bf("v_d", [T, D]), bf("on_d", [T, D])
                qn, kn = inp("l1_q_norm", [64]), inp("l1_k_norm", [64])
                phase_qkv(P, nc, C, T, cur, qT_d, kT_d, v_d, inp("l1_mix_norm", [D]), inp("l1_diff_w_in", [D, 3 * D]), qn, kn)
                phase_attn(P, nc, C, T, qT_d, kT_d, v_d, on_d, qn, kn, inp("l1_lambda_q1", [64]), inp("l1_lambda_k1", [64]),
                           inp("l1_lambda_q2", [64]), inp("l1_lambda_k2", [64]), inp("l1_diff_sub_norm", [128]),
                           0.8 - 0.6 * float(np.exp(-0.3 * 1)))
                phase_attn_out(P, nc, C, T, cur, on_d, nxt, inp("l1_diff_w_out", [D, D]))
            elif ph == "moe":
                phase_glu(P, nc, C, T, cur, nxt, b_cur, b_nxt, inp("l1_ffn_norm", [D]),
                          inp("l1_moe_w_gate_up", [8, D, 2 * 3584]), inp("l1_moe_w_down", [8, 3584, D]), 3584, 8,
                          router=inp("l1_router", [D, 8]), N=N, tag="f1")
            cur, b_cur = nxt, b_nxt
        P.barrier()
        print("ops:", P.n_ops, "sems:", P.nsem)
    return nc


_NC_CACHE = {}
PHASES = ("hgrn", "glu0", "attn", "moe")


def kernel(**inputs):
    x = np.ascontiguousarray(np.asarray(inputs["x"], dtype=np.float32))
    B, T, _ = x.shape
    if T not in _NC_CACHE:
        _NC_CACHE[T] = build_program(T, phases=PHASES, N=1024)
    nc = _NC_CACHE[T]
    shared = {k: np.ascontiguousarray(np.asarray(v, dtype=np.float32)) for k, v in inputs.items() if k != "x"}
    in_maps = []
    for b in range(B):
        m = dict(shared)
        m["x"] = x[b]
        in_maps.append(m)
    res = run_bass_kernel_spmd(nc, in_maps, core_ids=list(range(B)))
    return np.stack([np.asarray(r["out"], dtype=np.float32) for r in res.results], axis=0)
```

```python
import contextlib
import numpy as np
import concourse.bass as bass
import concourse.mybir as mybir
from concourse.bass_utils import run_bass_kernel_spmd

F32 = mybir.dt.float32
BF16 = mybir.dt.bfloat16
AF = mybir.ActivationFunctionType
ALU = mybir.AluOpType
AX = mybir.AxisListType

D = 1024
KC = 8
EPS = 1e-6
SEM_LIMIT = 30000
MAX_OPS = [10 ** 9]


class Buf:
    __slots__ = ("name", "w", "r", "uid", "excl")
    _n = [0]

    def __init__(self, name, excl=False):
        Buf._n[0] += 1
        self.uid = Buf._n[0]
        self.excl = excl or name.startswith(("p", "tp", "acc"))
        self.name = name
        self.w = None
        self.r = []


class Prog:
    def __init__(self, nc, stack):
        self.nc = nc
        self.stack = stack
        self.eng = {"pe": nc.tensor, "act": nc.scalar, "dve": nc.vector, "pool": nc.gpsimd, "sp": nc.sync}
        self.cur = {}
        self.waited = {k: {} for k in self.eng}
        self.semobj = {}
        self.nsem = 0
        self.n_ops = 0

    def _newsem(self, key):
        s = self.stack.enter_context(self.nc.semaphore("sm%d_%s" % (self.nsem, key)))
        self.nsem += 1
        self.semobj[id(s)] = s
        return s

    def _tick(self, key, inc):
        ent = self.cur.get(key)
        if ent is None or ent[1] + inc > SEM_LIMIT:
            ent = [self._newsem(key), 0]
            self.cur[key] = ent
        ent[1] += inc
        return ent[0], ent[1]

    def _wait(self, e, dep):
        if dep is None:
            return
        sem, val = dep
        w = self.waited[e]
        if w.get(id(sem), 0) >= val:
            return
        self.eng[e].wait_ge(sem, val)
        w[id(sem)] = val

    def op(self, e, fn, reads=(), writes=(), chan=None):
        if self.n_ops >= MAX_OPS[0]:
            return None
        for b in reads:
            self._wait(e, b.w)
            if b.excl:
                for r in b.r:
                    self._wait(e, r)
        for b in writes:
            self._wait(e, b.w)
            for r in b.r:
                self._wait(e, r)
        ins = fn(self.eng[e])
        if chan is None:
            sem, val = self._tick(e, 1)
            ins.then_inc(sem, 1)
        else:
            key = ("ld%d" % writes[0].uid) if chan != "st" else ("st%d" % reads[0].uid)
            sem, val = self._tick(key, 16)
            ins.then_inc(sem, 16)
        tok = (sem, val)
        for b in reads:
            b.r.append(tok)
            if len(b.r) > 64:
                b.r = b.r[-64:] if False else b.r
        for b in writes:
            b.w = tok
            b.r = []
        self.n_ops += 1
        return tok

    def barrier(self, engines=None):
        for e in (engines or list(self.eng)):
            for key, (sem, val) in list(self.cur.items()):
                if val > 0:
                    self._wait(e, (sem, val))


class Ctx:
    pass


def weave(gens, pattern):
    live = {k: g for k, g in gens.items() if g is not None}
    for ch in pattern:
        g = live.get(ch)
        if g is not None:
            try:
                next(g)
            except StopIteration:
                live.pop(ch)
    for k in list(live):
        for _ in live[k]:
            pass


def _alloc(stack, nc, name, shape, dt):
    return stack.enter_context(nc.sbuf_tensor(name, list(shape), dt))


def _psum(stack, nc, name, shape, dt=F32):
    return stack.enter_context(nc.psum_tensor(name, list(shape), dt))


def emit_consts(P, nc, stack, C):
    C.ident_f = _alloc(stack, nc, "ident_f", [128, 128], F32)
    C.ident = _alloc(stack, nc, "ident", [128, 128], BF16)
    C.b_ident = Buf("ident")
    C.b_identf = Buf("identf")

    P.op("pool", lambda e: e.memset(C.ident_f[:], 1.0), writes=[C.b_identf])
    P.op("pool", lambda e: e.affine_select(out=C.ident_f[:], in_=C.ident_f[:], pattern=[[-1, 128]],
                                           compare_op=ALU.is_equal, fill=0.0, base=0, channel_multiplier=1),
         writes=[C.b_identf])
    P.op("dve", lambda e: e.tensor_copy(out=C.ident[:], in_=C.ident_f[:]), reads=[C.b_identf], writes=[C.b_ident])


def load_bcast(P, nc, dst, b_dst, src_vec_ap, chan="ld"):
    P.op("sp", lambda e: e.dma_start(out=dst, in_=src_vec_ap.partition_broadcast(128)), writes=[b_dst], chan=chan)


def rmsnorm_rows(P, nc, S, h_ap, b_h, g_bc, b_g, xn_out, b_xn, tag):
    P.op("act", lambda e: e.activation(out=S.junk[:], in_=h_ap, func=AF.Square, accum_out=S.ss[:, 0:1]),
         reads=[b_h], writes=[S.b_junk, S.b_ss])
    P.op("act", lambda e: e.activation(out=S.ss[:, 1:2], in_=S.ss[:, 0:1], func=AF.Ln, scale=1.0 / D, bias=S.eps_t[:, 0:1]),
         reads=[S.b_ss, S.b_eps], writes=[S.b_ss])
    P.op("act", lambda e: e.activation(out=S.ss[:, 2:3], in_=S.ss[:, 1:2], func=AF.Exp, scale=-0.5), reads=[S.b_ss], writes=[S.b_ss])
    P.op("dve", lambda e: e.scalar_tensor_tensor(out=xn_out, in0=h_ap, scalar=S.ss[:, 2:3], in1=g_bc,
                                                 op0=ALU.mult, op1=ALU.mult),
         reads=[b_h, S.b_ss, b_g], writes=[b_xn])


def alloc_norm_scratch(P, nc, stack, tag):
    S = Ctx()
    S.junk = _alloc(stack, nc, "junk_" + tag, [128, D], BF16)
    S.ss = _alloc(stack, nc, "ss_" + tag, [128, 4], F32)
    S.eps_t = _alloc(stack, nc, "eps_" + tag, [128, 1], F32)
    S.one_t = _alloc(stack, nc, "one_" + tag, [128, 1], F32)
    S.b_junk, S.b_ss, S.b_eps = Buf("junk"), Buf("ss"), Buf("eps")
    P.op("dve", lambda e: e.memset(S.eps_t[:], EPS), writes=[S.b_eps])
    P.op("dve", lambda e: e.memset(S.one_t[:], 1.0), writes=[S.b_eps])
    return S


def transpose_rows(P, nc, C, src_tile_ap_fn, b_src, tp, b_tp, dst_ap, b_dst, evac="act"):
    def pe(e):
        ins = None
        for kc in range(KC):
            ins = e.transpose(out=tp[:, kc * 128:(kc + 1) * 128], in_=src_tile_ap_fn(kc), identity=C.ident[:])
        return ins
    P.op("pe", pe, reads=[b_src, C.b_ident], writes=[b_tp])
    src3 = tp[:].rearrange("p (k t) -> p k t", k=KC)
    if evac == "act":
        P.op("act", lambda e: e.copy(out=dst_ap, in_=src3), reads=[b_tp], writes=[b_dst])
    else:
        P.op("dve", lambda e: e.tensor_copy(out=dst_ap, in_=src3), reads=[b_tp], writes=[b_dst])


def phase_glu(P, nc, C, T, h_in, h_out, bufs_in, bufs_out, norm_g, w_gu, w_down, F, E, router=None,
              N=1024, FG=4, tag="glu"):
    NS = N // 128
    NQ = N // 512
    FCH = F // 128
    groups = [(g0, min(FG, FCH - g0)) for g0 in range(0, FCH, FG)]
    nblk = T // N
    with contextlib.ExitStack() as st:
        S2 = [alloc_norm_scratch(P, nc, st, tag + "a"), alloc_norm_scratch(P, nc, st, tag + "b")]
        g_bc = _alloc(st, nc, "gbc_" + tag, [128, D], F32)
        b_g = Buf("g")
        load_bcast(P, nc, g_bc[:], b_g, norm_g)
        h_t = _alloc(st, nc, "h_" + tag, [128, NS, D], F32)
        b_h = [Buf("h%d" % i) for i in range(NS)]
        xnT = _alloc(st, nc, "xnT_" + tag, [128, KC, N], BF16)
        b_xnT = [Buf("xnT%d" % i) for i in range(NS)]
        xn = [_alloc(st, nc, "xn%d_%s" % (i, tag), [128, D], BF16) for i in range(2)]
        b_xn = [Buf("xn0"), Buf("xn1")]
        NW = 3
        wgu_t = [_alloc(st, nc, "wgu%d_%s" % (i, tag), [128, KC, 2, FG * 128], BF16) for i in range(NW)]
        wd_t = [_alloc(st, nc, "wd%d_%s" % (i, tag), [128, FG, D], BF16) for i in range(NW)]
        b_wgu = [Buf("wgu%d" % i) for i in range(NW)]
        b_wd = [Buf("wd%d" % i) for i in range(NW)]
        hT = [_alloc(st, nc, "hT%d_%s" % (i, tag), [128, FG, N], BF16) for i in range(2)]
        b_hT = [[Buf("hT%d_%d" % (i, j)) for j in range(FG)] for i in range(2)]
        sg = [_alloc(st, nc, "sg%d_%s" % (i, tag), [128, 512], BF16) for i in range(2)]
        b_sg = [Buf("sg0"), Buf("sg1")]
        tp = _psum(st, nc, "tp_" + tag, [128, KC * 128], BF16)
        b_tp = Buf("tp")
        pg = [_psum(st, nc, "pg%d_%s" % (i, tag), [128, 512]) for i in range(2)]
        pu = [_psum(st, nc, "pu%d_%s" % (i, tag), [128, 512]) for i in range(2)]
        py = [_psum(st, nc, "py%d_%s" % (i, tag), [128, 512]) for i in range(3)]
        b_pg = [Buf("pg0"), Buf("pg1")]
        b_pu = [Buf("pu0"), Buf("pu1")]
        b_py = [Buf("py0"), Buf("py1"), Buf("py2")]
        if E > 1:
            xn32_2 = [_alloc(st, nc, "xn32_%d_%s" % (i, tag), [128, D], F32) for i in range(2)]
            b_xn32_2 = [Buf("xn32a"), Buf("xn32b")]
            r32 = _alloc(st, nc, "r32_" + tag, [128, KC, E], F32)
            b_r = Buf("r32")
            P.op("sp", lambda eng: eng.dma_start(out=r32[:], in_=router.rearrange("(k p) e -> p k e", p=128)), writes=[b_r], chan="ld")
            xT32 = _alloc(st, nc, "xT32_" + tag, [128, KC, 128], F32)
            b_xT32 = Buf("xT32")
            lg = _alloc(st, nc, "lg_" + tag, [128, NS, 8], F32)
            gates = _alloc(st, nc, "gates_" + tag, [128, NS, 8], F32)
            top8 = _alloc(st, nc, "top8_" + tag, [128, 8], F32)
            gsm = _alloc(st, nc, "gsm_" + tag, [128, 4], F32)
            b_lg = [Buf("lg%d" % i) for i in range(NS)]
            b_gates = [Buf("gates%d" % i) for i in range(NS)]
            b_top8, b_gsm = Buf("top8"), Buf("gsm")

        wcount = 0
        for blk in range(nblk):
            if blk == 0:
                for ts in range(NS):
                    P.op("sp", lambda e, ts=ts: e.dma_start(out=h_t[:, ts, :], in_=h_in[ts * 128:(ts + 1) * 128, :]),
                         reads=[bufs_in[0]], writes=[b_h[ts]], chan="ld")
            for ts in range(NS):
                s2 = ts % 2
                S = S2[s2]
                if E > 1:
                    xn32, b_xn32 = xn32_2[s2], b_xn32_2[s2]
                    rmsnorm_rows(P, nc, S, h_t[:, ts, :], b_h[ts], g_bc[:], b_g, xn32[:], b_xn32, tag)
                    P.op("act", lambda e, s2=s2: e.copy(out=xn[s2][:], in_=xn32[:]), reads=[b_xn32],
                         writes=[b_xn[s2]])
                    def t32(e):
                        ins = None
                        for kc in range(KC):
                            ins = e.transpose(out=pg[kc // 4][:, (kc % 4) * 128:(kc % 4 + 1) * 128], in_=xn32[:, kc * 128:(kc + 1) * 128],
                                              identity=C.ident_f[:])
                        return ins
                    P.op("pe", t32, reads=[b_xn32, C.b_identf], writes=[b_pg[0], b_pg[1]])
                    for hf in range(2):
                        P.op("act", lambda e, hf=hf: e.copy(out=xT32[:, hf * 4:hf * 4 + 4, :],
                                                            in_=pg[hf][:].rearrange("p (k t) -> p k t", k=4)),
                             reads=[b_pg[hf]], writes=[b_xT32])

                    def mlg(e):
                        ins = None
                        for kc in range(KC):
                            ins = e.matmul(pu[0][:, 0:E], lhsT=xT32[:, kc, :], rhs=r32[:, kc, :], start=(kc == 0), stop=(kc == KC - 1))
                        return ins
                    P.op("pe", mlg, reads=[b_xT32, b_r], writes=[b_pu[0]])
                    P.op("dve", lambda e, ts=ts: e.tensor_copy(out=lg[:, ts, :], in_=pu[0][:, 0:E]), reads=[b_pu[0]], writes=[b_lg[ts]])
                    P.op("dve", lambda e, ts=ts: e.max(out=top8[:], in_=lg[:, ts, :]), reads=[b_lg[ts]], writes=[b_top8])
                    P.op("dve", lambda e: e.tensor_scalar(out=gsm[:, 0:1], in0=top8[:, 0:1], scalar1=-1.0, scalar2=None,
                                                          op0=ALU.mult), reads=[b_top8], writes=[b_gsm])
                    P.op("act", lambda e, ts=ts: e.activation(out=gates[:, ts, :], in_=lg[:, ts, :], func=AF.Exp,
                                                              bias=gsm[:, 0:1], scale=1.0),
                         reads=[b_lg[ts], b_gsm], writes=[b_gates[ts]])
                    P.op("dve", lambda e, ts=ts: e.tensor_scalar(out=lg[:, ts, :], in0=lg[:, ts, :], scalar1=top8[:, 1:2],
                                                                 scalar2=None, op0=ALU.is_ge),
                         reads=[b_top8], writes=[b_lg[ts]])
                    P.op("dve", lambda e, ts=ts: e.tensor_tensor(out=gates[:, ts, :], in0=gates[:, ts, :], in1=lg[:, ts, :],
                                                                 op=ALU.mult), reads=[b_lg[ts]], writes=[b_gates[ts]])
                    P.op("dve", lambda e, ts=ts: e.reduce_sum(out=gsm[:, 1:2], in_=gates[:, ts, :], axis=AX.X),
                         reads=[b_gates[ts]], writes=[b_gsm])
                    P.op("dve", lambda e: e.reciprocal(out=gsm[:, 2:3], in_=gsm[:, 1:2]), reads=[b_gsm], writes=[b_gsm])
                    P.op("dve", lambda e, ts=ts: e.tensor_scalar(out=gates[:, ts, :], in0=gates[:, ts, :], scalar1=gsm[:, 2:3],
                                                                 scalar2=None, op0=ALU.mult),
                         reads=[b_gsm], writes=[b_gates[ts]])
                else:
                    rmsnorm_rows(P, nc, S, h_t[:, ts, :], b_h[ts], g_bc[:], b_g, xn[s2][:], b_xn[s2], tag)
                transpose_rows(P, nc, C, lambda kc, s2=s2: xn[s2][:, kc * 128:(kc + 1) * 128], b_xn[s2], tp, b_tp,
                               xnT[:, :, ts * 128:(ts + 1) * 128], b_xnT[ts], evac="act")
            for ex in range(E):
                wgu_e = w_gu[ex] if E > 1 else w_gu
                wd_e = w_down[ex] if E > 1 else w_down
                for (g0, gn) in groups:
                    ws = wcount % NW
                    hs_ = wcount % 2
                    wcount += 1
                    for half in range(2):
                        srcw = wgu_e[:, half * F + g0 * 128: half * F + (g0 + gn) * 128].rearrange("(k p) f -> p k f", p=128)
                        P.op("pool", lambda e, srcw=srcw, ws=ws, half=half, gn=gn: e.dma_start(
                            out=wgu_t[ws][:, :, half, 0:gn * 128], in_=srcw), writes=[b_wgu[ws]], chan="ld")
                    srcd = wd_e[g0 * 128:(g0 + gn) * 128, :].rearrange("(c p) d -> p c d", p=128)
                    P.op("pool", lambda e, srcd=srcd, ws=ws, gn=gn: e.dma_start(out=wd_t[ws][:, 0:gn, :], in_=srcd),
                         writes=[b_wd[ws]], chan="ld")
                    pi = 0
                    for fcl in range(gn):
                        for q in range(NQ):
                            p2 = pi % 2
                            pi += 1
                            tsl = [b_xnT[q * 4 + i] for i in range(4)]

                            def mm(e, which, dst, fcl=fcl, q=q, ws=ws):
                                ins = None
                                for kc in range(KC):
                                    ins = e.matmul(dst[:], lhsT=wgu_t[ws][:, kc, which, fcl * 128:(fcl + 1) * 128],
                                                   rhs=xnT[:, kc, q * 512:(q + 1) * 512], start=(kc == 0), stop=(kc == KC - 1))
                                return ins
                            P.op("pe", lambda e, p2=p2, mm=mm: mm(e, 0, pg[p2]), reads=tsl + [b_wgu[ws]], writes=[b_pg[p2]])
                            P.op("pe", lambda e, p2=p2, mm=mm: mm(e, 1, pu[p2]), reads=tsl + [b_wgu[ws]], writes=[b_pu[p2]])
                            P.op("act", lambda e, p2=p2: e.activation(out=sg[p2][:], in_=pg[p2][:], func=AF.Silu),
                                 reads=[b_pg[p2]], writes=[b_sg[p2]])
                            P.op("dve", lambda e, p2=p2, hs_=hs_, fcl=fcl, q=q: e.tensor_tensor(
                                out=hT[hs_][:, fcl, q * 512:(q + 1) * 512], in0=sg[p2][:], in1=pu[p2][:], op=ALU.mult),
                                reads=[b_sg[p2], b_pu[p2]], writes=[b_hT[hs_][fcl]])
                    yi = 0
                    for ts in range(NS):
                        for dh in range(2):
                            y2 = yi % 3
                            yi += 1

                            def mmd(e, ts=ts, dh=dh, y2=y2, ws=ws, gn=gn, hs_=hs_):
                                ins = None
                                for fcl in range(gn):
                                    ins = e.matmul(py[y2][:], lhsT=hT[hs_][:, fcl, ts * 128:(ts + 1) * 128],
                                                   rhs=wd_t[ws][:, fcl, dh * 512:(dh + 1) * 512], start=(fcl == 0), stop=(fcl == gn - 1))
                                return ins
                            P.op("pe", mmd, reads=b_hT[hs_][0:gn] + [b_wd[ws]], writes=[b_py[y2]])
                            hs = h_t[:, ts, dh * 512:(dh + 1) * 512]
                            if E > 1:
                                P.op("dve", lambda e, hs=hs, y2=y2, ts=ts, ex=ex: e.scalar_tensor_tensor(
                                    out=hs, in0=py[y2][:], scalar=gates[:, ts, ex:ex + 1], in1=hs, op0=ALU.mult, op1=ALU.add),
                                    reads=[b_py[y2], b_gates[ts]], writes=[b_h[ts]])
                            else:
                                P.op("dve", lambda e, hs=hs, y2=y2: e.tensor_tensor(out=hs, in0=py[y2][:], in1=hs, op=ALU.add),
                                     reads=[b_py[y2]], writes=[b_h[ts]])
            for ts in range(NS):
                r0 = blk * N + ts * 128
                P.op("sp", lambda e, ts=ts, r0=r0: e.dma_start(out=h_out[r0:r0 + 128, :], in_=h_t[:, ts, :]), reads=[b_h[ts]],
                     writes=[bufs_out[blk]] if ts == NS - 1 else [], chan="st")
                if blk + 1 < nblk:
                    P.op("sp", lambda e, ts=ts, r0=r0: e.dma_start(out=h_t[:, ts, :], in_=h_in[r0 + N:r0 + N + 128, :]),
                         reads=[bufs_in[blk + 1]], writes=[b_h[ts]], chan="ld")
        P.barrier()


def phase_hgrn(P, nc, C, T, h_in, h_out, bufs_in, bufs_out, NB, mix_norm, w_in, out_norm, w_out, lower_bounds, tag="hg"):
    H = 8
    nblk = T // 128
    per = NB // 128
    with contextlib.ExitStack() as st:
        S = alloc_norm_scratch(P, nc, st, tag)
        A = lambda name, shape, dt: _alloc(st, nc, name + "_" + tag, shape, dt)
        g_bc = A("gbc", [128, D], F32); b_g = Buf("g")
        load_bcast(P, nc, g_bc[:], b_g, mix_norm)
        on_bc = A("onbc", [128, D], F32); b_on = Buf("on")
        load_bcast(P, nc, on_bc[:], b_on, out_norm)
        lb_bc = A("lbbc", [128, D], F32); oml_bc = A("omlbc", [128, D], F32); b_lb = Buf("lb")
        with contextlib.ExitStack() as st2:
            lbr = _alloc(st2, nc, "lbraw_" + tag, [128, 3, D], F32); b_lbr = Buf("lbr")
            for r in range(3):
                P.op("sp", lambda e, r=r: e.dma_start(out=lbr[:, r, :], in_=lower_bounds[r, :].partition_broadcast(128)),
                     writes=[b_lbr], chan="ld")
            P.op("act", lambda e: e.activation(out=lbr[:], in_=lbr[:], func=AF.Exp), writes=[b_lbr])
            P.op("dve", lambda e: e.tensor_tensor(out=oml_bc[:], in0=lbr[:, 0, :], in1=lbr[:, 1, :], op=ALU.add),
                 reads=[b_lbr], writes=[b_lb])
            P.op("dve", lambda e: e.tensor_tensor(out=oml_bc[:], in0=oml_bc[:], in1=lbr[:, 2, :], op=ALU.add),
                 reads=[b_lbr], writes=[b_lb])
            P.op("dve", lambda e: e.reciprocal(out=oml_bc[:], in_=oml_bc[:]), writes=[b_lb])
            P.op("dve", lambda e: e.tensor_tensor(out=lb_bc[:], in0=lbr[:, 0, :], in1=oml_bc[:], op=ALU.mult),
                 reads=[b_lbr], writes=[b_lb])
            P.op("dve", lambda e: e.tensor_scalar(out=oml_bc[:], in0=lb_bc[:], scalar1=-1.0, scalar2=1.0, op0=ALU.mult, op1=ALU.add),
                 writes=[b_lb])
            P.barrier()
        mask = A("mask", [128, 4, 128], F32); b_mask = Buf("mask")
        esel = A("esel", [128, 2], F32); b_esel = Buf("esel")

        P.op("pool", lambda e: e.memset(mask[:], 1.0), writes=[b_mask])
        P.op("pool", lambda e: e.affine_select(out=mask[:], in_=mask[:], pattern=[[0, 4], [1, 128]], compare_op=ALU.is_ge,
                                               fill=0.0, base=0, channel_multiplier=-1), writes=[b_mask])
        P.op("pool", lambda e: e.affine_select(out=mask[0:64], in_=mask[0:64], pattern=[[0, 4], [-1, 128]],
                                               compare_op=ALU.is_ge, fill=0.0, base=63, channel_multiplier=0),
             writes=[b_mask])
        P.op("pool", lambda e: e.memset(esel[:], 0.0), writes=[b_esel])
        P.op("pool", lambda e: e.memset(esel[0:64, 0:1], 1.0), writes=[b_esel])
        P.op("pool", lambda e: e.memset(esel[64:128, 1:2], 1.0), writes=[b_esel])
        win = A("win", [128, KC, 4 * D], BF16); b_win = Buf("win")
        wout = A("wout", [128, KC, D], BF16); b_wout = Buf("wout")
        for q4 in range(4):
            srcw = w_in[:, q4 * D:(q4 + 1) * D].rearrange("(k p) f -> p k f", p=128)
            P.op("pool", lambda e, srcw=srcw, q4=q4: e.dma_start(out=win[:, :, q4 * D:(q4 + 1) * D], in_=srcw),
                 writes=[b_win], chan="ld")
        P.op("pool", lambda e: e.dma_start(out=wout[:], in_=w_out.rearrange("(k p) f -> p k f", p=128)),
             writes=[b_wout], chan="ld")
        def two(name, shape, dt):
            return [A(name + str(i), shape, dt) for i in range(2)], [Buf(name + str(i)) for i in range(2)]
        h_t, b_h = two("h", [128, D], F32)
        qtT, b_qtT = two("qtT", [128, H, 128], BF16)
        qtA, b_qtA = two("qtA", [128, H, 128], BF16)
        qtB, b_qtB = two("qtB", [128, H, 128], BF16)
        ktT, b_ktT = two("ktT", [128, H, 128], BF16)
        kt, b_kt = two("kt", [128, D], BF16)
        v16, b_v16 = two("v16", [128, D], BF16)
        gsn, b_gsn = two("gsn", [128, D], F32)
        dec, b_dec = two("dec", [128, H, 2], F32)
        for i in range(2):
            P.op("pool", lambda e, i=i: e.memset(qtA[i][:], 0.0), writes=[b_qtA[i]])
            P.op("pool", lambda e, i=i: e.memset(qtB[i][:], 0.0), writes=[b_qtB[i]])
        xn = A("xn", [128, D], BF16); b_xn = Buf("xn")
        xnT = A("xnT", [128, KC, 128], BF16); b_xnT = Buf("xnT")
        qs = A("qs", [128, D], F32); b_qs = Buf("qs")
        fg = A("fg", [128, D], F32); b_fg = Buf("fg")
        kk = A("kk", [128, D], F32); b_kk = Buf("kk")
        eb = A("eb", [128, D], F32); b_eb = Buf("eb")
        enb = A("enb", [128, D], F32); b_enb = Buf("enb")
        qt = A("qt", [128, D], BF16); b_qt = Buf("qt")
        SA = A("SA", [128, H, 128], BF16); b_SA = Buf("SA")
        SB = A("SB", [128, H, 128], BF16); b_SB = Buf("SB")
        P.op("pool", lambda e: e.memset(SA[:], 0.0), writes=[b_SA])
        scT = A("scT", [128, H, 128], BF16); b_scT = Buf("scT")
        osb = A("osb", [128, D], F32); b_osb = Buf("osb")
        sq = A("sq", [128, D], F32); b_sq = Buf("sq")
        ssq = A("ssq", [128, 3, H], F32); b_ssq = Buf("ssq")
        on16 = A("on16", [128, D], BF16); b_on16 = Buf("on16")
        onT = A("onT", [128, KC, 128], BF16); b_onT = Buf("onT")
        hout = A("hout", [128, D], F32); b_hout = Buf("hout")
        pz = [_psum(st, nc, "pz%d_%s" % (i, tag), [128, 512]) for i in range(2)]; b_pz = [Buf("pz0"), Buf("pz1")]
        tp = _psum(st, nc, "tp_" + tag, [128, KC * 128], BF16); b_tp = Buf("tp")
        pbe_bank = _psum(st, nc, "pbe_" + tag, [128, 512]); b_pbe = Buf("pbe")
        pbe = pbe_bank[:, 0:2 * H].rearrange("p (h c) -> p h c", c=2)
        pk = [_psum(st, nc, "pk%d_%s" % (i, tag), [128, 4, 128]) for i in range(4)]; b_pk = [Buf("pk%d" % i) for i in range(4)]

        def front(i):
            s = i % 2
            src = h_in[i * 128:(i + 1) * 128, :]
            P.op("sp", lambda e: e.dma_start(out=h_t[s][:], in_=src), reads=[bufs_in[i // per]], writes=[b_h[s]], chan="ld")
            rmsnorm_rows(P, nc, S, h_t[s][:], b_h[s], g_bc[:], b_g, xn[:], b_xn, tag)
            transpose_rows(P, nc, C, lambda kc: xn[:, kc * 128:(kc + 1) * 128], b_xn, tp, b_tp, xnT[:], b_xnT, evac="act")
            for oi, cb in enumerate((0, 1, 6, 7, 2, 3, 4, 5)):
                p2 = oi % 2

                def mm(e, cb=cb, p2=p2):
                    ins = None
                    for kc in range(KC):
                        ins = e.matmul(pz[p2][:], lhsT=xnT[:, kc, :], rhs=win[:, kc, cb * 512:(cb + 1) * 512],
                                       start=(kc == 0), stop=(kc == KC - 1))
                    return ins
                P.op("pe", mm, reads=[b_xnT, b_win], writes=[b_pz[p2]])
                cs = slice((cb % 2) * 512, (cb % 2) * 512 + 512)
                if cb < 2:
                    P.op("act", lambda e, p2=p2, cs=cs: e.activation(out=qs[:, cs], in_=pz[p2][:], func=AF.Silu),
                         reads=[b_pz[p2]], writes=[b_qs])
                elif cb < 4:
                    P.op("act", lambda e, p2=p2, cs=cs: e.activation(out=fg[:, cs], in_=pz[p2][:], func=AF.Sigmoid, scale=-1.0),
                         reads=[b_pz[p2]], writes=[b_fg])
                elif cb < 6:
                    P.op("act", lambda e, p2=p2, cs=cs: e.copy(out=v16[s][:, cs], in_=pz[p2][:]),
                         reads=[b_pz[p2]], writes=[b_v16[s]])
                else:
                    P.op("act", lambda e, p2=p2, cs=cs: e.activation(out=gsn[s][:, cs], in_=pz[p2][:], func=AF.Silu),
                         reads=[b_pz[p2]], writes=[b_gsn[s]])
            P.op("dve", lambda e: e.tensor_tensor(out=gsn[s][:], in0=gsn[s][:], in1=on_bc[:], op=ALU.mult),
                 reads=[b_on], writes=[b_gsn[s]])
            P.op("dve", lambda e: e.tensor_tensor(out=kk[:], in0=fg[:], in1=oml_bc[:], op=ALU.mult), reads=[b_lb, b_fg], writes=[b_kk])
            P.op("act", lambda e: e.activation(out=fg[:], in_=kk[:], func=AF.Ln, scale=-1.0, bias=S.one_t[:, 0:1]),
                 reads=[b_kk, S.b_eps], writes=[b_fg])
            yield
            for half in range(2):
                P.op("pe", lambda e, half=half: e.matmul(pz[half][:], lhsT=mask[:, 0, :], rhs=fg[:, half * 512:(half + 1) * 512],
                                                         start=True, stop=True),
                     reads=[b_mask, b_fg], writes=[b_pz[half]])

            def mbe(e):
                ins = None
                for hh in range(H):
                    ins = e.matmul(pbe[:, hh, :], lhsT=fg[:, hh * 128:(hh + 1) * 128], rhs=esel[:, 0:2], start=True, stop=True)
                return ins
            P.op("pe", mbe, reads=[b_fg, b_esel], writes=[b_pbe])
            for half in range(2):
                cs = slice(half * 512, half * 512 + 512)
                P.op("act", lambda e, half=half, cs=cs: e.activation(out=eb[:, cs], in_=pz[half][:], func=AF.Exp),
                     reads=[b_pz[half]], writes=[b_eb])
                P.op("act", lambda e, half=half, cs=cs: e.activation(out=enb[:, cs], in_=pz[half][:], func=AF.Exp, scale=-1.0),
                     reads=[b_pz[half]], writes=[b_enb])
            P.op("act", lambda e: e.activation(out=dec[s][:], in_=pbe[:], func=AF.Exp), reads=[b_pbe], writes=[b_dec[s]])
            P.op("dve", lambda e: e.tensor_tensor(out=qt[:], in0=qs[:], in1=eb[:], op=ALU.mult), reads=[b_qs, b_eb], writes=[b_qt])
            P.op("dve", lambda e: e.tensor_tensor(out=kt[s][:], in0=kk[:], in1=enb[:], op=ALU.mult), reads=[b_kk, b_enb],
                 writes=[b_kt[s]])
            yield
            def tq(e):
                ins = None
                for hh in range(H):
                    ins = e.transpose(out=tp[:, hh * 128:(hh + 1) * 128], in_=qt[:, hh * 128:(hh + 1) * 128], identity=C.ident[:])
                return ins
            P.op("pe", tq, reads=[b_qt, C.b_ident], writes=[b_tp])
            tp3 = tp[:].rearrange("p (k t) -> p k t", k=H)
            P.op("dve", lambda e: e.tensor_copy(out=qtT[s][:], in_=tp3), reads=[b_tp], writes=[b_qtT[s]])
            P.op("dve", lambda e: e.tensor_copy(out=qtA[s][:, :, 0:64], in_=tp3[:, :, 0:64]), reads=[b_tp], writes=[b_qtA[s]])
            P.op("dve", lambda e: e.tensor_copy(out=qtB[s][:, :, 64:128], in_=tp3[:, :, 64:128]), reads=[b_tp], writes=[b_qtB[s]])

            def tk(e):
                ins = None
                for hh in range(H):
                    ins = e.transpose(out=tp[:, hh * 128:(hh + 1) * 128], in_=kt[s][:, hh * 128:(hh + 1) * 128], identity=C.ident[:])
                return ins
            P.op("pe", tk, reads=[b_kt[s], C.b_ident], writes=[b_tp])
            P.op("act", lambda e: e.copy(out=ktT[s][:], in_=tp3), reads=[b_tp], writes=[b_ktT[s]])

        def state_update(s, c, Sin, b_Sin, Sout, b_Sout, banks):
            rows = slice(c * 64, c * 64 + 64)
            for half in range(2):
                bk = banks[half]

                def mu(e, half=half, bk=bk):
                    ins = None
                    for j in range(4):
                        hh = half * 4 + j
                        e.matmul(pk[bk][:, j, :], lhsT=kt[s][rows, hh * 128:(hh + 1) * 128], rhs=v16[s][rows, hh * 128:(hh + 1) * 128],
                                 start=True, stop=False)
                        ins = e.matmul(pk[bk][:, j, :], lhsT=C.ident[:], rhs=Sin[:, hh, :], start=False, stop=True)
                    return ins
                P.op("pe", mu, reads=[b_kt[s], b_v16[s], b_Sin, C.b_ident], writes=[b_pk[bk]])
                dsl = dec[s][:, half * 4:half * 4 + 4, c:c + 1].to_broadcast([128, 4, 128])
                P.op("dve", lambda e, half=half, bk=bk, dsl=dsl: e.tensor_tensor(
                    out=Sout[:, half * 4:half * 4 + 4, :], in0=pk[bk][:], in1=dsl, op=ALU.mult),
                    reads=[b_pk[bk], b_dec[s]], writes=[b_Sout])

        def back(i):
            s = i % 2
            state_update(s, 0, SA, b_SA, SB, b_SB, (0, 1))
            yield
            for half in range(2):
                bk = 2 + half

                def msc(e, half=half, bk=bk):
                    ins = None
                    for j in range(4):
                        hh = half * 4 + j
                        ins = e.matmul(pk[bk][:, j, :], lhsT=ktT[s][:, hh, :], rhs=qtT[s][:, hh, :], start=True, stop=True)
                    return ins
                P.op("pe", msc, reads=[b_ktT[s], b_qtT[s]], writes=[b_pk[bk]])
                P.op("dve", lambda e, half=half, bk=bk: e.tensor_tensor(out=scT[:, half * 4:half * 4 + 4, :], in0=pk[bk][:],
                                                                         in1=mask[:], op=ALU.mult),
                     reads=[b_pk[bk], b_mask], writes=[b_scT])
            yield
            for half in range(2):
                bk = half

                def mo(e, half=half, bk=bk):
                    ins = None
                    for j in range(4):
                        hh = half * 4 + j
                        e.matmul(pk[bk][:, j, :], lhsT=scT[:, hh, :], rhs=v16[s][:, hh * 128:(hh + 1) * 128], start=True, stop=False)
                        e.matmul(pk[bk][:, j, :], lhsT=qtA[s][:, hh, :], rhs=SA[:, hh, :], start=False, stop=False)
                        ins = e.matmul(pk[bk][:, j, :], lhsT=qtB[s][:, hh, :], rhs=SB[:, hh, :], start=False, stop=True)
                    return ins
                P.op("pe", mo, reads=[b_scT, b_v16[s], b_qtA[s], b_qtB[s], b_SA, b_SB], writes=[b_pk[bk]])
                P.op("act", lambda e, half=half, bk=bk: e.copy(out=osb[:, half * 512:(half + 1) * 512],
                                                              in_=pk[bk][:].rearrange("p j v -> p (j v)")),
                     reads=[b_pk[bk]], writes=[b_osb])
            yield
            state_update(s, 1, SB, b_SB, SA, b_SA, (2, 3))
            yield
            P.op("dve", lambda e: e.tensor_tensor(out=sq[:], in0=osb[:], in1=osb[:], op=ALU.mult), reads=[b_osb], writes=[b_sq])
            P.op("dve", lambda e: e.tensor_reduce(out=ssq[:, 0, :], in_=sq[:].rearrange("p (h v) -> p h v", h=H), axis=AX.X, op=ALU.add),
                 reads=[b_sq], writes=[b_ssq])
            P.op("act", lambda e: e.activation(out=ssq[:, 1, :], in_=ssq[:, 0, :], func=AF.Ln, scale=1.0 / 128, bias=S.eps_t[:, 0:1]),
                 reads=[S.b_eps], writes=[b_ssq])
            P.op("act", lambda e: e.activation(out=ssq[:, 2, :], in_=ssq[:, 1, :], func=AF.Exp, scale=-0.5), writes=[b_ssq])
            P.op("dve", lambda e: e.tensor_tensor(out=sq[:].rearrange("p (h v) -> p h v", h=H), in0=osb[:].rearrange("p (h v) -> p h v", h=H),
                                                  in1=ssq[:, 2, :].unsqueeze(2).to_broadcast([128, H, 128]), op=ALU.mult),
                 reads=[b_osb, b_ssq], writes=[b_sq])
            P.op("dve", lambda e: e.tensor_tensor(out=on16[:], in0=sq[:], in1=gsn[s][:], op=ALU.mult), reads=[b_gsn[s]],
                 writes=[b_sq, b_on16])
            yield
            transpose_rows(P, nc, C, lambda kc: on16[:, kc * 128:(kc + 1) * 128], b_on16, tp, b_tp, onT[:], b_onT, evac="act")
            for half in range(2):
                bk = half

                def my(e, half=half, bk=bk):
                    ins = None
                    pyv = pk[bk][:].rearrange("p j v -> p (j v)")
                    for kc in range(KC):
                        ins = e.matmul(pyv, lhsT=onT[:, kc, :], rhs=wout[:, kc, half * 512:(half + 1) * 512],
                                       start=(kc == 0), stop=(kc == KC - 1))
                    return ins
                P.op("pe", my, reads=[b_onT, b_wout], writes=[b_pk[bk]])
                P.op("dve", lambda e, half=half, bk=bk: e.tensor_tensor(
                    out=hout[:, half * 512:(half + 1) * 512], in0=pk[bk][:].rearrange("p j v -> p (j v)"),
                    in1=h_t[s][:, half * 512:(half + 1) * 512], op=ALU.add), reads=[b_pk[bk], b_h[s]], writes=[b_hout])
            dst = h_out[i * 128:(i + 1) * 128, :]
            wr = [bufs_out[i // per]] if (i % per == per - 1) else []
            P.op("sp", lambda e: e.dma_start(out=dst, in_=hout[:]), reads=[b_hout], writes=wr, chan="st")

        for _ in front(0):
            pass
        for i in range(nblk):
            gf = front(i + 1) if i + 1 < nblk else None
            weave({"f": gf, "b": back(i)}, "fbbfbbfbb")
        P.barrier()


def phase_qkv(P, nc, C, T, h_in, qT_d, kT_d, v_d, mix_norm, w_in, q_norm, k_norm, tag="qkv"):
    H = 8
    NB = min(512, T)
    NS = NB // 128
    nblk = T // NB
    with contextlib.ExitStack() as st:
        S = alloc_norm_scratch(P, nc, st, tag)
        A = lambda name, shape, dt: _alloc(st, nc, name + "_" + tag, shape, dt)
        g_bc = A("gbc", [128, D], F32); b_g = Buf("g")
        load_bcast(P, nc, g_bc[:], b_g, mix_norm)
        gq = A("gq", [128, 16, 64], F32); gk = A("gk", [128, 16, 64], F32); b_gq = Buf("gq"); b_gk = Buf("gk")
        P.op("sp", lambda e: e.dma_start(out=gq[:, 0, :], in_=q_norm.partition_broadcast(128)), writes=[b_gq], chan="ld")
        P.op("sp", lambda e: e.dma_start(out=gk[:, 0, :], in_=k_norm.partition_broadcast(128)), writes=[b_gk], chan="ld")
        for r in range(1, 16):
            P.op("pool", lambda e, r=r: e.tensor_copy(out=gq[:, r, :], in_=gq[:, 0, :]), reads=[b_gq], writes=[b_gq])
            P.op("pool", lambda e, r=r: e.tensor_copy(out=gk[:, r, :], in_=gk[:, 0, :]), reads=[b_gk], writes=[b_gk])
        win = A("win", [128, KC, 3 * D], BF16); b_win = Buf("win")
        for q3 in range(3):
            srcw = w_in[:, q3 * D:(q3 + 1) * D].rearrange("(k p) f -> p k f", p=128)
            P.op("pool", lambda e, srcw=srcw, q3=q3: e.dma_start(out=win[:, :, q3 * D:(q3 + 1) * D], in_=srcw),
                 writes=[b_win], chan="ld")
        h_t = [A("h%d" % i, [128, NS, D], F32) for i in range(2)]; b_h = [Buf("h0"), Buf("h1")]
        xn = [A("xn%d" % i, [128, D], BF16) for i in range(2)]; b_xn = [Buf("xn0"), Buf("xn1")]
        xnT = [A("xnT%d" % i, [128, KC, 128], BF16) for i in range(2)]; b_xnT = [Buf("xnT0"), Buf("xnT1")]
        qf = [[A("qf%d%d" % (w, i), [128, D], F32) for i in range(2)] for w in range(2)]
        b_qf = [[Buf("qf%d%d" % (w, i)) for i in range(2)] for w in range(2)]
        sq = [[A("sq%d%d" % (w, i), [128, D], F32) for i in range(2)] for w in range(2)]
        b_sq = [[Buf("sq%d%d" % (w, i)) for i in range(2)] for w in range(2)]
        ssq = [[A("ssq%d%d" % (w, i), [128, 3, 16], F32) for i in range(2)] for w in range(2)]
        b_ssq = [[Buf("ssq%d%d" % (w, i)) for i in range(2)] for w in range(2)]
        qn16 = [A("qn16_%d" % w, [128, D], BF16) for w in range(2)]; b_qn16 = [Buf("qn0"), Buf("qn1")]
        qTs = [A("qTs%d" % i, [128, H, NB], BF16) for i in range(2)]; b_qTs = [Buf("qTs0"), Buf("qTs1")]
        kTs = [A("kTs%d" % i, [128, H, NB], BF16) for i in range(2)]; b_kTs = [Buf("kTs0"), Buf("kTs1")]
        v16 = [A("v16%d" % i, [128, NS, D], BF16) for i in range(2)]; b_v16 = [Buf("v0"), Buf("v1")]
        pz = [_psum(st, nc, "pz%d_%s" % (i, tag), [128, 512]) for i in range(4)]; b_pz = [Buf("pz%d" % i) for i in range(4)]
        tp = _psum(st, nc, "tp_" + tag, [128, KC * 128], BF16); b_tp = Buf("tp")
        tpq = [_psum(st, nc, "tpq%d_%s" % (i, tag), [128, KC * 128], BF16) for i in range(2)]; b_tpq = [Buf("tpq0"), Buf("tpq1")]

        def load(blk):
            s = blk % 2
            src = h_in[blk * NB:(blk + 1) * NB, :].rearrange("(s p) d -> p s d", p=128)
            P.op("sp", lambda e: e.dma_start(out=h_t[s][:], in_=src), writes=[b_h[s]], chan="ld")

        pzc = [0]

        def stageA(u):
            blk, ts = divmod(u, NS)
            s = blk % 2
            u2 = u % 2
            if ts == 0 and blk + 1 < nblk:
                load(blk + 1)
            rmsnorm_rows(P, nc, S, h_t[s][:, ts, :], b_h[s], g_bc[:], b_g, xn[u2][:], b_xn[u2], tag)
            transpose_rows(P, nc, C, lambda kc: xn[u2][:, kc * 128:(kc + 1) * 128], b_xn[u2], tp, b_tp, xnT[u2][:], b_xnT[u2], evac="dve")
            for which in range(3):
                for half in range(2):
                    cb = which * 2 + half
                    p4 = pzc[0] % 4
                    pzc[0] += 1

                    def mm(e, cb=cb, p4=p4):
                        ins = None
                        for kc in range(KC):
                            ins = e.matmul(pz[p4][:], lhsT=xnT[u2][:, kc, :], rhs=win[:, kc, cb * 512:(cb + 1) * 512],
                                           start=(kc == 0), stop=(kc == KC - 1))
                        return ins
                    P.op("pe", mm, reads=[b_xnT[u2], b_win], writes=[b_pz[p4]])
                    cs = slice(half * 512, half * 512 + 512)
                    if which == 2:
                        P.op("act", lambda e, p4=p4, cs=cs: e.copy(out=v16[s][:, ts, cs], in_=pz[p4][:]),
                             reads=[b_pz[p4]], writes=[b_v16[s]])
                    else:
                        P.op("dve", lambda e, p4=p4, cs=cs, which=which: e.tensor_copy(out=qf[which][u2][:, cs], in_=pz[p4][:]),
                             reads=[b_pz[p4]], writes=[b_qf[which][u2]])
                        P.op("act", lambda e, cs=cs, which=which: e.activation(out=sq[which][u2][:, cs], in_=qf[which][u2][:, cs], func=AF.Square),
                             reads=[b_qf[which][u2]], writes=[b_sq[which][u2]])

        def stageB(u):
            blk, ts = divmod(u, NS)
            s = blk % 2
            u2 = u % 2
            for which in range(2):
                gg, b_gg = (gq, b_gq) if which == 0 else (gk, b_gk)
                dstT, b_dstT = (qTs[s], b_qTs[s]) if which == 0 else (kTs[s], b_kTs[s])
                tpx, b_tpx = tpq[which], b_tpq[which]
                qf_, sq_, ssq_ = qf[which][u2], sq[which][u2], ssq[which][u2]
                bq, bs, bss = b_qf[which][u2], b_sq[which][u2], b_ssq[which][u2]
                P.op("dve", lambda e, sq_=sq_, ssq_=ssq_: e.tensor_reduce(out=ssq_[:, 0, :], in_=sq_[:].rearrange("p (g d) -> p g d", g=16),
                                                                         axis=AX.X, op=ALU.add), reads=[bs], writes=[bss])
                P.op("act", lambda e, ssq_=ssq_: e.activation(out=ssq_[:, 1, :], in_=ssq_[:, 0, :], func=AF.Ln, scale=1.0 / 64,
                                                              bias=S.eps_t[:, 0:1]), reads=[S.b_eps], writes=[bss])
                P.op("act", lambda e, ssq_=ssq_: e.activation(out=ssq_[:, 2, :], in_=ssq_[:, 1, :], func=AF.Exp, scale=-0.5), writes=[bss])
                P.op("dve", lambda e, qf_=qf_, ssq_=ssq_: e.tensor_tensor(
                    out=qf_[:].rearrange("p (g d) -> p g d", g=16), in0=qf_[:].rearrange("p (g d) -> p g d", g=16),
                    in1=ssq_[:, 2, :].unsqueeze(2).to_broadcast([128, 16, 64]), op=ALU.mult), reads=[bss], writes=[bq])
                P.op("dve", lambda e, gg=gg, qf_=qf_, which=which: e.tensor_tensor(
                    out=qn16[which][:], in0=qf_[:], in1=gg[:].rearrange("p g d -> p (g d)"), op=ALU.mult),
                    reads=[bq, b_gg], writes=[b_qn16[which]])

                def tq(e, tpx=tpx, which=which):
                    ins = None
                    for hh in range(H):
                        ins = e.transpose(out=tpx[:, hh * 128:(hh + 1) * 128], in_=qn16[which][:, hh * 128:(hh + 1) * 128],
                                          identity=C.ident[:])
                    return ins
                P.op("pe", tq, reads=[b_qn16[which], C.b_ident], writes=[b_tpx])
                P.op("dve", lambda e, tpx=tpx, dstT=dstT, ts=ts: e.tensor_copy(
                    out=dstT[:, :, ts * 128:(ts + 1) * 128], in_=tpx[:].rearrange("p (k t) -> p k t", k=H)),
                    reads=[b_tpx], writes=[b_dstT])
            if ts == NS - 1:
                tsl = slice(blk * NB, (blk + 1) * NB)
                P.op("sp", lambda e: e.dma_start(out=qT_d[:, :, tsl].rearrange("h p t -> p h t"), in_=qTs[s][:]),
                     reads=[b_qTs[s]], chan="st")
                P.op("sp", lambda e: e.dma_start(out=kT_d[:, :, tsl].rearrange("h p t -> p h t"), in_=kTs[s][:]),
                     reads=[b_kTs[s]], chan="st")
                P.op("sp", lambda e: e.dma_start(out=v_d[tsl, :].rearrange("(s p) d -> p s d", p=128), in_=v16[s][:]),
                     reads=[b_v16[s]], chan="st")

        load(0)
        nu = nblk * NS
        stageA(0)
        for u in range(nu):
            if u + 1 < nu:
                stageA(u + 1)
            stageB(u)
        P.barrier()


def phase_attn(P, nc, C, T, qT_d, kT_d, v_d, on_d, q_norm, k_norm, lq1, lk1, lq2, lk2, sub_norm, lambda_init, tag="att"):
    H = 8
    NI = T // 256
    NJ = T // 128
    with contextlib.ExitStack() as st:
        A = lambda name, shape, dt: _alloc(st, nc, name + "_" + tag, shape, dt)
        vec = A("vec", [128, 6, 64], F32); b_vec = Buf("vec")
        for i, v_ in enumerate((q_norm, k_norm, lq1, lk1, lq2, lk2)):
            P.op("sp", lambda e, i=i, v_=v_: e.dma_start(out=vec[:, i, :], in_=v_.partition_broadcast(128)), writes=[b_vec], chan="ld")
        subn = A("subn", [128, 128], F32); b_subn = Buf("subn")
        load_bcast(P, nc, subn[:], b_subn, sub_norm)
        P.op("dve", lambda e: e.tensor_scalar(out=subn[:], in0=subn[:], scalar1=float(1.0 - lambda_init), scalar2=None, op0=ALU.mult),
             writes=[b_subn])
        sc = A("sc", [128, 12], F32); b_sc = Buf("sc")
        junk = A("junk", [128, 128], F32); b_junk = Buf("junk")
        eps_t = A("eps", [128, 1], F32); b_eps = Buf("eps")
        P.op("dve", lambda e: e.memset(eps_t[:], EPS), writes=[b_eps])
        P.op("dve", lambda e: e.tensor_reduce(out=sc[:, 0:2], in_=vec[:, 0:2, :], axis=AX.X, op=ALU.max, apply_absolute_value=True),
             reads=[b_vec], writes=[b_sc])
        P.op("dve", lambda e: e.tensor_tensor(out=sc[:, 2:3], in0=sc[:, 0:1], in1=sc[:, 1:2], op=ALU.mult), writes=[b_sc])
        P.op("dve", lambda e: e.tensor_scalar(out=sc[:, 3:4], in0=sc[:, 2:3], scalar1=-8.0, scalar2=None, op0=ALU.mult), writes=[b_sc])
        P.op("dve", lambda e: e.scalar_tensor_tensor(out=junk[:, 0:64], in0=vec[:, 2, :], scalar=1.0, in1=vec[:, 3, :], op0=ALU.mult,
                                                     op1=ALU.mult, accum_out=sc[:, 4:5]), reads=[b_vec], writes=[b_junk, b_sc])
        P.op("dve", lambda e: e.scalar_tensor_tensor(out=junk[:, 0:64], in0=vec[:, 4, :], scalar=1.0, in1=vec[:, 5, :], op0=ALU.mult,
                                                     op1=ALU.mult, accum_out=sc[:, 5:6]), reads=[b_vec], writes=[b_junk, b_sc])
        P.op("act", lambda e: e.activation(out=sc[:, 6:8], in_=sc[:, 4:6], func=AF.Exp), writes=[b_sc])
        P.op("dve", lambda e: e.tensor_tensor(out=sc[:, 8:9], in0=sc[:, 7:8], in1=sc[:, 6:7], op=ALU.subtract), writes=[b_sc])
        P.op("dve", lambda e: e.tensor_scalar(out=sc[:, 8:9], in0=sc[:, 8:9], scalar1=-float(lambda_init), scalar2=None, op0=ALU.add),
             writes=[b_sc])
        NM = 2 * NI + 1
        dtab_i = A("dtabi", [128, NM], mybir.dt.int32); dtab = A("dtab", [128, NM], F32); b_dtab = Buf("dtab")
        P.op("pool", lambda e: e.iota(out=dtab_i[:], pattern=[[-128, NM]], base=128, channel_multiplier=1), writes=[b_dtab])
        P.op("dve", lambda e: e.tensor_copy(out=dtab[:], in_=dtab_i[:]), writes=[b_dtab])
        btab = A("btab", [128, H, NM], F32); b_btab = Buf("btab")
        for hh in range(H):
            slope = 2.0 ** (-8.0 * (hh + 1) / H)
            P.op("dve", lambda e, hh=hh, slope=slope: e.tensor_scalar(out=btab[:, hh, :], in0=dtab[:], scalar1=slope, scalar2=sc[:, 3:4],
                                                                      op0=ALU.mult, op1=ALU.add), reads=[b_dtab, b_sc], writes=[b_btab])
        kT_t = [A("kT%d" % i, [128, 2, T], BF16) for i in range(2)]; b_kT = [Buf("kT0"), Buf("kT1")]
        qT_t = [A("qT%d" % i, [128, 2, T], BF16) for i in range(2)]; b_qT = [Buf("qT0"), Buf("qT1")]
        v_t = [A("vt%d" % i, [128, NJ, 129], BF16) for i in range(2)]; b_v = [Buf("vt0"), Buf("vt1")]
        qaug_i = A("qaugi", [128, 256], mybir.dt.int32); b_qaug = Buf("qaug")
        P.op("pool", lambda e: e.iota(out=qaug_i[64:65, :], pattern=[[-8, 256]], base=0, channel_multiplier=0), writes=[b_qaug])
        for i in range(2):
            P.op("pool", lambda e, i=i: e.memset(v_t[i][:, :, 128:129], 1.0), writes=[b_v[i]])
            for c in range(2):
                for I in range(NI):
                    P.op("dve", lambda e, i=i, c=c, I=I: e.tensor_copy(out=qT_t[i][64:65, c, I * 256:(I + 1) * 256], in_=qaug_i[64:65, :]),
                         reads=[b_qaug], writes=[b_qT[i]])
        NPT = 4
        PT = [A("PT%d" % i, [128, 2, 256], BF16) for i in range(NPT)]; b_PT = [Buf("PT%d" % i) for i in range(NPT)]
        tri = A("tri", [128, 2, 128], BF16); b_tri = Buf("tri")
        P.op("pool", lambda e: e.memset(tri[:], 1.0), writes=[b_tri])
        P.op("pool", lambda e: e.affine_select(out=tri[:], in_=tri[:], pattern=[[0, 2], [1, 128]], compare_op=ALU.is_ge, fill=0.0,
                                               base=0, channel_multiplier=-1), writes=[b_tri])
        o_t = A("o", [128, 128], F32); b_o = Buf("o")
        rz = A("rz", [128, 8], F32); b_rz = Buf("rz")
        on16 = [A("on16_%d" % i, [128, 128], BF16) for i in range(2)]; b_on16 = [Buf("on0"), Buf("on1")]
        NPS = 3
        pS = [_psum(st, nc, "pS%d_%s" % (i, tag), [128, 2, 256]) for i in range(NPS)]; b_pS = [Buf("pS%d" % i) for i in range(NPS)]
        acc = [[_psum(st, nc, "acc%d%d_%s" % (par, sb, tag), [128, 512]) for sb in range(2)] for par in range(2)]
        b_acc = [[Buf("acc%d%d" % (par, sb)) for sb in range(2)] for par in range(2)]

        def load_head(hh):
            s = hh % 2
            slope = 2.0 ** (-8.0 * (hh + 1) / H)
            for c in range(2):
                P.op("sp", lambda e, c=c: e.dma_start(out=kT_t[s][0:64, c, :], in_=kT_d[hh, c * 64:(c + 1) * 64, :]),
                     writes=[b_kT[s]], chan="ld")
                P.op("sp", lambda e, c=c: e.dma_start(out=qT_t[s][0:64, c, :], in_=qT_d[hh, c * 64:(c + 1) * 64, :]),
                     writes=[b_qT[s]], chan="ld")
            P.op("dve", lambda e: e.memset(kT_t[s][64:65, :, :], slope), writes=[b_kT[s]])
            P.op("sp", lambda e: e.dma_start(out=v_t[s][:, :, 0:128],
                                             in_=v_d[:, hh * 128:(hh + 1) * 128].rearrange("(j p) v -> p j v", p=128)),
                 writes=[b_v[s]], chan="ld")

        SKIP = 160.0
        pairs = []
        for hh in range(H):
            slope = 2.0 ** (-8.0 * (hh + 1) / H)
            for I in range(NI):
                js = [j for j in range(2 * I + 2) if slope * max(0, 256 * I - (128 * j + 127)) < SKIP]
                for j in js:
                    pairs.append((hh, I, j, js[0], js))
        fin = [0]

        def emit_qk(n):
            hh, I, j, j0, js = pairs[n]
            s = hh % 2
            lo = 128 if j == 2 * I + 1 else 0
            ps = n % NPS

            def mqk(e):
                ins = None
                for c in range(2):
                    ins = e.matmul(pS[ps][:, c, lo:256], lhsT=kT_t[s][0:65, c, j * 128:(j + 1) * 128],
                                   rhs=qT_t[s][0:65, c, I * 256 + lo:(I + 1) * 256], start=True, stop=True)
                return ins
            P.op("pe", mqk, reads=[b_kT[s], b_qT[s]], writes=[b_pS[ps]])

        def emit_exp(n):
            hh, I, j, j0, js = pairs[n]
            lo = 128 if j == 2 * I + 1 else 0
            ps = n % NPS
            pt = n % NPT
            mi = (2 * I - j) + 1
            P.op("act", lambda e: e.activation(out=PT[pt][:, :, lo:256], in_=pS[ps][:, :, lo:256], func=AF.Exp, scale=0.125,
                                               bias=btab[:, hh, mi:mi + 1]), reads=[b_pS[ps], b_btab], writes=[b_PT[pt]])
            if j >= 2 * I:
                sbd = j - 2 * I
                P.op("dve", lambda e: e.tensor_tensor(out=PT[pt][:, :, sbd * 128:(sbd + 1) * 128], in0=PT[pt][:, :, sbd * 128:(sbd + 1) * 128],
                                                      in1=tri[:], op=ALU.mult), reads=[b_tri], writes=[b_PT[pt]])

        def emit_pv(n):
            hh, I, j, j0, js = pairs[n]
            s = hh % 2
            lo = 128 if j == 2 * I + 1 else 0
            pt = n % NPT
            for sb in range(lo // 128, 2):
                last = (j == 2 * I + sb)

                par = I % 2

                def mpv(e, sb=sb, last=last, par=par):
                    ins = None
                    for c in range(2):
                        ins = e.matmul(acc[par][sb][:, c * 256:c * 256 + 129], lhsT=PT[pt][:, c, sb * 128:(sb + 1) * 128], rhs=v_t[s][:, j, :],
                                       start=(j == j0 and c == 0), stop=last, skip_group_check=True)
                    return ins
                P.op("pe", mpv, reads=[b_PT[pt], b_v[s]], writes=[b_acc[par][sb]])
                if last:
                    f2 = fin[0] % 2
                    fin[0] += 1
                    a0, a1 = acc[par][sb][:, 0:129], acc[par][sb][:, 256:385]
                    rb = [b_acc[par][sb]]
                    P.op("dve", lambda e, a0=a0: e.reciprocal(out=rz[:, 0:1], in_=a0[:, 128:129]), reads=rb, writes=[b_rz])
                    P.op("dve", lambda e, a1=a1: e.reciprocal(out=rz[:, 1:2], in_=a1[:, 128:129]), reads=rb, writes=[b_rz])
                    P.op("dve", lambda e: e.tensor_tensor(out=rz[:, 2:3], in0=rz[:, 1:2], in1=sc[:, 8:9], op=ALU.mult),
                         reads=[b_sc], writes=[b_rz])
                    P.op("dve", lambda e, a0=a0: e.tensor_scalar(out=o_t[:], in0=a0[:, 0:128], scalar1=rz[:, 0:1], scalar2=None,
                                                                 op0=ALU.mult), reads=rb + [b_rz], writes=[b_o])
                    P.op("dve", lambda e, a1=a1: e.scalar_tensor_tensor(out=o_t[:], in0=a1[:, 0:128], scalar=rz[:, 2:3], in1=o_t[:],
                                                                        op0=ALU.mult, op1=ALU.add), reads=rb + [b_rz], writes=[b_o])
                    P.op("dve", lambda e: e.scalar_tensor_tensor(out=junk[:], in0=o_t[:], scalar=1.0, in1=o_t[:], op0=ALU.mult,
                                                                 op1=ALU.mult, accum_out=rz[:, 3:4]), reads=[b_o], writes=[b_junk, b_rz])
                    P.op("act", lambda e: e.activation(out=rz[:, 4:5], in_=rz[:, 3:4], func=AF.Ln, scale=1.0 / 128, bias=eps_t[:, 0:1]),
                         reads=[b_eps], writes=[b_rz])
                    P.op("act", lambda e: e.activation(out=rz[:, 5:6], in_=rz[:, 4:5], func=AF.Exp, scale=-0.5), writes=[b_rz])
                    P.op("dve", lambda e, f2=f2: e.scalar_tensor_tensor(out=on16[f2][:], in0=o_t[:], scalar=rz[:, 5:6], in1=subn[:],
                                                                        op0=ALU.mult, op1=ALU.mult),
                         reads=[b_o, b_rz, b_subn], writes=[b_on16[f2]])
                    t0 = I * 256 + sb * 128
                    P.op("sp", lambda e, f2=f2, t0=t0: e.dma_start(out=on_d[t0:t0 + 128, hh * 128:(hh + 1) * 128], in_=on16[f2][:]),
                         reads=[b_on16[f2]], chan="st")

        LA = 2
        load_head(0)
        loaded = {0}
        npairs = len(pairs)
        for n in range(min(LA, npairs)):
            emit_qk(n)
        for n in range(npairs):
            hh = pairs[n][0]
            if hh + 1 < H and (hh + 1) not in loaded:
                load_head(hh + 1)
                loaded.add(hh + 1)
            if n + LA < npairs:
                emit_qk(n + LA)
            emit_exp(n)
            emit_pv(n)
        P.barrier()


def phase_attn_out(P, nc, C, T, h_in, on_d, h_out, w_out, tag="ao"):
    nblk = T // 128
    with contextlib.ExitStack() as st:
        A = lambda name, shape, dt: _alloc(st, nc, name + "_" + tag, shape, dt)
        wout = A("wout", [128, KC, D], BF16); b_wout = Buf("wout")
        P.op("pool", lambda e: e.dma_start(out=wout[:], in_=w_out.rearrange("(k p) f -> p k f", p=128)), writes=[b_wout], chan="ld")
        h_t = [A("h%d" % i, [128, D], F32) for i in range(2)]; b_h = [Buf("h0"), Buf("h1")]
        on = [A("on%d" % i, [128, D], BF16) for i in range(2)]; b_on = [Buf("on0"), Buf("on1")]
        onT = A("onT", [128, KC, 128], BF16); b_onT = Buf("onT")
        ho = [A("ho%d" % i, [128, D], F32) for i in range(2)]; b_ho = [Buf("ho0"), Buf("ho1")]
        tp = _psum(st, nc, "tp_" + tag, [128, KC * 128], BF16); b_tp = Buf("tp")
        py = [_psum(st, nc, "py%d_%s" % (i, tag), [128, 512]) for i in range(2)]; b_py = [Buf("py0"), Buf("py1")]

        def load(i):
            s = i % 2
            P.op("sp", lambda e: e.dma_start(out=h_t[s][:], in_=h_in[i * 128:(i + 1) * 128, :]), writes=[b_h[s]], chan="ld")
            P.op("sp", lambda e: e.dma_start(out=on[s][:], in_=on_d[i * 128:(i + 1) * 128, :]), writes=[b_on[s]], chan="ld")
        load(0)
        for i in range(nblk):
            s = i % 2
            if i + 1 < nblk:
                load(i + 1)
            transpose_rows(P, nc, C, lambda kc: on[s][:, kc * 128:(kc + 1) * 128], b_on[s], tp, b_tp, onT[:], b_onT, evac="dve")
            for half in range(2):
                def my(e, half=half):
                    ins = None
                    for kc in range(KC):
                        ins = e.matmul(py[half][:], lhsT=onT[:, kc, :], rhs=wout[:, kc, half * 512:(half + 1) * 512],
                                       start=(kc == 0), stop=(kc == KC - 1))
                    return ins
                P.op("pe", my, reads=[b_onT, b_wout], writes=[b_py[half]])
                P.op("dve", lambda e, half=half: e.tensor_tensor(out=ho[s][:, half * 512:(half + 1) * 512], in0=py[half][:],
                                                                 in1=h_t[s][:, half * 512:(half + 1) * 512], op=ALU.add),
                     reads=[b_py[half], b_h[s]], writes=[b_ho[s]])
            P.op("sp", lambda e: e.dma_start(out=h_out[i * 128:(i + 1) * 128, :], in_=ho[s][:]), reads=[b_ho[s]], chan="st")
        P.barrier()


def build_program(T, phases=("glu0",), N=1024):
    nc = bass.Bass("TRN2", target_bir_lowering=False)
    stack = contextlib.ExitStack()
    with stack:
        P = Prog(nc, stack)
        C = Ctx()
        inp = lambda name, shape: nc.dram_tensor(name, list(shape), F32, kind="ExternalInput").ap()
        x = inp("x", [T, D])
        out = nc.dram_tensor("out", [T, D], F32, kind="ExternalOutput").ap()
        emit_consts(P, nc, stack, C)
        nblk = T // N
        cur, b_cur = x, [Buf("x%d" % i) for i in range(nblk)]
        nscratch = 0
        for pi, ph in enumerate(phases):
            last = pi == len(phases) - 1
            if last:
                nxt = out
            else:
                nxt = nc.dram_tensor("scr%d" % nscratch, [T, D], F32, kind="Internal").ap()
                nscratch += 1
            b_nxt = [Buf("s%d_%d" % (pi, i)) for i in range(nblk)]
            if ph == "glu0":
                phase_glu(P, nc, C, T, cur, nxt, b_cur, b_nxt, inp("l0_ffn_norm", [D]),
                          inp("l0_ffn_w_gate_up", [D, 2 * 2816]), inp("l0_ffn_w_down", [2816, D]), 2816, 1, N=N, tag="f0")
            elif ph == "hgrn":
                phase_hgrn(P, nc, C, T, cur, nxt, b_cur, b_nxt, N, inp("l0_mix_norm", [D]), inp("l0_hgrn_w_in", [D, 4 * D]),
                           inp("l0_hgrn_out_norm", [D]), inp("l0_hgrn_w_out", [D, D]), inp("lower_bounds", [3, D]))
            elif ph == "attn":
                bf = lambda name, shape: nc.dram_tensor(name, list(shape), BF16, kind="Internal").ap()
                qT_d, kT_d = bf("qT_d", [8, 128, T]), bf("kT_d", [8, 128, T])
                v_d, on_d = bf("v_d", [T, D]), bf("on_d", [T, D])
                qn, kn = inp("l1_q_norm", [64]), inp("l1_k_norm", [64])
                phase_qkv(P, nc, C, T, cur, qT_d, kT_d, v_d, inp("l1_mix_norm", [D]), inp("l1_diff_w_in", [D, 3 * D]), qn, kn)
                phase_attn(P, nc, C, T, qT_d, kT_d, v_d, on_d, qn, kn, inp("l1_lambda_q1", [64]), inp("l1_lambda_k1", [64]),
                           inp("l1_lambda_q2", [64]), inp("l1_lambda_k2", [64]), inp("l1_diff_sub_norm", [128]),
                           0.8 - 0.6 * float(np.exp(-0.3 * 1)))
                phase_attn_out(P, nc, C, T, cur, on_d, nxt, inp("l1_diff_w_out", [D, D]))
            elif ph == "moe":
                phase_glu(P, nc, C, T, cur, nxt, b_cur, b_nxt, inp("l1_ffn_norm", [D]),
                          inp("l1_moe_w_gate_up", [8, D, 2 * 3584]), inp("l1_moe_w_down", [8, 3584, D]), 3584, 8,
                          router=inp("l1_router", [D, 8]), N=N, tag="f1")
            cur, b_cur = nxt, b_nxt
        P.barrier()
        print("ops:", P.n_ops, "sems:", P.nsem)
    return nc


_NC_CACHE = {}
PHASES = ("hgrn", "glu0", "attn", "moe")


def kernel(**inputs):
    x = np.ascontiguousarray(np.asarray(inputs["x"], dtype=np.float32))
    B, T, _ = x.shape
    if T not in _NC_CACHE:
        _NC_CACHE[T] = build_program(T, phases=PHASES, N=1024)
    nc = _NC_CACHE[T]
    shared = {k: np.ascontiguousarray(np.asarray(v, dtype=np.float32)) for k, v in inputs.items() if k != "x"}
    in_maps = []
    for b in range(B):
        m = dict(shared)
        m["x"] = x[b]
        in_maps.append(m)
    res = run_bass_kernel_spmd(nc, in_maps, core_ids=list(range(B)))
    return np.stack([np.asarray(r["out"], dtype=np.float32) for r in res.results], axis=0)
```

```python
import contextlib
import numpy as np
import concourse.bass as bass
import concourse.mybir as mybir
from concourse.bass_utils import run_bass_kernel_spmd

F32 = mybir.dt.float32
BF16 = mybir.dt.bfloat16
AF = mybir.ActivationFunctionType
ALU = mybir.AluOpType
AX = mybir.AxisListType

D = 1024
KC = 8
EPS = 1e-6
SEM_LIMIT = 30000
MAX_OPS = [10 ** 9]


class Buf:
    __slots__ = ("name", "w", "r", "uid", "excl")
    _n = [0]

    def __init__(self, name, excl=False):
        Buf._n[0] += 1
        self.uid = Buf._n[0]
        self.excl = excl or name.startswith(("p", "tp", "acc"))
        self.name = name
        self.w = None
        self.r = []


class Prog:
    def __init__(self, nc, stack):
        self.nc = nc
        self.stack = stack
        self.eng = {"pe": nc.tensor, "act": nc.scalar, "dve": nc.vector, "pool": nc.gpsimd, "sp": nc.sync}
        self.cur = {}
        self.waited = {k: {} for k in self.eng}
        self.semobj = {}
        self.nsem = 0
        self.n_ops = 0

    def _newsem(self, key):
        s = self.stack.enter_context(self.nc.semaphore("sm%d_%s" % (self.nsem, key)))
        self.nsem += 1
        self.semobj[id(s)] = s
        return s

    def _tick(self, key, inc):
        ent = self.cur.get(key)
        if ent is None or ent[1] + inc > SEM_LIMIT:
            ent = [self._newsem(key), 0]
            self.cur[key] = ent
        ent[1] += inc
        return ent[0], ent[1]

    def _wait(self, e, dep):
        if dep is None:
            return
        sem, val = dep
        w = self.waited[e]
        if w.get(id(sem), 0) >= val:
            return
        self.eng[e].wait_ge(sem, val)
        w[id(sem)] = val

    def op(self, e, fn, reads=(), writes=(), chan=None):
        if self.n_ops >= MAX_OPS[0]:
            return None
        for b in reads:
            self._wait(e, b.w)
            if b.excl:
                for r in b.r:
                    self._wait(e, r)
        for b in writes:
            self._wait(e, b.w)
            for r in b.r:
                self._wait(e, r)
        ins = fn(self.eng[e])
        if chan is None:
            sem, val = self._tick(e, 1)
            ins.then_inc(sem, 1)
        else:
            key = ("ld%d" % writes[0].uid) if chan != "st" else ("st%d" % reads[0].uid)
            sem, val = self._tick(key, 16)
            ins.then_inc(sem, 16)
        tok = (sem, val)
        for b in reads:
            b.r.append(tok)
            if len(b.r) > 64:
                b.r = b.r[-64:] if False else b.r
        for b in writes:
            b.w = tok
            b.r = []
        self.n_ops += 1
        return tok

    def barrier(self, engines=None):
        for e in (engines or list(self.eng)):
            for key, (sem, val) in list(self.cur.items()):
                if val > 0:
                    self._wait(e, (sem, val))


class Ctx:
    pass


def weave(gens, pattern):
    live = {k: g for k, g in gens.items() if g is not None}
    for ch in pattern:
        g = live.get(ch)
        if g is not None:
            try:
                next(g)
            except StopIteration:
                live.pop(ch)
    for k in list(live):
        for _ in live[k]:
            pass


def _alloc(stack, nc, name, shape, dt):
    return stack.enter_context(nc.sbuf_tensor(name, list(shape), dt))


def _psum(stack, nc, name, shape, dt=F32):
    return stack.enter_context(nc.psum_tensor(name, list(shape), dt))


def emit_consts(P, nc, stack, C):
    C.ident_f = _alloc(stack, nc, "ident_f", [128, 128], F32)
    C.ident = _alloc(stack, nc, "ident", [128, 128], BF16)
    C.b_ident = Buf("ident")
    C.b_identf = Buf("identf")

    P.op("pool", lambda e: e.memset(C.ident_f[:], 1.0), writes=[C.b_identf])
    P.op("pool", lambda e: e.affine_select(out=C.ident_f[:], in_=C.ident_f[:], pattern=[[-1, 128]],
                                           compare_op=ALU.is_equal, fill=0.0, base=0, channel_multiplier=1),
         writes=[C.b_identf])
    P.op("dve", lambda e: e.tensor_copy(out=C.ident[:], in_=C.ident_f[:]), reads=[C.b_identf], writes=[C.b_ident])


def load_bcast(P, nc, dst, b_dst, src_vec_ap, chan="ld"):
    P.op("sp", lambda e: e.dma_start(out=dst, in_=src_vec_ap.partition_broadcast(128)), writes=[b_dst], chan=chan)


def rmsnorm_rows(P, nc, S, h_ap, b_h, g_bc, b_g, xn_out, b_xn, tag):
    P.op("act", lambda e: e.activation(out=S.junk[:], in_=h_ap, func=AF.Square, accum_out=S.ss[:, 0:1]),
         reads=[b_h], writes=[S.b_junk, S.b_ss])
    P.op("act", lambda e: e.activation(out=S.ss[:, 1:2], in_=S.ss[:, 0:1], func=AF.Ln, scale=1.0 / D, bias=S.eps_t[:, 0:1]),
         reads=[S.b_ss, S.b_eps], writes=[S.b_ss])
    P.op("act", lambda e: e.activation(out=S.ss[:, 2:3], in_=S.ss[:, 1:2], func=AF.Exp, scale=-0.5), reads=[S.b_ss], writes=[S.b_ss])
    P.op("dve", lambda e: e.scalar_tensor_tensor(out=xn_out, in0=h_ap, scalar=S.ss[:, 2:3], in1=g_bc,
                                                 op0=ALU.mult, op1=ALU.mult),
         reads=[b_h, S.b_ss, b_g], writes=[b_xn])


def alloc_norm_scratch(P, nc, stack, tag):
    S = Ctx()
    S.junk = _alloc(stack, nc, "junk_" + tag, [128, D], BF16)
    S.ss = _alloc(stack, nc, "ss_" + tag, [128, 4], F32)
    S.eps_t = _alloc(stack, nc, "eps_" + tag, [128, 1], F32)
    S.one_t = _alloc(stack, nc, "one_" + tag, [128, 1], F32)
    S.b_junk, S.b_ss, S.b_eps = Buf("junk"), Buf("ss"), Buf("eps")
    P.op("dve", lambda e: e.memset(S.eps_t[:], EPS), writes=[S.b_eps])
    P.op("dve", lambda e: e.memset(S.one_t[:], 1.0), writes=[S.b_eps])
    return S


def transpose_rows(P, nc, C, src_tile_ap_fn, b_src, tp, b_tp, dst_ap, b_dst, evac="act"):
    def pe(e):
        ins = None
        for kc in range(KC):
            ins = e.transpose(out=tp[:, kc * 128:(kc + 1) * 128], in_=src_tile_ap_fn(kc), identity=C.ident[:])
        return ins
    P.op("pe", pe, reads=[b_src, C.b_ident], writes=[b_tp])
    src3 = tp[:].rearrange("p (k t) -> p k t", k=KC)
    if evac == "act":
        P.op("act", lambda e: e.copy(out=dst_ap, in_=src3), reads=[b_tp], writes=[b_dst])
    else:
        P.op("dve", lambda e: e.tensor_copy(out=dst_ap, in_=src3), reads=[b_tp], writes=[b_dst])


def phase_glu(P, nc, C, T, h_in, h_out, bufs_in, bufs_out, norm_g, w_gu, w_down, F, E, router=None,
              N=1024, FG=4, tag="glu"):
    NS = N // 128
    NQ = N // 512
    FCH = F // 128
    groups = [(g0, min(FG, FCH - g0)) for g0 in range(0, FCH, FG)]
    nblk = T // N
    with contextlib.ExitStack() as st:
        S2 = [alloc_norm_scratch(P, nc, st, tag + "a"), alloc_norm_scratch(P, nc, st, tag + "b")]
        g_bc = _alloc(st, nc, "gbc_" + tag, [128, D], F32)
        b_g = Buf("g")
        load_bcast(P, nc, g_bc[:], b_g, norm_g)
        h_t = _alloc(st, nc, "h_" + tag, [128, NS, D], F32)
        b_h = [Buf("h%d" % i) for i in range(NS)]
        xnT = _alloc(st, nc, "xnT_" + tag, [128, KC, N], BF16)
        b_xnT = [Buf("xnT%d" % i) for i in range(NS)]
        xn = [_alloc(st, nc, "xn%d_%s" % (i, tag), [128, D], BF16) for i in range(2)]
        b_xn = [Buf("xn0"), Buf("xn1")]
        NW = 3
        wgu_t = [_alloc(st, nc, "wgu%d_%s" % (i, tag), [128, KC, 2, FG * 128], BF16) for i in range(NW)]
        wd_t = [_alloc(st, nc, "wd%d_%s" % (i, tag), [128, FG, D], BF16) for i in range(NW)]
        b_wgu = [Buf("wgu%d" % i) for i in range(NW)]
        b_wd = [Buf("wd%d" % i) for i in range(NW)]
        hT = [_alloc(st, nc, "hT%d_%s" % (i, tag), [128, FG, N], BF16) for i in range(2)]
        b_hT = [[Buf("hT%d_%d" % (i, j)) for j in range(FG)] for i in range(2)]
        sg = [_alloc(st, nc, "sg%d_%s" % (i, tag), [128, 512], BF16) for i in range(2)]
        b_sg = [Buf("sg0"), Buf("sg1")]
        tp = _psum(st, nc, "tp_" + tag, [128, KC * 128], BF16)
        b_tp = Buf("tp")
        pg = [_psum(st, nc, "pg%d_%s" % (i, tag), [128, 512]) for i in range(2)]
        pu = [_psum(st, nc, "pu%d_%s" % (i, tag), [128, 512]) for i in range(2)]
        py = [_psum(st, nc, "py%d_%s" % (i, tag), [128, 512]) for i in range(3)]
        b_pg = [Buf("pg0"), Buf("pg1")]
        b_pu = [Buf("pu0"), Buf("pu1")]
        b_py = [Buf("py0"), Buf("py1"), Buf("py2")]
        if E > 1:
            xn32_2 = [_alloc(st, nc, "xn32_%d_%s" % (i, tag), [128, D], F32) for i in range(2)]
            b_xn32_2 = [Buf("xn32a"), Buf("xn32b")]
            r32 = _alloc(st, nc, "r32_" + tag, [128, KC, E], F32)
            b_r = Buf("r32")
            P.op("sp", lambda eng: eng.dma_start(out=r32[:], in_=router.rearrange("(k p) e -> p k e", p=128)), writes=[b_r], chan="ld")
            xT32 = _alloc(st, nc, "xT32_" + tag, [128, KC, 128], F32)
            b_xT32 = Buf("xT32")
            lg = _alloc(st, nc, "lg_" + tag, [128, NS, 8], F32)
            gates = _alloc(st, nc, "gates_" + tag, [128, NS, 8], F32)
            top8 = _alloc(st, nc, "top8_" + tag, [128, 8], F32)
            gsm = _alloc(st, nc, "gsm_" + tag, [128, 4], F32)
            b_lg = [Buf("lg%d" % i) for i in range(NS)]
            b_gates = [Buf("gates%d" % i) for i in range(NS)]
            b_top8, b_gsm = Buf("top8"), Buf("gsm")

        wcount = 0
        for blk in range(nblk):
            if blk == 0:
                for ts in range(NS):
                    P.op("sp", lambda e, ts=ts: e.dma_start(out=h_t[:, ts, :], in_=h_in[ts * 128:(ts + 1) * 128, :]),
                         reads=[bufs_in[0]], writes=[b_h[ts]], chan="ld")
            for ts in range(NS):
                s2 = ts % 2
                S = S2[s2]
                if E > 1:
                    xn32, b_xn32 = xn32_2[s2], b_xn32_2[s2]
                    rmsnorm_rows(P, nc, S, h_t[:, ts, :], b_h[ts], g_bc[:], b_g, xn32[:], b_xn32, tag)
                    P.op("act", lambda e, s2=s2: e.copy(out=xn[s2][:], in_=xn32[:]), reads=[b_xn32],
                         writes=[b_xn[s2]])
                    def t32(e):
                        ins = None
                        for kc in range(KC):
                            ins = e.transpose(out=pg[kc // 4][:, (kc % 4) * 128:(kc % 4 + 1) * 128], in_=xn32[:, kc * 128:(kc + 1) * 128],
                                              identity=C.ident_f[:])
                        return ins
                    P.op("pe", t32, reads=[b_xn32, C.b_identf], writes=[b_pg[0], b_pg[1]])
                    for hf in range(2):
                        P.op("act", lambda e, hf=hf: e.copy(out=xT32[:, hf * 4:hf * 4 + 4, :],
                                                            in_=pg[hf][:].rearrange("p (k t) -> p k t", k=4)),
                             reads=[b_pg[hf]], writes=[b_xT32])

                    def mlg(e):
                        ins = None
                        for kc in range(KC):
                            ins = e.matmul(pu[0][:, 0:E], lhsT=xT32[:, kc, :], rhs=r32[:, kc, :], start=(kc == 0), stop=(kc == KC - 1))
                        return ins
                    P.op("pe", mlg, reads=[b_xT32, b_r], writes=[b_pu[0]])
                    P.op("dve", lambda e, ts=ts: e.tensor_copy(out=lg[:, ts, :], in_=pu[0][:, 0:E]), reads=[b_pu[0]], writes=[b_lg[ts]])
                    P.op("dve", lambda e, ts=ts: e.max(out=top8[:], in_=lg[:, ts, :]), reads=[b_lg[ts]], writes=[b_top8])
                    P.op("dve", lambda e: e.tensor_scalar(out=gsm[:, 0:1], in0=top8[:, 0:1], scalar1=-1.0, scalar2=None,
                                                          op0=ALU.mult), reads=[b_top8], writes=[b_gsm])
                    P.op("act", lambda e, ts=ts: e.activation(out=gates[:, ts, :], in_=lg[:, ts, :], func=AF.Exp,
                                                              bias=gsm[:, 0:1], scale=1.0),
                         reads=[b_lg[ts], b_gsm], writes=[b_gates[ts]])
                    P.op("dve", lambda e, ts=ts: e.tensor_scalar(out=lg[:, ts, :], in0=lg[:, ts, :], scalar1=top8[:, 1:2],
                                                                 scalar2=None, op0=ALU.is_ge),
                         reads=[b_top8], writes=[b_lg[ts]])
                    P.op("dve", lambda e, ts=ts: e.tensor_tensor(out=gates[:, ts, :], in0=gates[:, ts, :], in1=lg[:, ts, :],
                                                                 op=ALU.mult), reads=[b_lg[ts]], writes=[b_gates[ts]])
                    P.op("dve", lambda e, ts=ts: e.reduce_sum(out=gsm[:, 1:2], in_=gates[:, ts, :], axis=AX.X),
                         reads=[b_gates[ts]], writes=[b_gsm])
                    P.op("dve", lambda e: e.reciprocal(out=gsm[:, 2:3], in_=gsm[:, 1:2]), reads=[b_gsm], writes=[b_gsm])
                    P.op("dve", lambda e, ts=ts: e.tensor_scalar(out=gates[:, ts, :], in0=gates[:, ts, :], scalar1=gsm[:, 2:3],
                                                                 scalar2=None, op0=ALU.mult),
                         reads=[b_gsm], writes=[b_gates[ts]])
                else:
                    rmsnorm_rows(P, nc, S, h_t[:, ts, :], b_h[ts], g_bc[:], b_g, xn[s2][:], b_xn[s2], tag)
                transpose_rows(P, nc, C, lambda kc, s2=s2: xn[s2][:, kc * 128:(kc + 1) * 128], b_xn[s2], tp, b_tp,
                               xnT[:, :, ts * 128:(ts + 1) * 128], b_xnT[ts], evac="act")
            for ex in range(E):
                wgu_e = w_gu[ex] if E > 1 else w_gu
                wd_e = w_down[ex] if E > 1 else w_down
                for (g0, gn) in groups:
                    ws = wcount % NW
                    hs_ = wcount % 2
                    wcount += 1
                    for half in range(2):
                        srcw = wgu_e[:, half * F + g0 * 128: half * F + (g0 + gn) * 128].rearrange("(k p) f -> p k f", p=128)
                        P.op("pool", lambda e, srcw=srcw, ws=ws, half=half, gn=gn: e.dma_start(
                            out=wgu_t[ws][:, :, half, 0:gn * 128], in_=srcw), writes=[b_wgu[ws]], chan="ld")
                    srcd = wd_e[g0 * 128:(g0 + gn) * 128, :].rearrange("(c p) d -> p c d", p=128)
                    P.op("pool", lambda e, srcd=srcd, ws=ws, gn=gn: e.dma_start(out=wd_t[ws][:, 0:gn, :], in_=srcd),
                         writes=[b_wd[ws]], chan="ld")
                    pi = 0
                    for fcl in range(gn):
                        for q in range(NQ):
                            p2 = pi % 2
                            pi += 1
                            tsl = [b_xnT[q * 4 + i] for i in range(4)]

                            def mm(e, which, dst, fcl=fcl, q=q, ws=ws):
                                ins = None
                                for kc in range(KC):
                                    ins = e.matmul(dst[:], lhsT=wgu_t[ws][:, kc, which, fcl * 128:(fcl + 1) * 128],
                                                   rhs=xnT[:, kc, q * 512:(q + 1) * 512], start=(kc == 0), stop=(kc == KC - 1))
                                return ins
                            P.op("pe", lambda e, p2=p2, mm=mm: mm(e, 0, pg[p2]), reads=tsl + [b_wgu[ws]], writes=[b_pg[p2]])
                            P.op("pe", lambda e, p2=p2, mm=mm: mm(e, 1, pu[p2]), reads=tsl + [b_wgu[ws]], writes=[b_pu[p2]])
                            P.op("act", lambda e, p2=p2: e.activation(out=sg[p2][:], in_=pg[p2][:], func=AF.Silu),
                                 reads=[b_pg[p2]], writes=[b_sg[p2]])
                            P.op("dve", lambda e, p2=p2, hs_=hs_, fcl=fcl, q=q: e.tensor_tensor(
                                out=hT[hs_][:, fcl, q * 512:(q + 1) * 512], in0=sg[p2][:], in1=pu[p2][:], op=ALU.mult),
                                reads=[b_sg[p2], b_pu[p2]], writes=[b_hT[hs_][fcl]])
                    yi = 0
                    for ts in range(NS):
                        for dh in range(2):
                            y2 = yi % 3
                            yi += 1

                            def mmd(e, ts=ts, dh=dh, y2=y2, ws=ws, gn=gn, hs_=hs_):
                                ins = None
                                for fcl in range(gn):
                                    ins = e.matmul(py[y2][:], lhsT=hT[hs_][:, fcl, ts * 128:(ts + 1) * 128],
                                                   rhs=wd_t[ws][:, fcl, dh * 512:(dh + 1) * 512], start=(fcl == 0), stop=(fcl == gn - 1))
                                return ins
                            P.op("pe", mmd, reads=b_hT[hs_][0:gn] + [b_wd[ws]], writes=[b_py[y2]])
                            hs = h_t[:, ts, dh * 512:(dh + 1) * 512]
                            if E > 1:
                                P.op("dve", lambda e, hs=hs, y2=y2, ts=ts, ex=ex: e.scalar_tensor_tensor(
                                    out=hs, in0=py[y2][:], scalar=gates[:, ts, ex:ex + 1], in1=hs, op0=ALU.mult, op1=ALU.add),
                                    reads=[b_py[y2], b_gates[ts]], writes=[b_h[ts]])
                            else:
                                P.op("dve", lambda e, hs=hs, y2=y2: e.tensor_tensor(out=hs, in0=py[y2][:], in1=hs, op=ALU.add),
                                     reads=[b_py[y2]], writes=[b_h[ts]])
            for ts in range(NS):
                r0 = blk * N + ts * 128
                P.op("sp", lambda e, ts=ts, r0=r0: e.dma_start(out=h_out[r0:r0 + 128, :], in_=h_t[:, ts, :]), reads=[b_h[ts]],
                     writes=[bufs_out[blk]] if ts == NS - 1 else [], chan="st")
                if blk + 1 < nblk:
                    P.op("sp", lambda e, ts=ts, r0=r0: e.dma_start(out=h_t[:, ts, :], in_=h_in[r0 + N:r0 + N + 128, :]),
                         reads=[bufs_in[blk + 1]], writes=[b_h[ts]], chan="ld")
        P.barrier()


def phase_hgrn(P, nc, C, T, h_in, h_out, bufs_in, bufs_out, NB, mix_norm, w_in, out_norm, w_out, lower_bounds, tag="hg"):
    H = 8
    nblk = T // 128
    per = NB // 128
    with contextlib.ExitStack() as st:
        S = alloc_norm_scratch(P, nc, st, tag)
        A = lambda name, shape, dt: _alloc(st, nc, name + "_" + tag, shape, dt)
        g_bc = A("gbc", [128, D], F32); b_g = Buf("g")
        load_bcast(P, nc, g_bc[:], b_g, mix_norm)
        on_bc = A("onbc", [128, D], F32); b_on = Buf("on")
        load_bcast(P, nc, on_bc[:], b_on, out_norm)
        lb_bc = A("lbbc", [128, D], F32); oml_bc = A("omlbc", [128, D], F32); b_lb = Buf("lb")
        with contextlib.ExitStack() as st2:
            lbr = _alloc(st2, nc, "lbraw_" + tag, [128, 3, D], F32); b_lbr = Buf("lbr")
            for r in range(3):
                P.op("sp", lambda e, r=r: e.dma_start(out=lbr[:, r, :], in_=lower_bounds[r, :].partition_broadcast(128)),
                     writes=[b_lbr], chan="ld")
            P.op("act", lambda e: e.activation(out=lbr[:], in_=lbr[:], func=AF.Exp), writes=[b_lbr])
            P.op("dve", lambda e: e.tensor_tensor(out=oml_bc[:], in0=lbr[:, 0, :], in1=lbr[:, 1, :], op=ALU.add),
                 reads=[b_lbr], writes=[b_lb])
            P.op("dve", lambda e: e.tensor_tensor(out=oml_bc[:], in0=oml_bc[:], in1=lbr[:, 2, :], op=ALU.add),
                 reads=[b_lbr], writes=[b_lb])
            P.op("dve", lambda e: e.reciprocal(out=oml_bc[:], in_=oml_bc[:]), writes=[b_lb])
            P.op("dve", lambda e: e.tensor_tensor(out=lb_bc[:], in0=lbr[:, 0, :], in1=oml_bc[:], op=ALU.mult),
                 reads=[b_lbr], writes=[b_lb])
            P.op("dve", lambda e: e.tensor_scalar(out=oml_bc[:], in0=lb_bc[:], scalar1=-1.0, scalar2=1.0, op0=ALU.mult, op1=ALU.add),
                 writes=[b_lb])
            P.barrier()
        mask = A("mask", [128, 4, 128], F32); b_mask = Buf("mask")
        esel = A("esel", [128, 2], F32); b_esel = Buf("esel")

        P.op("pool", lambda e: e.memset(mask[:], 1.0), writes=[b_mask])
        P.op("pool", lambda e: e.affine_select(out=mask[:], in_=mask[:], pattern=[[0, 4], [1, 128]], compare_op=ALU.is_ge,
                                               fill=0.0, base=0, channel_multiplier=-1), writes=[b_mask])
        P.op("pool", lambda e: e.affine_select(out=mask[0:64], in_=mask[0:64], pattern=[[0, 4], [-1, 128]],
                                               compare_op=ALU.is_ge, fill=0.0, base=63, channel_multiplier=0),
             writes=[b_mask])
        P.op("pool", lambda e: e.memset(esel[:], 0.0), writes=[b_esel])
        P.op("pool", lambda e: e.memset(esel[0:64, 0:1], 1.0), writes=[b_esel])
        P.op("pool", lambda e: e.memset(esel[64:128, 1:2], 1.0), writes=[b_esel])
        win = A("win", [128, KC, 4 * D], BF16); b_win = Buf("win")
        wout = A("wout", [128, KC, D], BF16); b_wout = Buf("wout")
        for q4 in range(4):
            srcw = w_in[:, q4 * D:(q4 + 1) * D].rearrange("(k p) f -> p k f", p=128)
            P.op("pool", lambda e, srcw=srcw, q4=q4: e.dma_start(out=win[:, :, q4 * D:(q4 + 1) * D], in_=srcw),
                 writes=[b_win], chan="ld")
        P.op("pool", lambda e: e.dma_start(out=wout[:], in_=w_out.rearrange("(k p) f -> p k f", p=128)),
             writes=[b_wout], chan="ld")
        def two(name, shape, dt):
            return [A(name + str(i), shape, dt) for i in range(2)], [Buf(name + str(i)) for i in range(2)]
        h_t, b_h = two("h", [128, D], F32)
        qtT, b_qtT = two("qtT", [128, H, 128], BF16)
        qtA, b_qtA = two("qtA", [128, H, 128], BF16)
        qtB, b_qtB = two("qtB", [128, H, 128], BF16)
        ktT, b_ktT = two("ktT", [128, H, 128], BF16)
        kt, b_kt = two("kt", [128, D], BF16)
        v16, b_v16 = two("v16", [128, D], BF16)
        gsn, b_gsn = two("gsn", [128, D], F32)
        dec, b_dec = two("dec", [128, H, 2], F32)
        for i in range(2):
            P.op("pool", lambda e, i=i: e.memset(qtA[i][:], 0.0), writes=[b_qtA[i]])
            P.op("pool", lambda e, i=i: e.memset(qtB[i][:], 0.0), writes=[b_qtB[i]])
        xn = A("xn", [128, D], BF16); b_xn = Buf("xn")
        xnT = A("xnT", [128, KC, 128], BF16); b_xnT = Buf("xnT")
        qs = A("qs", [128, D], F32); b_qs = Buf("qs")
        fg = A("fg", [128, D], F32); b_fg = Buf("fg")
        kk = A("kk", [128, D], F32); b_kk = Buf("kk")
        eb = A("eb", [128, D], F32); b_eb = Buf("eb")
        enb = A("enb", [128, D], F32); b_enb = Buf("enb")
        qt = A("qt", [128, D], BF16); b_qt = Buf("qt")
        SA = A("SA", [128, H, 128], BF16); b_SA = Buf("SA")
        SB = A("SB", [128, H, 128], BF16); b_SB = Buf("SB")
        P.op("pool", lambda e: e.memset(SA[:], 0.0), writes=[b_SA])
        scT = A("scT", [128, H, 128], BF16); b_scT = Buf("scT")
        osb = A("osb", [128, D], F32); b_osb = Buf("osb")
        sq = A("sq", [128, D], F32); b_sq = Buf("sq")
        ssq = A("ssq", [128, 3, H], F32); b_ssq = Buf("ssq")
        on16 = A("on16", [128, D], BF16); b_on16 = Buf("on16")
        onT = A("onT", [128, KC, 128], BF16); b_onT = Buf("onT")
        hout = A("hout", [128, D], F32); b_hout = Buf("hout")
        pz = [_psum(st, nc, "pz%d_%s" % (i, tag), [128, 512]) for i in range(2)]; b_pz = [Buf("pz0"), Buf("pz1")]
        tp = _psum(st, nc, "tp_" + tag, [128, KC * 128], BF16); b_tp = Buf("tp")
        pbe_bank = _psum(st, nc, "pbe_" + tag, [128, 512]); b_pbe = Buf("pbe")
        pbe = pbe_bank[:, 0:2 * H].rearrange("p (h c) -> p h c", c=2)
        pk = [_psum(st, nc, "pk%d_%s" % (i, tag), [128, 4, 128]) for i in range(4)]; b_pk = [Buf("pk%d" % i) for i in range(4)]

        def front(i):
            s = i % 2
            src = h_in[i * 128:(i + 1) * 128, :]
            P.op("sp", lambda e: e.dma_start(out=h_t[s][:], in_=src), reads=[bufs_in[i // per]], writes=[b_h[s]], chan="ld")
            rmsnorm_rows(P, nc, S, h_t[s][:], b_h[s], g_bc[:], b_g, xn[:], b_xn, tag)
            transpose_rows(P, nc, C, lambda kc: xn[:, kc * 128:(kc + 1) * 128], b_xn, tp, b_tp, xnT[:], b_xnT, evac="act")
            for oi, cb in enumerate((0, 1, 6, 7, 2, 3, 4, 5)):
                p2 = oi % 2

                def mm(e, cb=cb, p2=p2):
                    ins = None
                    for kc in range(KC):
                        ins = e.matmul(pz[p2][:], lhsT=xnT[:, kc, :], rhs=win[:, kc, cb * 512:(cb + 1) * 512],
                                       start=(kc == 0), stop=(kc == KC - 1))
                    return ins
                P.op("pe", mm, reads=[b_xnT, b_win], writes=[b_pz[p2]])
                cs = slice((cb % 2) * 512, (cb % 2) * 512 + 512)
                if cb < 2:
                    P.op("act", lambda e, p2=p2, cs=cs: e.activation(out=qs[:, cs], in_=pz[p2][:], func=AF.Silu),
                         reads=[b_pz[p2]], writes=[b_qs])
                elif cb < 4:
                    P.op("act", lambda e, p2=p2, cs=cs: e.activation(out=fg[:, cs], in_=pz[p2][:], func=AF.Sigmoid, scale=-1.0),
                         reads=[b_pz[p2]], writes=[b_fg])
                elif cb < 6:
                    P.op("act", lambda e, p2=p2, cs=cs: e.copy(out=v16[s][:, cs], in_=pz[p2][:]),
                         reads=[b_pz[p2]], writes=[b_v16[s]])
                else:
                    P.op("act", lambda e, p2=p2, cs=cs: e.activation(out=gsn[s][:, cs], in_=pz[p2][:], func=AF.Silu),
                         reads=[b_pz[p2]], writes=[b_gsn[s]])
            P.op("dve", lambda e: e.tensor_tensor(out=gsn[s][:], in0=gsn[s][:], in1=on_bc[:], op=ALU.mult),
                 reads=[b_on], writes=[b_gsn[s]])
            P.op("dve", lambda e: e.tensor_tensor(out=kk[:], in0=fg[:], in1=oml_bc[:], op=ALU.mult), reads=[b_lb, b_fg], writes=[b_kk])
            P.op("act", lambda e: e.activation(out=fg[:], in_=kk[:], func=AF.Ln, scale=-1.0, bias=S.one_t[:, 0:1]),
                 reads=[b_kk, S.b_eps], writes=[b_fg])
            yield
            for half in range(2):
                P.op("pe", lambda e, half=half: e.matmul(pz[half][:], lhsT=mask[:, 0, :], rhs=fg[:, half * 512:(half + 1) * 512],
                                                         start=True, stop=True),
                     reads=[b_mask, b_fg], writes=[b_pz[half]])

            def mbe(e):
                ins = None
                for hh in range(H):
                    ins = e.matmul(pbe[:, hh, :], lhsT=fg[:, hh * 128:(hh + 1) * 128], rhs=esel[:, 0:2], start=True, stop=True)
                return ins
            P.op("pe", mbe, reads=[b_fg, b_esel], writes=[b_pbe])
            for half in range(2):
                cs = slice(half * 512, half * 512 + 512)
                P.op("act", lambda e, half=half, cs=cs: e.activation(out=eb[:, cs], in_=pz[half][:], func=AF.Exp),
                     reads=[b_pz[half]], writes=[b_eb])
                P.op("act", lambda e, half=half, cs=cs: e.activation(out=enb[:, cs], in_=pz[half][:], func=AF.Exp, scale=-1.0),
                     reads=[b_pz[half]], writes=[b_enb])
            P.op("act", lambda e: e.activation(out=dec[s][:], in_=pbe[:], func=AF.Exp), reads=[b_pbe], writes=[b_dec[s]])
            P.op("dve", lambda e: e.tensor_tensor(out=qt[:], in0=qs[:], in1=eb[:], op=ALU.mult), reads=[b_qs, b_eb], writes=[b_qt])
            P.op("dve", lambda e: e.tensor_tensor(out=kt[s][:], in0=kk[:], in1=enb[:], op=ALU.mult), reads=[b_kk, b_enb],
                 writes=[b_kt[s]])
            yield
            def tq(e):
                ins = None
                for hh in range(H):
                    ins = e.transpose(out=tp[:, hh * 128:(hh + 1) * 128], in_=qt[:, hh * 128:(hh + 1) * 128], identity=C.ident[:])
                return ins
            P.op("pe", tq, reads=[b_qt, C.b_ident], writes=[b_tp])
            tp3 = tp[:].rearrange("p (k t) -> p k t", k=H)
            P.op("dve", lambda e: e.tensor_copy(out=qtT[s][:], in_=tp3), reads=[b_tp], writes=[b_qtT[s]])
            P.op("dve", lambda e: e.tensor_copy(out=qtA[s][:, :, 0:64], in_=tp3[:, :, 0:64]), reads=[b_tp], writes=[b_qtA[s]])
            P.op("dve", lambda e: e.tensor_copy(out=qtB[s][:, :, 64:128], in_=tp3[:, :, 64:128]), reads=[b_tp], writes=[b_qtB[s]])

            def tk(e):
                ins = None
                for hh in range(H):
                    ins = e.transpose(out=tp[:, hh * 128:(hh + 1) * 128], in_=kt[s][:, hh * 128:(hh + 1) * 128], identity=C.ident[:])
                return ins
            P.op("pe", tk, reads=[b_kt[s], C.b_ident], writes=[b_tp])
            P.op("act", lambda e: e.copy(out=ktT[s][:], in_=tp3), reads=[b_tp], writes=[b_ktT[s]])

        def state_update(s, c, Sin, b_Sin, Sout, b_Sout, banks):
            rows = slice(c * 64, c * 64 + 64)
            for half in range(2):
                bk = banks[half]

                def mu(e, half=half, bk=bk):
                    ins = None
                    for j in range(4):
                        hh = half * 4 + j
                        e.matmul(pk[bk][:, j, :], lhsT=kt[s][rows, hh * 128:(hh + 1) * 128], rhs=v16[s][rows, hh * 128:(hh + 1) * 128],
                                 start=True, stop=False)
                        ins = e.matmul(pk[bk][:, j, :], lhsT=C.ident[:], rhs=Sin[:, hh, :], start=False, stop=True)
                    return ins
                P.op("pe", mu, reads=[b_kt[s], b_v16[s], b_Sin, C.b_ident], writes=[b_pk[bk]])
                dsl = dec[s][:, half * 4:half * 4 + 4, c:c + 1].to_broadcast([128, 4, 128])
                P.op("dve", lambda e, half=half, bk=bk, dsl=dsl: e.tensor_tensor(
                    out=Sout[:, half * 4:half * 4 + 4, :], in0=pk[bk][:], in1=dsl, op=ALU.mult),
                    reads=[b_pk[bk], b_dec[s]], writes=[b_Sout])

        def back(i):
            s = i % 2
            state_update(s, 0, SA, b_SA, SB, b_SB, (0, 1))
            yield
            for half in range(2):
                bk = 2 + half

                def msc(e, half=half, bk=bk):
                    ins = None
                    for j in range(4):
                        hh = half * 4 + j
                        ins = e.matmul(pk[bk][:, j, :], lhsT=ktT[s][:, hh, :], rhs=qtT[s][:, hh, :], start=True, stop=True)
                    return ins
                P.op("pe", msc, reads=[b_ktT[s], b_qtT[s]], writes=[b_pk[bk]])
                P.op("dve", lambda e, half=half, bk=bk: e.tensor_tensor(out=scT[:, half * 4:half * 4 + 4, :], in0=pk[bk][:],
                                                                         in1=mask[:], op=ALU.mult),
                     reads=[b_pk[bk], b_mask], writes=[b_scT])
            yield
            for half in range(2):
                bk = half

                def mo(e, half=half, bk=bk):
                    ins = None
                    for j in range(4):
                        hh = half * 4 + j
                        e.matmul(pk[bk][:, j, :], lhsT=scT[:, hh, :], rhs=v16[s][:, hh * 128:(hh + 1) * 128], start=True, stop=False)
                        e.matmul(pk[bk][:, j, :], lhsT=qtA[s][:, hh, :], rhs=SA[:, hh, :], start=False, stop=False)
                        ins = e.matmul(pk[bk][:, j, :], lhsT=qtB[s][:, hh, :], rhs=SB[:, hh, :], start=False, stop=True)
                    return ins
                P.op("pe", mo, reads=[b_scT, b_v16[s], b_qtA[s], b_qtB[s], b_SA, b_SB], writes=[b_pk[bk]])
                P.op("act", lambda e, half=half, bk=bk: e.copy(out=osb[:, half * 512:(half + 1) * 512],
                                                              in_=pk[bk][:].rearrange("p j v -> p (j v)")),
                     reads=[b_pk[bk]], writes=[b_osb])
            yield
            state_update(s, 1, SB, b_SB, SA, b_SA, (2, 3))
            yield
            P.op("dve", lambda e: e.tensor_tensor(out=sq[:], in0=osb[:], in1=osb[:], op=ALU.mult), reads=[b_osb], writes=[b_sq])
            P.op("dve", lambda e: e.tensor_reduce(out=ssq[:, 0, :], in_=sq[:].rearrange("p (h v) -> p h v", h=H), axis=AX.X, op=ALU.add),
                 reads=[b_sq], writes=[b_ssq])
            P.op("act", lambda e: e.activation(out=ssq[:, 1, :], in_=ssq[:, 0, :], func=AF.Ln, scale=1.0 / 128, bias=S.eps_t[:, 0:1]),
                 reads=[S.b_eps], writes=[b_ssq])
            P.op("act", lambda e: e.activation(out=ssq[:, 2, :], in_=ssq[:, 1, :], func=AF.Exp, scale=-0.5), writes=[b_ssq])
            P.op("dve", lambda e: e.tensor_tensor(out=sq[:].rearrange("p (h v) -> p h v", h=H), in0=osb[:].rearrange("p (h v) -> p h v", h=H),
                                                  in1=ssq[:, 2, :].unsqueeze(2).to_broadcast([128, H, 128]), op=ALU.mult),
                 reads=[b_osb, b_ssq], writes=[b_sq])
            P.op("dve", lambda e: e.tensor_tensor(out=on16[:], in0=sq[:], in1=gsn[s][:], op=ALU.mult), reads=[b_gsn[s]],
                 writes=[b_sq, b_on16])
            yield
            transpose_rows(P, nc, C, lambda kc: on16[:, kc * 128:(kc + 1) * 128], b_on16, tp, b_tp, onT[:], b_onT, evac="act")
            for half in range(2):
                bk = half

                def my(e, half=half, bk=bk):
                    ins = None
                    pyv = pk[bk][:].rearrange("p j v -> p (j v)")
                    for kc in range(KC):
                        ins = e.matmul(pyv, lhsT=onT[:, kc, :], rhs=wout[:, kc, half * 512:(half + 1) * 512],
                                       start=(kc == 0), stop=(kc == KC - 1))
                    return ins
                P.op("pe", my, reads=[b_onT, b_wout], writes=[b_pk[bk]])
                P.op("dve", lambda e, half=half, bk=bk: e.tensor_tensor(
                    out=hout[:, half * 512:(half + 1) * 512], in0=pk[bk][:].rearrange("p j v -> p (j v)"),
                    in1=h_t[s][:, half * 512:(half + 1) * 512], op=ALU.add), reads=[b_pk[bk], b_h[s]], writes=[b_hout])
            dst = h_out[i * 128:(i + 1) * 128, :]
            wr = [bufs_out[i // per]] if (i % per == per - 1) else []
            P.op("sp", lambda e: e.dma_start(out=dst, in_=hout[:]), reads=[b_hout], writes=wr, chan="st")

        for _ in front(0):
            pass
        for i in range(nblk):
            gf = front(i + 1) if i + 1 < nblk else None
            weave({"f": gf, "b": back(i)}, "fbbfbbfbb")
        P.barrier()


def phase_qkv(P, nc, C, T, h_in, qT_d, kT_d, v_d, mix_norm, w_in, q_norm, k_norm, tag="qkv"):
    H = 8
    NB = min(512, T)
    NS = NB // 128
    nblk = T // NB
    with contextlib.ExitStack() as st:
        S = alloc_norm_scratch(P, nc, st, tag)
        A = lambda name, shape, dt: _alloc(st, nc, name + "_" + tag, shape, dt)
        g_bc = A("gbc", [128, D], F32); b_g = Buf("g")
        load_bcast(P, nc, g_bc[:], b_g, mix_norm)
        gq = A("gq", [128, 16, 64], F32); gk = A("gk", [128, 16, 64], F32); b_gq = Buf("gq"); b_gk = Buf("gk")
        P.op("sp", lambda e: e.dma_start(out=gq[:, 0, :], in_=q_norm.partition_broadcast(128)), writes=[b_gq], chan="ld")
        P.op("sp", lambda e: e.dma_start(out=gk[:, 0, :], in_=k_norm.partition_broadcast(128)), writes=[b_gk], chan="ld")
        for r in range(1, 16):
            P.op("pool", lambda e, r=r: e.tensor_copy(out=gq[:, r, :], in_=gq[:, 0, :]), reads=[b_gq], writes=[b_gq])
            P.op("pool", lambda e, r=r: e.tensor_copy(out=gk[:, r, :], in_=gk[:, 0, :]), reads=[b_gk], writes=[b_gk])
        win = A("win", [128, KC, 3 * D], BF16); b_win = Buf("win")
        for q3 in range(3):
            srcw = w_in[:, q3 * D:(q3 + 1) * D].rearrange("(k p) f -> p k f", p=128)
            P.op("pool", lambda e, srcw=srcw, q3=q3: e.dma_start(out=win[:, :, q3 * D:(q3 + 1) * D], in_=srcw),
                 writes=[b_win], chan="ld")
        h_t = [A("h%d" % i, [128, NS, D], F32) for i in range(2)]; b_h = [Buf("h0"), Buf("h1")]
        xn = [A("xn%d" % i, [128, D], BF16) for i in range(2)]; b_xn = [Buf("xn0"), Buf("xn1")]
        xnT = [A("xnT%d" % i, [128, KC, 128], BF16) for i in range(2)]; b_xnT = [Buf("xnT0"), Buf("xnT1")]
        qf = [[A("qf%d%d" % (w, i), [128, D], F32) for i in range(2)] for w in range(2)]
        b_qf = [[Buf("qf%d%d" % (w, i)) for i in range(2)] for w in range(2)]
        sq = [[A("sq%d%d" % (w, i), [128, D], F32) for i in range(2)] for w in range(2)]
        b_sq = [[Buf("sq%d%d" % (w, i)) for i in range(2)] for w in range(2)]
        ssq = [[A("ssq%d%d" % (w, i), [128, 3, 16], F32) for i in range(2)] for w in range(2)]
        b_ssq = [[Buf("ssq%d%d" % (w, i)) for i in range(2)] for w in range(2)]
        qn16 = [A("qn16_%d" % w, [128, D], BF16) for w in range(2)]; b_qn16 = [Buf("qn0"), Buf("qn1")]
        qTs = [A("qTs%d" % i, [128, H, NB], BF16) for i in range(2)]; b_qTs = [Buf("qTs0"), Buf("qTs1")]
        kTs = [A("kTs%d" % i, [128, H, NB], BF16) for i in range(2)]; b_kTs = [Buf("kTs0"), Buf("kTs1")]
        v16 = [A("v16%d" % i, [128, NS, D], BF16) for i in range(2)]; b_v16 = [Buf("v0"), Buf("v1")]
        pz = [_psum(st, nc, "pz%d_%s" % (i, tag), [128, 512]) for i in range(4)]; b_pz = [Buf("pz%d" % i) for i in range(4)]
        tp = _psum(st, nc, "tp_" + tag, [128, KC * 128], BF16); b_tp = Buf("tp")
        tpq = [_psum(st, nc, "tpq%d_%s" % (i, tag), [128, KC * 128], BF16) for i in range(2)]; b_tpq = [Buf("tpq0"), Buf("tpq1")]

        def load(blk):
            s = blk % 2
            src = h_in[blk * NB:(blk + 1) * NB, :].rearrange("(s p) d -> p s d", p=128)
            P.op("sp", lambda e: e.dma_start(out=h_t[s][:], in_=src), writes=[b_h[s]], chan="ld")

        pzc = [0]

        def stageA(u):
            blk, ts = divmod(u, NS)
            s = blk % 2
            u2 = u % 2
            if ts == 0 and blk + 1 < nblk:
                load(blk + 1)
            rmsnorm_rows(P, nc, S, h_t[s][:, ts, :], b_h[s], g_bc[:], b_g, xn[u2][:], b_xn[u2], tag)
            transpose_rows(P, nc, C, lambda kc: xn[u2][:, kc * 128:(kc + 1) * 128], b_xn[u2], tp, b_tp, xnT[u2][:], b_xnT[u2], evac="dve")
            for which in range(3):
                for half in range(2):
                    cb = which * 2 + half
                    p4 = pzc[0] % 4
                    pzc[0] += 1

                    def mm(e, cb=cb, p4=p4):
                        ins = None
                        for kc in range(KC):
                            ins = e.matmul(pz[p4][:], lhsT=xnT[u2][:, kc, :], rhs=win[:, kc, cb * 512:(cb + 1) * 512],
                                           start=(kc == 0), stop=(kc == KC - 1))
                        return ins
                    P.op("pe", mm, reads=[b_xnT[u2], b_win], writes=[b_pz[p4]])
                    cs = slice(half * 512, half * 512 + 512)
                    if which == 2:
                        P.op("act", lambda e, p4=p4, cs=cs: e.copy(out=v16[s][:, ts, cs], in_=pz[p4][:]),
                             reads=[b_pz[p4]], writes=[b_v16[s]])
                    else:
                        P.op("dve", lambda e, p4=p4, cs=cs, which=which: e.tensor_copy(out=qf[which][u2][:, cs], in_=pz[p4][:]),
                             reads=[b_pz[p4]], writes=[b_qf[which][u2]])
                        P.op("act", lambda e, cs=cs, which=which: e.activation(out=sq[which][u2][:, cs], in_=qf[which][u2][:, cs], func=AF.Square),
                             reads=[b_qf[which][u2]], writes=[b_sq[which][u2]])

        def stageB(u):
            blk, ts = divmod(u, NS)
            s = blk % 2
            u2 = u % 2
            for which in range(2):
                gg, b_gg = (gq, b_gq) if which == 0 else (gk, b_gk)
                dstT, b_dstT = (qTs[s], b_qTs[s]) if which == 0 else (kTs[s], b_kTs[s])
                tpx, b_tpx = tpq[which], b_tpq[which]
                qf_, sq_, ssq_ = qf[which][u2], sq[which][u2], ssq[which][u2]
                bq, bs, bss = b_qf[which][u2], b_sq[which][u2], b_ssq[which][u2]
                P.op("dve", lambda e, sq_=sq_, ssq_=ssq_: e.tensor_reduce(out=ssq_[:, 0, :], in_=sq_[:].rearrange("p (g d) -> p g d", g=16),
                                                                         axis=AX.X, op=ALU.add), reads=[bs], writes=[bss])
                P.op("act", lambda e, ssq_=ssq_: e.activation(out=ssq_[:, 1, :], in_=ssq_[:, 0, :], func=AF.Ln, scale=1.0 / 64,
                                                              bias=S.eps_t[:, 0:1]), reads=[S.b_eps], writes=[bss])
                P.op("act", lambda e, ssq_=ssq_: e.activation(out=ssq_[:, 2, :], in_=ssq_[:, 1, :], func=AF.Exp, scale=-0.5), writes=[bss])
                P.op("dve", lambda e, qf_=qf_, ssq_=ssq_: e.tensor_tensor(
                    out=qf_[:].rearrange("p (g d) -> p g d", g=16), in0=qf_[:].rearrange("p (g d) -> p g d", g=16),
                    in1=ssq_[:, 2, :].unsqueeze(2).to_broadcast([128, 16, 64]), op=ALU.mult), reads=[bss], writes=[bq])
                P.op("dve", lambda e, gg=gg, qf_=qf_, which=which: e.tensor_tensor(
                    out=qn16[which][:], in0=qf_[:], in1=gg[:].rearrange("p g d -> p (g d)"), op=ALU.mult),
                    reads=[bq, b_gg], writes=[b_qn16[which]])

                def tq(e, tpx=tpx, which=which):
                    ins = None
                    for hh in range(H):
                        ins = e.transpose(out=tpx[:, hh * 128:(hh + 1) * 128], in_=qn16[which][:, hh * 128:(hh + 1) * 128],
                                          identity=C.ident[:])
                    return ins
                P.op("pe", tq, reads=[b_qn16[which], C.b_ident], writes=[b_tpx])
                P.op("dve", lambda e, tpx=tpx, dstT=dstT, ts=ts: e.tensor_copy(
                    out=dstT[:, :, ts * 128:(ts + 1) * 128], in_=tpx[:].rearrange("p (k t) -> p k t", k=H)),
                    reads=[b_tpx], writes=[b_dstT])
            if ts == NS - 1:
                tsl = slice(blk * NB, (blk + 1) * NB)
                P.op("sp", lambda e: e.dma_start(out=qT_d[:, :, tsl].rearrange("h p t -> p h t"), in_=qTs[s][:]),
                     reads=[b_qTs[s]], chan="st")
                P.op("sp", lambda e: e.dma_start(out=kT_d[:, :, tsl].rearrange("h p t -> p h t"), in_=kTs[s][:]),
                     reads=[b_kTs[s]], chan="st")
                P.op("sp", lambda e: e.dma_start(out=v_d[tsl, :].rearrange("(s p) d -> p s d", p=128), in_=v16[s][:]),
                     reads=[b_v16[s]], chan="st")

        load(0)
        nu = nblk * NS
        stageA(0)
        for u in range(nu):
            if u + 1 < nu:
                stageA(u + 1)
            stageB(u)
        P.barrier()


def phase_attn(P, nc, C, T, qT_d, kT_d, v_d, on_d, q_norm, k_norm, lq1, lk1, lq2, lk2, sub_norm, lambda_init, tag="att"):
    H = 8
    NI = T // 256
    NJ = T // 128
    with contextlib.ExitStack() as st:
        A = lambda name, shape, dt: _alloc(st, nc, name + "_" + tag, shape, dt)
        vec = A("vec", [128, 6, 64], F32); b_vec = Buf("vec")
        for i, v_ in enumerate((q_norm, k_norm, lq1, lk1, lq2, lk2)):
            P.op("sp", lambda e, i=i, v_=v_: e.dma_start(out=vec[:, i, :], in_=v_.partition_broadcast(128)), writes=[b_vec], chan="ld")
        subn = A("subn", [128, 128], F32); b_subn = Buf("subn")
        load_bcast(P, nc, subn[:], b_subn, sub_norm)
        P.op("dve", lambda e: e.tensor_scalar(out=subn[:], in0=subn[:], scalar1=float(1.0 - lambda_init), scalar2=None, op0=ALU.mult),
             writes=[b_subn])
        sc = A("sc", [128, 12], F32); b_sc = Buf("sc")
        junk = A("junk", [128, 128], F32); b_junk = Buf("junk")
        eps_t = A("eps", [128, 1], F32); b_eps = Buf("eps")
        P.op("dve", lambda e: e.memset(eps_t[:], EPS), writes=[b_eps])
        P.op("dve", lambda e: e.tensor_reduce(out=sc[:, 0:2], in_=vec[:, 0:2, :], axis=AX.X, op=ALU.max, apply_absolute_value=True),
             reads=[b_vec], writes=[b_sc])
        P.op("dve", lambda e: e.tensor_tensor(out=sc[:, 2:3], in0=sc[:, 0:1], in1=sc[:, 1:2], op=ALU.mult), writes=[b_sc])
        P.op("dve", lambda e: e.tensor_scalar(out=sc[:, 3:4], in0=sc[:, 2:3], scalar1=-8.0, scalar2=None, op0=ALU.mult), writes=[b_sc])
        P.op("dve", lambda e: e.scalar_tensor_tensor(out=junk[:, 0:64], in0=vec[:, 2, :], scalar=1.0, in1=vec[:, 3, :], op0=ALU.mult,
                                                     op1=ALU.mult, accum_out=sc[:, 4:5]), reads=[b_vec], writes=[b_junk, b_sc])
        P.op("dve", lambda e: e.scalar_tensor_tensor(out=junk[:, 0:64], in0=vec[:, 4, :], scalar=1.0, in1=vec[:, 5, :], op0=ALU.mult,
                                                     op1=ALU.mult, accum_out=sc[:, 5:6]), reads=[b_vec], writes=[b_junk, b_sc])
        P.op("act", lambda e: e.activation(out=sc[:, 6:8], in_=sc[:, 4:6], func=AF.Exp), writes=[b_sc])
        P.op("dve", lambda e: e.tensor_tensor(out=sc[:, 8:9], in0=sc[:, 7:8], in1=sc[:, 6:7], op=ALU.subtract), writes=[b_sc])
        P.op("dve", lambda e: e.tensor_scalar(out=sc[:, 8:9], in0=sc[:, 8:9], scalar1=-float(lambda_init), scalar2=None, op0=ALU.add),
             writes=[b_sc])
        NM = 2 * NI + 1
        dtab_i = A("dtabi", [128, NM], mybir.dt.int32); dtab = A("dtab", [128, NM], F32); b_dtab = Buf("dtab")
        P.op("pool", lambda e: e.iota(out=dtab_i[:], pattern=[[-128, NM]], base=128, channel_multiplier=1), writes=[b_dtab])
        P.op("dve", lambda e: e.tensor_copy(out=dtab[:], in_=dtab_i[:]), writes=[b_dtab])
        btab = A("btab", [128, H, NM], F32); b_btab = Buf("btab")
        for hh in range(H):
            slope = 2.0 ** (-8.0 * (hh + 1) / H)
            P.op("dve", lambda e, hh=hh, slope=slope: e.tensor_scalar(out=btab[:, hh, :], in0=dtab[:], scalar1=slope, scalar2=sc[:, 3:4],
                                                                      op0=ALU.mult, op1=ALU.add), reads=[b_dtab, b_sc], writes=[b_btab])
        kT_t = [A("kT%d" % i, [128, 2, T], BF16) for i in range(2)]; b_kT = [Buf("kT0"), Buf("kT1")]
        qT_t = [A("qT%d" % i, [128, 2, T], BF16) for i in range(2)]; b_qT = [Buf("qT0"), Buf("qT1")]
        v_t = [A("vt%d" % i, [128, NJ, 129], BF16) for i in range(2)]; b_v = [Buf("vt0"), Buf("vt1")]
        qaug_i = A("qaugi", [128, 256], mybir.dt.int32); b_qaug = Buf("qaug")
        P.op("pool", lambda e: e.iota(out=qaug_i[64:65, :], pattern=[[-8, 256]], base=0, channel_multiplier=0), writes=[b_qaug])
        for i in range(2):
            P.op("pool", lambda e, i=i: e.memset(v_t[i][:, :, 128:129], 1.0), writes=[b_v[i]])
            for c in range(2):
                for I in range(NI):
                    P.op("dve", lambda e, i=i, c=c, I=I: e.tensor_copy(out=qT_t[i][64:65, c, I * 256:(I + 1) * 256], in_=qaug_i[64:65, :]),
                         reads=[b_qaug], writes=[b_qT[i]])
        NPT = 5
        PT = [A("PT%d" % i, [128, 2, 256], BF16) for i in range(NPT)]; b_PT = [Buf("PT%d" % i) for i in range(NPT)]
        tri = A("tri", [128, 2, 128], BF16); b_tri = Buf("tri")
        P.op("pool", lambda e: e.memset(tri[:], 1.0), writes=[b_tri])
        P.op("pool", lambda e: e.affine_select(out=tri[:], in_=tri[:], pattern=[[0, 2], [1, 128]], compare_op=ALU.is_ge, fill=0.0,
                                               base=0, channel_multiplier=-1), writes=[b_tri])
        o_t = A("o", [128, 128], F32); b_o = Buf("o")
        rz = A("rz", [128, 8], F32); b_rz = Buf("rz")
        on16 = [A("on16_%d" % i, [128, 128], BF16) for i in range(2)]; b_on16 = [Buf("on0"), Buf("on1")]
        NPS = 4
        pS = [_psum(st, nc, "pS%d_%s" % (i, tag), [128, 2, 256]) for i in range(NPS)]; b_pS = [Buf("pS%d" % i) for i in range(NPS)]
        acc = [[_psum(st, nc, "acc%d%d_%s" % (par, sb, tag), [128, 512]) for sb in range(2)] for par in range(2)]
        b_acc = [[Buf("acc%d%d" % (par, sb)) for sb in range(2)] for par in range(2)]

        def load_head(hh):
            s = hh % 2
            slope = 2.0 ** (-8.0 * (hh + 1) / H)
            for c in range(2):
                P.op("sp", lambda e, c=c: e.dma_start(out=kT_t[s][0:64, c, :], in_=kT_d[hh, c * 64:(c + 1) * 64, :]),
                     writes=[b_kT[s]], chan="ld")
                P.op("sp", lambda e, c=c: e.dma_start(out=qT_t[s][0:64, c, :], in_=qT_d[hh, c * 64:(c + 1) * 64, :]),
                     writes=[b_qT[s]], chan="ld")
            P.op("dve", lambda e: e.memset(kT_t[s][64:65, :, :], slope), writes=[b_kT[s]])
            P.op("sp", lambda e: e.dma_start(out=v_t[s][:, :, 0:128],
                                             in_=v_d[:, hh * 128:(hh + 1) * 128].rearrange("(j p) v -> p j v", p=128)),
                 writes=[b_v[s]], chan="ld")

        SKIP = 160.0
        pairs = []
        for hh in range(H):
            slope = 2.0 ** (-8.0 * (hh + 1) / H)
            for I in range(NI):
                js = [j for j in range(2 * I + 2) if slope * max(0, 256 * I - (128 * j + 127)) < SKIP]
                for j in js:
                    pairs.append((hh, I, j, js[0], js))
        fin = [0]

        def emit_qk(n):
            hh, I, j, j0, js = pairs[n]
            s = hh % 2
            lo = 128 if j == 2 * I + 1 else 0
            ps = n % NPS

            def mqk(e):
                ins = None
                for c in range(2):
                    ins = e.matmul(pS[ps][:, c, lo:256], lhsT=kT_t[s][0:65, c, j * 128:(j + 1) * 128],
                                   rhs=qT_t[s][0:65, c, I * 256 + lo:(I + 1) * 256], start=True, stop=True)
                return ins
            P.op("pe", mqk, reads=[b_kT[s], b_qT[s]], writes=[b_pS[ps]])

        def emit_exp(n):
            hh, I, j, j0, js = pairs[n]
            lo = 128 if j == 2 * I + 1 else 0
            ps = n % NPS
            pt = n % NPT
            mi = (2 * I - j) + 1
            P.op("act", lambda e: e.activation(out=PT[pt][:, :, lo:256], in_=pS[ps][:, :, lo:256], func=AF.Exp, scale=0.125,
                                               bias=btab[:, hh, mi:mi + 1]), reads=[b_pS[ps], b_btab], writes=[b_PT[pt]])
            if j >= 2 * I:
                sbd = j - 2 * I
                P.op("dve", lambda e: e.tensor_tensor(out=PT[pt][:, :, sbd * 128:(sbd + 1) * 128], in0=PT[pt][:, :, sbd * 128:(sbd + 1) * 128],
                                                      in1=tri[:], op=ALU.mult), reads=[b_tri], writes=[b_PT[pt]])

        def emit_pv(n):
            hh, I, j, j0, js = pairs[n]
            s = hh % 2
            lo = 128 if j == 2 * I + 1 else 0
            pt = n % NPT
            for sb in range(lo // 128, 2):
                last = (j == 2 * I + sb)

                par = I % 2

                def mpv(e, sb=sb, last=last, par=par):
                    ins = None
                    for c in range(2):
                        ins = e.matmul(acc[par][sb][:, c * 256:c * 256 + 129], lhsT=PT[pt][:, c, sb * 128:(sb + 1) * 128], rhs=v_t[s][:, j, :],
                                       start=(j == j0 and c == 0), stop=last, skip_group_check=True)
                    return ins
                P.op("pe", mpv, reads=[b_PT[pt], b_v[s]], writes=[b_acc[par][sb]])
                if last:
                    f2 = fin[0] % 2
                    fin[0] += 1
                    a0, a1 = acc[par][sb][:, 0:129], acc[par][sb][:, 256:385]
                    rb = [b_acc[par][sb]]
                    P.op("dve", lambda e, a0=a0: e.reciprocal(out=rz[:, 0:1], in_=a0[:, 128:129]), reads=rb, writes=[b_rz])
                    P.op("dve", lambda e, a1=a1: e.reciprocal(out=rz[:, 1:2], in_=a1[:, 128:129]), reads=rb, writes=[b_rz])
                    P.op("dve", lambda e: e.tensor_tensor(out=rz[:, 2:3], in0=rz[:, 1:2], in1=sc[:, 8:9], op=ALU.mult),
                         reads=[b_sc], writes=[b_rz])
                    P.op("dve", lambda e, a0=a0: e.tensor_scalar(out=o_t[:], in0=a0[:, 0:128], scalar1=rz[:, 0:1], scalar2=None,
                                                                 op0=ALU.mult), reads=rb + [b_rz], writes=[b_o])
                    P.op("dve", lambda e, a1=a1: e.scalar_tensor_tensor(out=o_t[:], in0=a1[:, 0:128], scalar=rz[:, 2:3], in1=o_t[:],
                                                                        op0=ALU.mult, op1=ALU.add), reads=rb + [b_rz], writes=[b_o])
                    P.op("dve", lambda e: e.scalar_tensor_tensor(out=junk[:], in0=o_t[:], scalar=1.0, in1=o_t[:], op0=ALU.mult,
                                                                 op1=ALU.mult, accum_out=rz[:, 3:4]), reads=[b_o], writes=[b_junk, b_rz])
                    P.op("act", lambda e: e.activation(out=rz[:, 4:5], in_=rz[:, 3:4], func=AF.Ln, scale=1.0 / 128, bias=eps_t[:, 0:1]),
                         reads=[b_eps], writes=[b_rz])
                    P.op("act", lambda e: e.activation(out=rz[:, 5:6], in_=rz[:, 4:5], func=AF.Exp, scale=-0.5), writes=[b_rz])
                    P.op("dve", lambda e, f2=f2: e.scalar_tensor_tensor(out=on16[f2][:], in0=o_t[:], scalar=rz[:, 5:6], in1=subn[:],
                                                                        op0=ALU.mult, op1=ALU.mult),
                         reads=[b_o, b_rz, b_subn], writes=[b_on16[f2]])
                    t0 = I * 256 + sb * 128
                    P.op("sp", lambda e, f2=f2, t0=t0: e.dma_start(out=on_d[t0:t0 + 128, hh * 128:(hh + 1) * 128], in_=on16[f2][:]),
                         reads=[b_on16[f2]], chan="st")

        LA = 3
        load_head(0)
        loaded = {0}
        npairs = len(pairs)
        for n in range(min(LA, npairs)):
            emit_qk(n)
        for n in range(npairs):
            hh = pairs[n][0]
            if hh + 1 < H and (hh + 1) not in loaded:
                load_head(hh + 1)
                loaded.add(hh + 1)
            if n + LA < npairs:
                emit_qk(n + LA)
            emit_exp(n)
            emit_pv(n)
        P.barrier()


def phase_attn_out(P, nc, C, T, h_in, on_d, h_out, w_out, tag="ao"):
    nblk = T // 128
    with contextlib.ExitStack() as st:
        A = lambda name, shape, dt: _alloc(st, nc, name + "_" + tag, shape, dt)
        wout = A("wout", [128, KC, D], BF16); b_wout = Buf("wout")
        P.op("pool", lambda e: e.dma_start(out=wout[:], in_=w_out.rearrange("(k p) f -> p k f", p=128)), writes=[b_wout], chan="ld")
        h_t = [A("h%d" % i, [128, D], F32) for i in range(2)]; b_h = [Buf("h0"), Buf("h1")]
        on = [A("on%d" % i, [128, D], BF16) for i in range(2)]; b_on = [Buf("on0"), Buf("on1")]
        onT = A("onT", [128, KC, 128], BF16); b_onT = Buf("onT")
        ho = [A("ho%d" % i, [128, D], F32) for i in range(2)]; b_ho = [Buf("ho0"), Buf("ho1")]
        tp = _psum(st, nc, "tp_" + tag, [128, KC * 128], BF16); b_tp = Buf("tp")
        py = [_psum(st, nc, "py%d_%s" % (i, tag), [128, 512]) for i in range(2)]; b_py = [Buf("py0"), Buf("py1")]

        def load(i):
            s = i % 2
            P.op("sp", lambda e: e.dma_start(out=h_t[s][:], in_=h_in[i * 128:(i + 1) * 128, :]), writes=[b_h[s]], chan="ld")
            P.op("sp", lambda e: e.dma_start(out=on[s][:], in_=on_d[i * 128:(i + 1) * 128, :]), writes=[b_on[s]], chan="ld")
        load(0)
        for i in range(nblk):
            s = i % 2
            if i + 1 < nblk:
                load(i + 1)
            transpose_rows(P, nc, C, lambda kc: on[s][:, kc * 128:(kc + 1) * 128], b_on[s], tp, b_tp, onT[:], b_onT, evac="dve")
            for half in range(2):
                def my(e, half=half):
                    ins = None
                    for kc in range(KC):
                        ins = e.matmul(py[half][:], lhsT=onT[:, kc, :], rhs=wout[:, kc, half * 512:(half + 1) * 512],
                                       start=(kc == 0), stop=(kc == KC - 1))
                    return ins
                P.op("pe", my, reads=[b_onT, b_wout], writes=[b_py[half]])
                P.op("dve", lambda e, half=half: e.tensor_tensor(out=ho[s][:, half * 512:(half + 1) * 512], in0=py[half][:],
                                                                 in1=h_t[s][:, half * 512:(half + 1) * 512], op=ALU.add),
                     reads=[b_py[half], b_h[s]], writes=[b_ho[s]])
            P.op("sp", lambda e: e.dma_start(out=h_out[i * 128:(i + 1) * 128, :], in_=ho[s][:]), reads=[b_ho[s]], chan="st")
        P.barrier()


def build_program(T, phases=("glu0",), N=1024):
    nc = bass.Bass("TRN2", target_bir_lowering=False)
    stack = contextlib.ExitStack()
    with stack:
        P = Prog(nc, stack)
        C = Ctx()
        inp = lambda name, shape: nc.dram_tensor(name, list(shape), F32, kind="ExternalInput").ap()
        x = inp("x", [T, D])
        out = nc.dram_tensor("out", [T, D], F32, kind="ExternalOutput").ap()
        emit_consts(P, nc, stack, C)
        nblk = T // N
        cur, b_cur = x, [Buf("x%d" % i) for i in range(nblk)]
        nscratch = 0
        for pi, ph in enumerate(phases):
            last = pi == len(phases) - 1
            if last:
                nxt = out
            else:
                nxt = nc.dram_tensor("scr%d" % nscratch, [T, D], F32, kind="Internal").ap()
                nscratch += 1
            b_nxt = [Buf("s%d_%d" % (pi, i)) for i in range(nblk)]
            if ph == "glu0":
                phase_glu(P, nc, C, T, cur, nxt, b_cur, b_nxt, inp("l0_ffn_norm", [D]),
                          inp("l0_ffn_w_gate_up", [D, 2 * 2816]), inp("l0_ffn_w_down", [2816, D]), 2816, 1, N=N, tag="f0")
            elif ph == "hgrn":
                phase_hgrn(P, nc, C, T, cur, nxt, b_cur, b_nxt, N, inp("l0_mix_norm", [D]), inp("l0_hgrn_w_in", [D, 4 * D]),
                           inp("l0_hgrn_out_norm", [D]), inp("l0_hgrn_w_out", [D, D]), inp("lower_bounds", [3, D]))
            elif ph == "attn":
                bf = lambda name, shape: nc.dram_tensor(name, list(shape), BF16, kind="Internal").ap()
                qT_d, kT_d = bf("qT_d", [8, 128, T]), bf("kT_d", [8, 128, T])
                v_d, on_d = bf("v_d", [T, D]), bf("on_d", [T, D])
                qn, kn = inp("l1_q_norm", [64]), inp("l1_k_norm", [64])
                phase_qkv(P, nc, C, T, cur, qT_d, kT_d, v_d, inp("l1_mix_norm", [D]), inp("l1_diff_w_in", [D, 3 * D]), qn, kn)
                phase_attn(P, nc, C, T, qT_d, kT_d, v_d, on_d, qn, kn, inp("l1_lambda_q1", [64]), inp("l1_lambda_k1", [64]),
                           inp("l1_lambda_q2", [64]), inp("l1_lambda_k2", [64]), inp("l1_diff_sub_norm", [128]),
                           0.8 - 0.6 * float(np.exp(-0.3 * 1)))
                phase_attn_out(P, nc, C, T, cur, on_d, nxt, inp("l1_diff_w_out", [D, D]))
            elif ph == "moe":
                phase_glu(P, nc, C, T, cur, nxt, b_cur, b_nxt, inp("l1_ffn_norm", [D]),
                          inp("l1_moe_w_gate_up", [8, D, 2 * 3584]), inp("l1_moe_w_down", [8, 3584, D]), 3584, 8,
                          router=inp("l1_router", [D, 8]), N=N, tag="f1")
            cur, b_cur = nxt, b_nxt
        P.barrier()
        print("ops:", P.n_ops, "sems:", P.nsem)
    return nc


_NC_CACHE = {}
PHASES = ("hgrn", "glu0", "attn", "moe")


def kernel(**inputs):
    x = np.ascontiguousarray(np.asarray(inputs["x"], dtype=np.float32))
    B, T, _ = x.shape
    if T not in _NC_CACHE:
        _NC_CACHE[T] = build_program(T, phases=PHASES, N=1024)
    nc = _NC_CACHE[T]
    shared = {k: np.ascontiguousarray(np.asarray(v, dtype=np.float32)) for k, v in inputs.items() if k != "x"}
    in_maps = []
    for b in range(B):
        m = dict(shared)
        m["x"] = x[b]
        in_maps.append(m)
    res = run_bass_kernel_spmd(nc, in_maps, core_ids=list(range(B)))
    return np.stack([np.asarray(r["out"], dtype=np.float32) for r in res.results], axis=0)
```
